# Optimizing a Trainium2 kernel written in Bass

```python
import jax, jax.numpy as jnp
from jax import lax
import numpy as np

D_MODEL = 2048
BATCH = 2
SEQ = 8192
DEPTH = 4

SGU_WIDTH = 1024
SGU_GROUPS = 8
SGU_GROUP_CH = SGU_WIDTH // SGU_GROUPS
CHUNK = 128
HEAD_DIM = 128
HEADS_PER_GROUP = 4
DILATED_GROUPS = ((128, 1), (512, 4), (2048, 16))
ATT_HEADS = HEADS_PER_GROUP * len(DILATED_GROUPS)
ATT_WIDTH = ATT_HEADS * HEAD_DIM
MERGED_ATT_WIDTH = HEADS_PER_GROUP * HEAD_DIM
QBLK = 128
ROT_DIM = HEAD_DIM // 4
ROPE_THETA = 500000.0
PROJ_SIZES = (SGU_WIDTH, SGU_WIDTH, ATT_WIDTH, ATT_WIDTH, ATT_WIDTH, D_MODEL, D_MODEL)
PROJ_WIDTH = sum(PROJ_SIZES)
PROJ_SPLITS = tuple(int(s) for s in np.cumsum(PROJ_SIZES)[:-1])
N_EXPERT_GROUPS = 4
EXPERTS_PER_GROUP = 8
N_EXPERTS = N_EXPERT_GROUPS * EXPERTS_PER_GROUP
TOP_K = 2
EXPERT_FF = 512
MOE_BLK = 256
PLE_DIM = 256
ALPHA = (2 * DEPTH) ** 0.25
BETA = (8 * DEPTH) ** -0.25
LN_EPS = 1e-5

kernel_name = "hybrid_sgu_dilated_attn_hmoe_deepnorm"


def layer_norm(x, g, b):
    xf = x.astype(jnp.float32)
    mu = xf.mean(-1, keepdims=True)
    xc = xf - mu
    var = jnp.mean(xc * xc, -1, keepdims=True)
    return (xc * lax.rsqrt(var + LN_EPS) * g + b).astype(x.dtype)


def rotary(x, cos, sin):
    half = ROT_DIM // 2
    xf = x.astype(jnp.float32)
    x1, x2 = xf[..., :half], xf[..., half:ROT_DIM]
    out = jnp.concatenate([x1 * cos - x2 * sin, x2 * cos + x1 * sin, xf[..., ROT_DIM:]], axis=-1)
    return out.astype(x.dtype)


def chunked_sgu(u, v, ln_g, ln_b, w_s, b_s):
    B, S, _ = u.shape
    u = jax.nn.gelu(u)
    v = layer_norm(jax.nn.gelu(v), ln_g, ln_b)
    vb = v.reshape(B, S // CHUNK, CHUNK, SGU_GROUPS, SGU_GROUP_CH)
    causal = jnp.tril(jnp.ones((CHUNK, CHUNK), dtype=w_s.dtype))
    ws = (w_s * causal).astype(v.dtype)
    z = jnp.einsum('gts,bnsgc->bntgc', ws, vb) + b_s.T.astype(v.dtype)[None, None, :, :, None]
    return u * z.reshape(B, S, SGU_WIDTH)


def dilated_window_attention(q, k, v, window, dilation):
    B, S, H, Dh = q.shape
    span = window // dilation
    L = S // dilation
    nb = -(-L // QBLK)
    Lp = nb * QBLK

    def to_blocks(a):
        a = a.reshape(B, L, dilation, H, Dh).transpose(0, 2, 1, 3, 4)
        a = jnp.pad(a, ((0, 0), (0, 0), (0, Lp - L), (0, 0), (0, 0)))
        return a.reshape(B, dilation, nb, QBLK, H, Dh)

    def band(a):
        prev = jnp.concatenate([jnp.zeros_like(a[:, :, :1]), a[:, :, :-1]], axis=2)
        return jnp.concatenate([prev, a], axis=3)

    qb, kb, vb = to_blocks(q), to_blocks(k), to_blocks(v)
    kw, vw = band(kb), band(vb)
    s = jnp.einsum('bcnqhd,bcnkhd->bcnhqk', qb, kw).astype(jnp.float32) * (Dh ** -0.5)
    qi = jnp.arange(QBLK)[:, None]
    kj = jnp.arange(2 * QBLK)[None, :]
    dist = qi + QBLK - kj
    blk = jnp.arange(nb)[:, None, None]
    valid = (dist >= 0) & (dist <= span) & ((blk - 1) * QBLK + kj >= 0)
    s = jnp.where(valid[None, None, :, None], s, -jnp.inf)
    m = s.max(-1, keepdims=True)
    e = jnp.exp(s - m)
    den = e.sum(-1, keepdims=True)
    o = jnp.einsum('bcnhqk,bcnkhd->bcnqhd', (e / den).astype(v.dtype), vw).astype(jnp.float32)
    lse = (m + jnp.log(den))[..., 0].transpose(0, 1, 2, 4, 3)
    o = o.reshape(B, dilation, Lp, H, Dh)[:, :, :L].transpose(0, 2, 1, 3, 4).reshape(B, S, H, Dh)
    lse = lse.reshape(B, dilation, Lp, H)[:, :, :L].transpose(0, 2, 1, 3).reshape(B, S, H)
    return o, lse


def dilated_mixture(q, k, v):
    B, S = q.shape[:2]
    outs, lses = [], []
    for g, (window, dilation) in enumerate(DILATED_GROUPS):
        sl = slice(g * HEADS_PER_GROUP, (g + 1) * HEADS_PER_GROUP)
        o, l = dilated_window_attention(q[:, :, sl], k[:, :, sl], v[:, :, sl], window, dilation)
        outs.append(o)
        lses.append(l)
    o = jnp.stack(outs, 0)
    w = jax.nn.softmax(jnp.stack(lses, 0), axis=0)
    merged = jnp.sum(w[..., None] * o, axis=0)
    return merged.reshape(B, S, MERGED_ATT_WIDTH).astype(q.dtype)


def hierarchical_moe(h, w_grp, b_grp, w_rt, b_rt, w1, w3, w2):
    B, S, D = h.shape
    N = B * S
    hf = h.reshape(N, D)
    grp_logits = (hf @ w_grp.astype(h.dtype)).astype(jnp.float32) + b_grp
    grp_prob = jax.nn.softmax(grp_logits, axis=-1)
    g_sel = jnp.argmax(grp_logits, axis=-1).astype(jnp.int32)
    p_g = jnp.take_along_axis(grp_prob, g_sel[:, None], axis=1)[:, 0]
    exp_logits = ((hf @ w_rt.astype(h.dtype)).astype(jnp.float32) + b_rt).reshape(N, N_EXPERT_GROUPS, EXPERTS_PER_GROUP)
    in_grp = jnp.take_along_axis(exp_logits, g_sel[:, None, None], axis=1)[:, 0]
    top_v, top_i = lax.top_k(in_grp, TOP_K)
    gate = jax.nn.softmax(top_v, axis=-1) * p_g[:, None]
    e_id = g_sel[:, None] * EXPERTS_PER_GROUP + top_i.astype(jnp.int32)

    A = N * TOP_K
    e_flat = e_id.reshape(A)
    tok = jnp.repeat(jnp.arange(N, dtype=jnp.int32), TOP_K)
    w_flat = gate.reshape(A)
    order = jnp.argsort(e_flat)
    e_s, tok_s, w_s = e_flat[order], tok[order], w_flat[order]
    counts = jnp.zeros((N_EXPERTS,), jnp.int32).at[e_flat].add(1)
    start = jnp.cumsum(counts) - counts
    padded = ((counts + MOE_BLK - 1) // MOE_BLK) * MOE_BLK
    pend = jnp.cumsum(padded)
    pstart = pend - padded
    dest = pstart[e_s] + (jnp.arange(A, dtype=jnp.int32) - start[e_s])
    n_blocks = (A + N_EXPERTS * (MOE_BLK - 1) + MOE_BLK - 1) // MOE_BLK
    P = n_blocks * MOE_BLK
    buf_tok = jnp.full((P,), N, jnp.int32).at[dest].set(tok_s)
    buf_w = jnp.zeros((P,), jnp.float32).at[dest].set(w_s)
    blk_e = jnp.minimum(jnp.searchsorted(pend, jnp.arange(n_blocks, dtype=jnp.int32) * MOE_BLK, side='right'), N_EXPERTS - 1).astype(jnp.int32)
    xpad = jnp.concatenate([hf, jnp.zeros((1, D), hf.dtype)], axis=0)
    xb = xpad[buf_tok].reshape(n_blocks, MOE_BLK, D)

    def expert_block(args):
        xblk, e = args
        hid = jax.nn.silu(xblk @ w1[e].astype(xblk.dtype)) * (xblk @ w3[e].astype(xblk.dtype))
        return hid @ w2[e].astype(xblk.dtype)

    yb = lax.map(expert_block, (xb, blk_e)).reshape(P, D)
    y = jax.ops.segment_sum(yb * buf_w.astype(yb.dtype)[:, None], buf_tok, num_segments=N + 1)[:N]
    return y.reshape(B, S, D)


def setup_inputs(seed: int = 0) -> dict:
    key = jax.random.key(seed)
    ks = jax.random.split(key, 32)
    f32 = jnp.float32
    n = lambda k, shape, scale: jax.random.normal(k, shape, f32) * scale
    x = jax.random.normal(ks[0], (BATCH, SEQ, D_MODEL), f32)
    p = jax.random.normal(ks[1], (DEPTH, BATCH, SEQ, PLE_DIM), f32)
    offsets = jax.random.randint(ks[2], (BATCH, 1), 0, 4096, dtype=jnp.int32)
    positions = offsets + jnp.arange(SEQ, dtype=jnp.int32)[None, :]
    col_scale = jnp.ones((PROJ_WIDTH,), f32).at[PROJ_SPLITS[3]:PROJ_SPLITS[4]].set(BETA)
    w_in = n(ks[3], (DEPTH, D_MODEL, PROJ_WIDTH), D_MODEL ** -0.5) * col_scale
    w_s = n(ks[4], (DEPTH, SGU_GROUPS, CHUNK, CHUNK), CHUNK ** -0.5)
    b_s = 1.0 + n(ks[5], (DEPTH, SGU_GROUPS, CHUNK), 0.1)
    ln_v_g = 1.0 + n(ks[6], (DEPTH, SGU_WIDTH), 0.02)
    ln_v_b = n(ks[7], (DEPTH, SGU_WIDTH), 0.02)
    w_a = n(ks[8], (DEPTH, SGU_WIDTH, D_MODEL), BETA * SGU_WIDTH ** -0.5)
    w_b = n(ks[9], (DEPTH, MERGED_ATT_WIDTH, D_MODEL), BETA * MERGED_ATT_WIDTH ** -0.5)
    w_o = n(ks[10], (DEPTH, D_MODEL, D_MODEL), BETA * D_MODEL ** -0.5)
    ln1_g = 1.0 + n(ks[11], (DEPTH, D_MODEL), 0.02)
    ln1_b = n(ks[12], (DEPTH, D_MODEL), 0.02)
    w_grp = n(ks[13], (DEPTH, D_MODEL, N_EXPERT_GROUPS), D_MODEL ** -0.5)
    b_grp = n(ks[14], (DEPTH, N_EXPERT_GROUPS), 0.01)
    w_rt = n(ks[15], (DEPTH, D_MODEL, N_EXPERTS), D_MODEL ** -0.5)
    b_rt = n(ks[16], (DEPTH, N_EXPERTS), 0.01)
    w1 = n(ks[17], (DEPTH, N_EXPERTS, D_MODEL, EXPERT_FF), D_MODEL ** -0.5)
    w3 = n(ks[18], (DEPTH, N_EXPERTS, D_MODEL, EXPERT_FF), D_MODEL ** -0.5)
    w2 = n(ks[19], (DEPTH, N_EXPERTS, EXPERT_FF, D_MODEL), BETA * EXPERT_FF ** -0.5)
    w_pg = n(ks[20], (DEPTH, D_MODEL, D_MODEL), D_MODEL ** -0.5)
    w_pp = n(ks[21], (DEPTH, PLE_DIM, D_MODEL), BETA * PLE_DIM ** -0.5)
    ln2_g = 1.0 + n(ks[22], (DEPTH, D_MODEL), 0.02)
    ln2_b = n(ks[23], (DEPTH, D_MODEL), 0.02)
    return {"x": x, "p": p, "positions": positions, "w_in": w_in, "w_s": w_s, "b_s": b_s,
            "ln_v_g": ln_v_g, "ln_v_b": ln_v_b, "w_a": w_a, "w_b": w_b, "w_o": w_o,
            "ln1_g": ln1_g, "ln1_b": ln1_b, "w_grp": w_grp, "b_grp": b_grp, "w_rt": w_rt,
            "b_rt": b_rt, "w1": w1, "w3": w3, "w2": w2, "w_pg": w_pg, "w_pp": w_pp,
            "ln2_g": ln2_g, "ln2_b": ln2_b}


def reference(x, p, positions, w_in, w_s, b_s, ln_v_g, ln_v_b, w_a, w_b, w_o, ln1_g, ln1_b,
              w_grp, b_grp, w_rt, b_rt, w1, w3, w2, w_pg, w_pp, ln2_g, ln2_b):
    B, S, D = x.shape
    dt = x.dtype
    inv_freq = ROPE_THETA ** (-jnp.arange(0, ROT_DIM, 2, dtype=jnp.float32) / ROT_DIM)
    ang = positions.astype(jnp.float32)[..., None] * inv_freq
    cos, sin = jnp.cos(ang)[:, :, None, :], jnp.sin(ang)[:, :, None, :]

    for i in range(DEPTH):
        proj = x @ w_in[i].astype(dt)
        u, vs, q, k, va, ga, gb = jnp.split(proj, PROJ_SPLITS, axis=-1)
        a_out = chunked_sgu(u, vs, ln_v_g[i], ln_v_b[i], w_s[i], b_s[i]) @ w_a[i].astype(dt)
        q = rotary(q.reshape(B, S, ATT_HEADS, HEAD_DIM), cos, sin)
        k = rotary(k.reshape(B, S, ATT_HEADS, HEAD_DIM), cos, sin)
        va = va.reshape(B, S, ATT_HEADS, HEAD_DIM)
        b_out = dilated_mixture(q, k, va) @ w_b[i].astype(dt)
        mixed = (jax.nn.sigmoid(ga) * a_out + jax.nn.sigmoid(gb) * b_out) @ w_o[i].astype(dt)
        x = layer_norm(ALPHA * x + mixed, ln1_g[i], ln1_b[i])
        y = hierarchical_moe(x, w_grp[i], b_grp[i], w_rt[i], b_rt[i], w1[i], w3[i], w2[i])
        ple = jax.nn.sigmoid(x @ w_pg[i].astype(dt)) * (p[i].astype(dt) @ w_pp[i].astype(dt))
        x = layer_norm(ALPHA * x + y + ple, ln2_g[i], ln2_b[i])
    return x
```

```python
import numpy as np
from contextlib import ExitStack
import concourse.bass as bass
import concourse.mybir as mybir
from concourse.bass_utils import run_bass_kernel_spmd

F32 = mybir.dt.float32
BF16 = mybir.dt.bfloat16
I32 = mybir.dt.int32
AF = mybir.ActivationFunctionType
ALU = mybir.AluOpType
AX = mybir.AxisListType

NCORES = 8
DEPTH = 4
T = 2048
NT = 16
D = 2048
PW = 10752
ALPHA = float(8 ** 0.25)
EPS = 1e-5
SCALE = float(128 ** -0.5)
NEG = -30000.0
SLOTB = 256
NBLK = 47
NSLOT = NBLK * SLOTB
KVX_ROWS = 2048 + 2048 + 512 + 512 + 128 + 128
KV_CHUNKS = ((0, 1024), (1024, 2048), (2048, 3072), (3072, 4096), (4096, 5120), (5120, 5376))
TWO_PI = 2.0 * np.pi
C1 = 6.28125
C2 = float(np.float32(TWO_PI - C1))
C3 = float(TWO_PI - C1 - np.float64(np.float32(TWO_PI - C1)))


class _Eng:
    def __init__(self, key, eng, sem):
        self.key, self.eng, self.sem, self.count, self.seen = key, eng, sem, 0, {}


class _Slot:
    def __init__(self, sem):
        self.sem, self.total = sem, 0


class Buf:
    def __init__(self, name):
        self.name, self.w, self.r, self.slot = name, None, {}, None


class V:
    def __init__(self, ap, buf):
        self.ap, self.buf = ap, buf


class Tile:
    def __init__(self, h, buf):
        self.h, self.buf = h, buf

    def __getitem__(self, idx):
        return V(self.h[idx], self.buf)

    def v(self, ap):
        return V(ap, self.buf)


class Tracker:
    def __init__(self, nc, es, nslots=93):
        self.nc = nc
        mk = lambda n: es.enter_context(nc.semaphore(n))
        self.E = {
            'pe': _Eng('pe', nc.tensor, mk('c_pe')),
            'dve': _Eng('dve', nc.vector, mk('c_dve')),
            'act': _Eng('act', nc.scalar, mk('c_act')),
            'pool': _Eng('pool', nc.gpsimd, mk('c_pool')),
            'sp': _Eng('sp', nc.sync, mk('c_sp')),
        }
        self.cc = _Eng('cc', None, mk('c_cc'))
        self.slots = [_Slot(mk(f'd{i}')) for i in range(nslots)]
        self.free = list(self.slots)
        self.bufs = []
        self.ninst = 0

    def buf(self, name):
        b = Buf(name)
        self.bufs.append(b)
        return b

    def release(self, bufs):
        for b in bufs:
            if b.slot is not None:
                self.free.append(b.slot)
                b.slot = None
            if b in self.bufs:
                self.bufs.remove(b)

    def _slot(self, b):
        if b.slot is None:
            b.slot = self.free.pop()
        return b.slot

    def _waits(self, E, reads, writes, skip_slot=None):
        need = {}

        def add(ev):
            if ev is None:
                return
            kind, obj = ev[0], ev[1]
            if kind == 'd':
                sem, val = obj.sem, obj.total
            else:
                if obj is E and E.key == 'pe':
                    return
                sem, val = obj.sem, ev[2]
            k = id(sem)
            if k not in need or need[k][1] < val:
                need[k] = (sem, val)

        for b in reads:
            add(b.w)
        for b in writes:
            if not (skip_slot is not None and b.w is not None and b.w[0] == 'd' and b.w[1] is skip_slot):
                add(b.w)
            for ev in b.r.values():
                add(ev)
        for k, (sem, val) in need.items():
            if E.seen.get(k, 0) >= val:
                continue
            E.eng.wait_ge(sem, val)
            E.seen[k] = val

    def I(self, ek, meth, **kw):
        E = self.E[ek]
        reads, writes, args = [], [], {}
        for k, v in kw.items():
            if isinstance(v, V):
                (writes if k in ('out', 'accum_out') else reads).append(v.buf)
                args[k] = v.ap
            else:
                args[k] = v
        self._waits(E, reads, writes)
        ins = getattr(E.eng, meth)(**args)
        E.count += 1
        ins.then_inc(E.sem, 1)
        ev = ('e', E, E.count)
        for b in reads:
            b.r[E.key] = ev
        for b in writes:
            b.w = ev
            b.r = {}
        self.ninst += 1
        return ins

    def dma(self, qk, out, in_, indirect=None, **kw):
        E = self.E[qk]
        reads, writes = [], []
        o, i = out, in_
        if isinstance(out, V):
            writes.append(out.buf)
            o = out.ap
        if isinstance(in_, V):
            reads.append(in_.buf)
            i = in_.ap
        if indirect is not None:
            reads.append(indirect[1].buf)
        sb = (writes + reads)[0]
        slot = self._slot(sb)
        self._waits(E, reads, writes, skip_slot=slot)
        if indirect is None:
            ins = E.eng.dma_start(out=o, in_=i, **kw)
        elif indirect[0] == 'in':
            ins = E.eng.indirect_dma_start(out=o, out_offset=None, in_=i,
                                           in_offset=bass.IndirectOffsetOnAxis(ap=indirect[1].ap, axis=0), **kw)
        else:
            ins = E.eng.indirect_dma_start(out=o, out_offset=bass.IndirectOffsetOnAxis(ap=indirect[1].ap, axis=0),
                                           in_=i, in_offset=None, **kw)
        slot.total += 16
        ins.then_inc(slot.sem, 16)
        ev = ('d', slot)
        for b in reads:
            b.r[('d', id(slot))] = ev
        for b in writes:
            b.w = ev
            b.r = {}
        self.ninst += 1
        return ins

    def barrier(self):
        for E in self.E.values():
            for F in list(self.E.values()) + [self.cc]:
                if F is E or F.count == 0:
                    continue
                k = id(F.sem)
                if E.seen.get(k, 0) < F.count:
                    E.eng.wait_ge(F.sem, F.count)
                    E.seen[k] = F.count
            for s in self.slots:
                if s.total == 0:
                    continue
                k = id(s.sem)
                if E.seen.get(k, 0) < s.total:
                    E.eng.wait_ge(s.sem, s.total)
                    E.seen[k] = s.total
        for b in self.bufs:
            b.w, b.r = None, {}

    def final_wait(self):
        E = self.E['sp']
        for s in self.slots:
            if s.total and E.seen.get(id(s.sem), 0) < s.total:
                E.eng.wait_ge(s.sem, s.total)
                E.seen[id(s.sem)] = s.total


class Phase:
    def __init__(self, tr, name):
        self.tr, self.nc, self.name = tr, tr.nc, name
        self.es = ExitStack()
        self.bufs = []
        self.n = 0

    def __enter__(self):
        self.es.__enter__()
        return self

    def __exit__(self, *a):
        self.tr.barrier()
        self.tr.release(self.bufs)
        return self.es.__exit__(*a)

    def sb(self, shape, dt, name=None):
        self.n += 1
        nm = f"{self.name}_{name or 's'}{self.n}"
        h = self.es.enter_context(self.nc.sbuf_tensor(nm, list(shape), dt))
        b = self.tr.buf(nm)
        self.bufs.append(b)
        return Tile(h, b)

    def ps(self, shape, dt, name=None):
        self.n += 1
        nm = f"{self.name}_{name or 'p'}{self.n}"
        h = self.es.enter_context(self.nc.psum_tensor(nm, list(shape), dt))
        b = self.tr.buf(nm)
        self.bufs.append(b)
        return Tile(h, b)


class Ring:
    def __init__(self, items):
        self.items, self.i = items, 0

    def next(self):
        x = self.items[self.i % len(self.items)]
        self.i += 1
        return x


def layer_norm_rows(tr, ph, src, dst, W, g_bc, b_bc, tmp):
    nch = W // 512
    st, mv, rs = tmp['st'], tmp['mv'], tmp['rs']
    for c in range(nch):
        tr.I('dve', 'bn_stats', out=st[:, c, :], in_=src[:, c * 512:(c + 1) * 512])
    tr.I('dve', 'bn_aggr', out=mv[:, :], in_=st[:, 0:nch, :])
    tr.I('dve', 'tensor_scalar', out=rs[:, 0:1], in0=mv[:, 1:2], scalar1=EPS, scalar2=None, op0=ALU.add)
    tr.I('pool', 'tensor_tensor', out=rs[:, 1:2], in0=rs[:, 0:1], in1=tmp['nh'][:, 0:1], op=ALU.pow)
    tr.I('dve', 'tensor_scalar', out=dst[:, 0:W], in0=src[:, 0:W], scalar1=mv[:, 0:1], scalar2=rs[:, 1:2],
         op0=ALU.subtract, op1=ALU.mult)
    tr.I('dve', 'tensor_tensor', out=dst[:, 0:W], in0=dst[:, 0:W], in1=g_bc[:, 0:W], op=ALU.mult)
    tr.I('dve', 'tensor_tensor', out=dst[:, 0:W], in0=dst[:, 0:W], in1=b_bc[:, 0:W], op=ALU.add)


def build(nlayers=DEPTH, halo='ag', dbg=(), stop_after=None, wdepth=DEPTH):
    nc = bass.Bass("TRN2", target_bir_lowering=False)
    dt = nc.dram_tensor

    def ein(name, shape, dtype=F32):
        return dt(name, list(shape), dtype, kind="ExternalInput").ap()

    def scr(name, shape, dtype):
        kind = "ExternalOutput" if name in dbg else "Internal"
        return dt(name, list(shape), dtype, kind=kind).ap()

    x_in = ein("x", [T, D])
    p_in = ein("p", [wdepth, T, 256])
    pos_in = ein("pos", [128, NT], I32)
    cst_in = ein("cst", [128, 1024])
    hidx_in = ein("hidx", [128, 48], I32)
    w_in = ein("w_in", [wdepth, D, PW])
    w_s = ein("w_s", [wdepth, 8, 128, 128])
    b_s = ein("b_s", [wdepth, 8 * 128])
    ln_v_g = ein("ln_v_g", [wdepth, 1024])
    ln_v_b = ein("ln_v_b", [wdepth, 1024])
    w_a = ein("w_a", [wdepth, 1024, D])
    w_b = ein("w_b", [wdepth, 512, D])
    w_o = ein("w_o", [wdepth, D, D])
    ln1_g = ein("ln1_g", [wdepth, D])
    ln1_b = ein("ln1_b", [wdepth, D])
    w_grp = ein("w_grp", [wdepth, D, 4])
    b_grp = ein("b_grp", [wdepth, 4])
    w_rt = ein("w_rt", [wdepth, D, 32])
    b_rt = ein("b_rt", [wdepth, 32])
    w1 = ein("w1", [wdepth, 32 * D, 512])
    w3 = ein("w3", [wdepth, 32 * D, 512])
    w2 = ein("w2", [wdepth, 32 * 512, D])
    w_pg = ein("w_pg", [wdepth, D, D])
    w_pp = ein("w_pp", [wdepth, 256, D])
    ln2_g = ein("ln2_g", [wdepth, D])
    ln2_b = ein("ln2_b", [wdepth, D])
    y_out = dt("y", [T, D], F32, kind="ExternalOutput").ap()

    XA = scr("XA", [T, D], F32)
    XB = scr("XB", [T, D], F32)
    QK = scr("QK", [T, 3072], BF16)
    VV = scr("VV", [T, 1536], BF16)
    KVX32 = dt("KVX", [KVX_ROWS, 256], F32)
    KVX = KVX32[:, :].bitcast(BF16)
    if halo == 'ag':
        KVH32 = dt("KVH", [4 * KVX_ROWS, 256], F32)
        KVH = KVH32[:, :].bitcast(BF16)
    else:
        KVH32 = None
        KVH = None
    OG = scr("OG", [3, T, 516], F32)
    GT = scr("GT", [4096, T], BF16)
    MT = scr("MT", [D, T], BF16)
    R2 = scr("R2", [T, D], F32)
    XS = scr("XS", [NSLOT, D], BF16)
    YS = scr("YS", [NSLOT, D], F32)

    with ExitStack() as es:
        tr = Tracker(nc, es)
        with Phase(tr, "g") as G:
            cst = G.sb([128, 1024], F32, "cst")
            tr.dma('sp', cst[:, :], cst_in[:, :])
            ident_f = cst[:, 0:128]
            identb = G.sb([128, 128], BF16, "identb")
            tr.I('dve', 'tensor_copy', out=identb[:, :], in_=cst[:, 0:128])
            ones_f = G.sb([128, 128], F32, "ones")
            tr.I('dve', 'memset', ap=ones_f[:, :], constant=1.0) if False else None
            nc_ones = tr.I('dve', 'tensor_scalar', out=ones_f[:, :], in0=cst[:, 0:128], scalar1=0.0, scalar2=1.0,
                           op0=ALU.mult, op1=ALU.add)
            nh = G.sb([128, 1], F32, "nh")
            tr.I('dve', 'tensor_scalar', out=nh[:, :], in0=cst[:, 0:1], scalar1=0.0, scalar2=-0.5,
                 op0=ALU.mult, op1=ALU.add)
            hidx = G.sb([128, 48], I32, "hidx")
            tr.dma('sp', hidx[:, :], hidx_in[:, :])
            lnt = {'st': G.sb([128, 4, 6], F32, "st"), 'mv': G.sb([128, 2], F32, "mv"),
                   'rs': G.sb([128, 2], F32, "rs"), 'nh': nh}
            cos4 = G.sb([128, NT, 4, 16], F32, "cos4")
            sin4 = G.sb([128, NT, 4, 16], F32, "sin4")
            with Phase(tr, "rt") as P:
                posi = P.sb([128, NT], I32)
                posf = P.sb([128, NT], F32)
                ang = P.sb([128, NT, 16], F32)
                kf = P.sb([128, NT, 16], F32)
                ki = P.sb([128, NT, 16], I32)
                r = P.sb([128, NT, 16], F32)
                t1 = P.sb([128, NT, 16], F32)
                t2 = P.sb([128, NT, 16], F32)
                sn = P.sb([128, NT, 16], F32)
                cs = P.sb([128, NT, 16], F32)
                tr.dma('sp', posi[:, :], pos_in[:, :])
                tr.I('dve', 'tensor_copy', out=posf[:, :], in_=posi[:, :])
                for t in range(NT):
                    tr.I('dve', 'tensor_scalar', out=ang[:, t, :], in0=cst[:, 768:784], scalar1=posf[:, t:t + 1],
                         scalar2=None, op0=ALU.mult)

                def wrap(dst, src):
                    tr.I('dve', 'tensor_scalar', out=t1[:, :, :], in0=src[:, :, :], scalar1=float(np.pi),
                         scalar2=-TWO_PI, op0=ALU.is_gt, op1=ALU.mult)
                    tr.I('dve', 'tensor_scalar', out=t2[:, :, :], in0=src[:, :, :], scalar1=-float(np.pi),
                         scalar2=TWO_PI, op0=ALU.is_lt, op1=ALU.mult)
                    tr.I('dve', 'tensor_tensor', out=t1[:, :, :], in0=t1[:, :, :], in1=t2[:, :, :], op=ALU.add)
                    tr.I('dve', 'tensor_tensor', out=dst[:, :, :], in0=src[:, :, :], in1=t1[:, :, :], op=ALU.add)

                tr.I('dve', 'tensor_scalar', out=kf[:, :, :], in0=ang[:, :, :], scalar1=float(1.0 / TWO_PI),
                     scalar2=None, op0=ALU.mult)
                tr.I('dve', 'tensor_copy', out=ki[:, :, :], in_=kf[:, :, :])
                tr.I('dve', 'tensor_copy', out=kf[:, :, :], in_=ki[:, :, :])
                tr.I('dve', 'scalar_tensor_tensor', out=r[:, :, :], in0=kf[:, :, :], scalar=-C1, in1=ang[:, :, :],
                     op0=ALU.mult, op1=ALU.add)
                tr.I('dve', 'scalar_tensor_tensor', out=r[:, :, :], in0=kf[:, :, :], scalar=-C2, in1=r[:, :, :],
                     op0=ALU.mult, op1=ALU.add)
                tr.I('dve', 'scalar_tensor_tensor', out=r[:, :, :], in0=kf[:, :, :], scalar=-C3, in1=r[:, :, :],
                     op0=ALU.mult, op1=ALU.add)
                wrap(r, r)
                tr.I('act', 'activation', out=sn[:, :, :], in_=r[:, :, :], func=AF.Sin)
                tr.I('dve', 'tensor_scalar', out=r[:, :, :], in0=r[:, :, :], scalar1=float(np.pi / 2), scalar2=None,
                     op0=ALU.add)
                wrap(r, r)
                tr.I('act', 'activation', out=cs[:, :, :], in_=r[:, :, :], func=AF.Sin)
                for h in range(4):
                    tr.I('dve', 'tensor_copy', out=cos4[:, :, h, :], in_=cs[:, :, :])
                    tr.I('dve', 'tensor_copy', out=sin4[:, :, h, :], in_=sn[:, :, :])

            wbound_reg = nc.gpsimd.to_reg(wdepth * 8192 - 1)
            for li in range(nlayers):
                xin = x_in if li == 0 else XB
                xout = y_out if li == nlayers - 1 else XB
                _layer(nc, tr, G, li, xin, xout, locals(), halo, stop_after)
                if stop_after is not None:
                    break
        tr.final_wait()
    return nc, tr


def _transpose_rows(tr, P, src_dram, xT, identb, cast_q='pool'):
    xb = Ring([P.sb([128, D], BF16, "xb") for _ in range(2)])
    tp = Ring([P.ps([128, 1024], BF16, "tp") for _ in range(4)])
    for t in range(NT):
        b = xb.next()
        tr.dma('pool', b[:, :], src_dram[t * 128:(t + 1) * 128, :])
        for half in range(2):
            ps = tp.next()
            for j in range(8):
                c = half * 8 + j
                tr.I('pe', 'transpose', out=ps[:, j * 128:(j + 1) * 128], in_=b[:, c * 128:(c + 1) * 128],
                     identity=identb[:, :])
            eng = 'act' if half == 0 else 'dve'
            src = ps.v(ps.h[:, :].rearrange("p (j k) -> p j k", k=128))
            if eng == 'act':
                tr.I('act', 'activation', out=xT[:, half * 8:(half + 1) * 8, t * 128:(t + 1) * 128], in_=src,
                     func=AF.Copy)
            else:
                tr.I('dve', 'tensor_copy', out=xT[:, half * 8:(half + 1) * 8, t * 128:(t + 1) * 128], in_=src)


def _wload(tr, dst, w2d, r0, nrows_c, c0, ncols, q='pool'):
    src = w2d[r0:r0 + nrows_c * 128, c0:c0 + ncols].rearrange("(c p) n -> p c n", p=128)
    tr.dma(q, dst, src)


def _layer(nc, tr, G, li, xin, xout, g, halo, stop_after):
    cst, identb, ones_f, hidx, lnt = (g['cst'], g['identb'], g['ones_f'], g['hidx'], g['lnt'])
    cos4, sin4 = g['cos4'], g['sin4']
    w_in, QK, VV, GT, KVX, KVH, OG, MT, XA, R2, XS, YS = (g['w_in'], g['QK'], g['VV'], g['GT'], g['KVX'], g['KVH'],
                                                           g['OG'], g['MT'], g['XA'], g['R2'], g['XS'], g['YS'])
    Wi = w_in[li]

    with Phase(tr, f"A{li}") as A:
        uT = A.sb([128, 8, T], BF16, "uT")
        with Phase(tr, f"B{li}") as B:
            xT = B.sb([128, 16, T], BF16, "xT")
            with Phase(tr, f"t{li}") as P:
                _transpose_rows(tr, P, xin, xT, identb)
            vln = B.sb([128, NT, 1024], BF16, "vln")
            with Phase(tr, f"p{li}") as P:
                wt = Ring([P.sb([128, 16, 512], BF16, "wt") for _ in range(2)])
                acc = Ring([P.ps([128, 512], F32, "acc") for _ in range(4)])
                ot = Ring([P.sb([128, 512], BF16, "ot") for _ in range(3)])
                rt = Ring([P.sb([128, 4, 4, 16], F32, "rt") for _ in range(2)])
                for j in (range(7, 13) if stop_after == 'KV' else range(4, 13)):
                    w = wt.next()
                    _wload(tr, w[:, :, :], Wi, 0, 16, j * 512, 512)
                    for t in range(NT):
                        ps = acc.next()
                        for kc in range(16):
                            tr.I('pe', 'matmul', out=ps[:, :], lhsT=xT[:, kc, t * 128:(t + 1) * 128], rhs=w[:, kc, :],
                                 start=(kc == 0), stop=(kc == 15))
                        o = ot.next()
                        if j < 10:
                            p3 = ps.v(ps.h[:, :].rearrange("p (h d) -> p h d", h=4))
                            o3 = o.v(o.h[:, :].rearrange("p (h d) -> p h d", h=4))
                            x1 = ps.v(p3.ap[:, :, 0:16])
                            x2 = ps.v(p3.ap[:, :, 16:32])
                            r_ = rt.next()
                            tr.I('dve', 'tensor_tensor', out=r_[:, 0, :, :], in0=x1, in1=cos4[:, t, :, :], op=ALU.mult)
                            tr.I('dve', 'tensor_tensor', out=r_[:, 1, :, :], in0=x2, in1=sin4[:, t, :, :], op=ALU.mult)
                            tr.I('dve', 'tensor_tensor', out=r_[:, 2, :, :], in0=x2, in1=cos4[:, t, :, :], op=ALU.mult)
                            tr.I('dve', 'tensor_tensor', out=r_[:, 3, :, :], in0=x1, in1=sin4[:, t, :, :], op=ALU.mult)
                            tr.I('dve', 'tensor_tensor', out=o.v(o3.ap[:, :, 0:16]), in0=r_[:, 0, :, :],
                                 in1=r_[:, 1, :, :], op=ALU.subtract)
                            tr.I('dve', 'tensor_tensor', out=o.v(o3.ap[:, :, 16:32]), in0=r_[:, 2, :, :],
                                 in1=r_[:, 3, :, :], op=ALU.add)
                            tr.I('act', 'activation', out=o.v(o3.ap[:, :, 32:128]), in_=ps.v(p3.ap[:, :, 32:128]),
                                 func=AF.Copy)
                            tr.dma('sp', QK[t * 128:(t + 1) * 128, (j - 4) * 512:(j - 3) * 512], o[:, :])
                        else:
                            tr.I('act', 'activation', out=o[:, :], in_=ps[:, :], func=AF.Copy)
                            tr.dma('sp', VV[t * 128:(t + 1) * 128, (j - 10) * 512:(j - 9) * 512], o[:, :])
                tr.barrier()
                dummy = P.sb([128, 1], F32, "dummy")
                tr.dma('sp', V(KVX[0:2048, :], dummy.buf), QK[:, 1536 + 1024:1536 + 1536])
                tr.dma('sp', V(KVX[2048:4096, :], dummy.buf), VV[:, 1024:1536])
                tr.dma('sp', V(KVX[4096:4608, :], dummy.buf), QK[T - 512:T, 1536 + 512:1536 + 1024])
                tr.dma('sp', V(KVX[4608:5120, :], dummy.buf), VV[T - 512:T, 512:1024])
                tr.dma('sp', V(KVX[5120:5248, :], dummy.buf), QK[T - 128:T, 1536:1536 + 512])
                tr.dma('sp', V(KVX[5248:5376, :], dummy.buf), VV[T - 128:T, 0:512])
                if halo == 'ag':
                    dslot = dummy.buf.slot
                    EP = tr.E['pool']
                    EP.eng.wait_ge(dslot.sem, dslot.total)
                    EP.seen[id(dslot.sem)] = dslot.total
                    off = 0
                    for (r0, r1) in KV_CHUNKS:
                        n_ = r1 - r0
                        ins = nc.gpsimd.collective_compute("AllGather", ALU.bypass, replica_groups=[[0, 1, 2, 3], [4, 5, 6, 7]],
                                                           ins=[g['KVX32'][r0:r1, :]], outs=[g['KVH32'][off:off + 4 * n_, :]])
                        ins.then_inc(tr.cc.sem)
                        tr.cc.count += 1
                        off += 4 * n_
                for j in (() if stop_after == 'KV' else (0, 1, 13, 14, 15, 16, 17, 18, 19, 20)):
                    w = wt.next()
                    _wload(tr, w[:, :, :], Wi, 0, 16, j * 512, 512)
                    for fc in range(4):
                        for tb in range(4):
                            ps = acc.next()
                            for kc in range(16):
                                tr.I('pe', 'matmul', out=ps[:, :], lhsT=w[:, kc, fc * 128:(fc + 1) * 128],
                                     rhs=xT[:, kc, tb * 512:(tb + 1) * 512], start=(kc == 0), stop=(kc == 15))
                            if j < 2:
                                tr.I('act', 'activation', out=uT[:, j * 4 + fc, tb * 512:(tb + 1) * 512], in_=ps[:, :],
                                     func=AF.Gelu_apprx_tanh)
                            else:
                                o = ot.next()
                                tr.I('act', 'activation', out=o[:, :], in_=ps[:, :], func=AF.Sigmoid)
                                row = ((j - 13) * 4 + fc) * 128
                                tr.dma('sp', GT[row:row + 128, tb * 512:(tb + 1) * 512], o[:, :])
            with Phase(tr, f"v{li}") as P:
              if stop_after != 'KV':
                    wv = P.sb([128, 16, 1024], BF16, "wv")
                    _wload(tr, wv[:, :, 0:512], Wi, 0, 16, 1024, 512)
                    _wload(tr, wv[:, :, 512:1024], Wi, 0, 16, 1536, 512)
                    gbc = P.sb([128, 1024], F32, "gbc")
                    bbc = P.sb([128, 1024], F32, "bbc")
                    tr.dma('sp', gbc[:, :], g['ln_v_g'][li, :].partition_broadcast(128))
                    tr.dma('sp', bbc[:, :], g['ln_v_b'][li, :].partition_broadcast(128))
                    acc2 = Ring([P.ps([128, 1024], F32, "acc2") for _ in range(2)])
                    vg = Ring([P.sb([128, 1024], F32, "vg") for _ in range(2)])
                    vn = Ring([P.sb([128, 1024], F32, "vn") for _ in range(2)])
                    for t in range(NT):
                        ps = acc2.next()
                        for hf in range(2):
                            for kc in range(16):
                                tr.I('pe', 'matmul', out=ps[:, hf * 512:(hf + 1) * 512], lhsT=xT[:, kc, t * 128:(t + 1) * 128],
                                     rhs=wv[:, kc, hf * 512:(hf + 1) * 512], start=(kc == 0), stop=(kc == 15))
                        v_ = vg.next()
                        tr.I('act', 'activation', out=v_[:, :], in_=ps[:, :], func=AF.Gelu_apprx_tanh)
                        n_ = vn.next()
                        layer_norm_rows(tr, P, v_, n_, 1024, gbc, bbc, lnt)
                        tr.I('act', 'activation', out=vln[:, t, :], in_=n_[:, :], func=AF.Copy)
            with Phase(tr, f"s{li}") as P:
              if stop_after != 'KV':
                    wsf = P.sb([128, 8, 128], F32, "wsf")
                    tr.dma('sp', wsf[:, :, :], g['w_s'][li].rearrange("g t s -> t g s"))
                    wsb = P.sb([128, 8, 128], BF16, "wsb")
                    for gg in range(8):
                        tr.I('dve', 'tensor_tensor', out=wsb[:, gg, :], in0=wsf[:, gg, :], in1=cst[:, 128:256], op=ALU.mult)
                    wsT = P.sb([128, 8, 128], BF16, "wsT")
                    tps = P.ps([128, 1024], BF16, "tps")
                    for gg in range(8):
                        tr.I('pe', 'transpose', out=tps[:, gg * 128:(gg + 1) * 128], in_=wsb[:, gg, :], identity=identb[:, :])
                    tr.I('dve', 'tensor_copy', out=wsT[:, :, :], in_=tps.v(tps.h[:, :].rearrange("p (g k) -> p g k", k=128)))
                    bsb = P.sb([128, 8, 128], F32, "bsb")
                    tr.dma('sp', bsb[:, :, :], g['b_s'][li, :].partition_broadcast(128).rearrange("p (g t) -> p g t", t=128))
                    zps = Ring([P.ps([128, 1024], F32, "zps") for _ in range(2)])
                    zt = Ring([P.sb([128, 8, 128], F32, "zt") for _ in range(2)])
                    for n in range(NT):
                        ps = zps.next()
                        for gg in range(8):
                            tr.I('pe', 'matmul', out=ps[:, gg * 128:(gg + 1) * 128], lhsT=vln[:, n, gg * 128:(gg + 1) * 128],
                                 rhs=wsT[:, gg, :], start=True, stop=True)
                        z = zt.next()
                        tr.I('dve', 'tensor_tensor', out=z[:, :, :], in0=ps.v(ps.h[:, :].rearrange("p (g t) -> p g t", t=128)),
                             in1=bsb[:, :, :], op=ALU.add)
                        tr.I('dve', 'tensor_tensor', out=uT[:, :, n * 128:(n + 1) * 128], in0=z[:, :, :],
                             in1=uT[:, :, n * 128:(n + 1) * 128], op=ALU.mult)
        if stop_after == 'A':
            with Phase(tr, "dbgA") as P:
                tr.dma('sp', g['XA'][0:128, :].rearrange("p (c t) -> p c t", c=1)[:, 0, 0:T] if False else
                       g['MT'][0:1024, :].rearrange("(c p) t -> p c t", p=128), uT[:, :, :])
            return
        aT = uT

        hsrc = KVH if KVH is not None else None
        if stop_after == 'KV':
            return

        with Phase(tr, f"at{li}") as P:
            qkv = Ring([P.sb([128, 5, 512], BF16, "qkv") for _ in range(4)])
            tps = Ring([P.ps([128, 2048], BF16, "tps") for _ in range(1)])
            qkT = Ring([P.sb([128, 1536], BF16, "qkT") for _ in range(2)])
            sps = Ring([P.ps([128, 1024], F32, "sps") for _ in range(1)])
            ssb = Ring([P.sb([128, 4, 256], F32, "ssb") for _ in range(2)])
            psb = Ring([P.sb([128, 4, 256], BF16, "psb") for _ in range(2)])
            ptp = Ring([P.ps([128, 1024], BF16, "ptp") for _ in range(2)])
            pT = Ring([P.sb([128, 8, 128], BF16, "pT") for _ in range(2)])
            ops_ = Ring([P.ps([128, 512], F32, "ops") for _ in range(2)])
            osb = Ring([P.sb([128, 516], F32, "osb") for _ in range(2)])
            sm = Ring([P.sb([128, 16], F32, "sm") for _ in range(2)])
            mask4 = P.sb([128, 4, 256], F32, "mask4")
            maskh4 = P.sb([128, 4, 256], F32, "maskh4")
            for h in range(4):
                tr.I('dve', 'tensor_copy', out=mask4[:, h, :], in_=cst[:, 256:512])
                tr.I('dve', 'tensor_copy', out=maskh4[:, h, :], in_=cst[:, 512:768])
            blocks = []
            hcol = 0
            for gi, dil in enumerate((1, 4, 16)):
                nb = 16 // dil
                for r in range(dil):
                    for n in range(nb):
                        hc = None
                        if n == 0:
                            hc = hcol
                            hcol += 2
                        blocks.append((gi, dil, r, n, hc))

            def loads(blk):
                gi, dil, r, n, hc = blk
                qv = QK.rearrange("(n i r) c -> r n i c", r=dil, i=128)
                vv = VV.rearrange("(n i r) c -> r n i c", r=dil, i=128)
                qc, kc_, vc = gi * 512, 1536 + gi * 512, gi * 512
                b = qkv.next()
                war = dict(b.buf.r)
                tr.dma('sp', b[:, 0, :], qv[r, n, :, qc:qc + 512])
                tr.dma('sp', b[:, 2, :], qv[r, n, :, kc_:kc_ + 512])
                tr.dma('sp', b[:, 4, :], vv[r, n, :, vc:vc + 512])
                first = (n == 0)
                if not first:
                    tr.dma('sp', b[:, 1, :], qv[r, n - 1, :, kc_:kc_ + 512])
                    tr.dma('sp', b[:, 3, :], vv[r, n - 1, :, vc:vc + 512])
                elif hsrc is not None:
                    b.buf.r = dict(war)
                    tr.dma('pool', b[:, 1, :], hsrc[:, :], indirect=('in', hidx[:, hc:hc + 1]))
                    tr.dma('pool', b[:, 3, :], hsrc[:, :], indirect=('in', hidx[:, hc + 1:hc + 2]))
                else:
                    tr.dma('sp', b[:, 1, :], qv[r, n, :, kc_:kc_ + 512])
                    tr.dma('sp', b[:, 3, :], vv[r, n, :, vc:vc + 512])
                return b

            def stage_a(blk, b):
                gi, dil, r, n, hc = blk
                first = (n == 0)
                tp = tps.next()
                for h in range(4):
                    tr.I('pe', 'transpose', out=tp[:, h * 128:(h + 1) * 128], in_=b[:, 0, h * 128:(h + 1) * 128],
                         identity=identb[:, :])
                    for hf in range(2):
                        o0 = 512 + h * 256 + hf * 128
                        tr.I('pe', 'transpose', out=tp[:, o0:o0 + 128], in_=b[:, 1 + hf, h * 128:(h + 1) * 128],
                             identity=identb[:, :])
                qt = qkT.next()
                tr.I('dve', 'tensor_copy', out=qt[:, :], in_=tp[:, 0:1536])
                sp_ = sps.next()
                for h in range(4):
                    tr.I('pe', 'matmul', out=sp_[:, h * 256:(h + 1) * 256], lhsT=qt[:, h * 128:(h + 1) * 128],
                         rhs=qt[:, 512 + h * 256:512 + (h + 1) * 256], start=True, stop=True)
                s_ = ssb.next()
                mk = maskh4 if first else mask4
                tr.I('dve', 'scalar_tensor_tensor', out=s_[:, :, :],
                     in0=sp_.v(sp_.h[:, :].rearrange("p (h k) -> p h k", k=256)), scalar=SCALE, in1=mk[:, :, :],
                     op0=ALU.mult, op1=ALU.add)
                m_ = sm.next()
                tr.I('dve', 'tensor_reduce', out=m_[:, 0:4], in_=s_[:, :, :], axis=AX.X, op=ALU.max, negate=True)
                p_ = psb.next()
                for h in range(4):
                    tr.I('act', 'activation', out=p_[:, h, :], in_=s_[:, h, :], func=AF.Exp, bias=m_[:, h:h + 1],
                         scale=1.0, accum_out=m_[:, 4 + h:5 + h])
                return (b, m_, p_)

            def stage_b(blk, st):
                gi, dil, r, n, hc = blk
                b, m_, p_ = st
                og = OG[gi].rearrange("(n i r) c -> r n i c", r=dil, i=128)
                pp = ptp.next()
                for h in range(4):
                    for hf in range(2):
                        j = h * 2 + hf
                        tr.I('pe', 'transpose', out=pp[:, j * 128:(j + 1) * 128],
                             in_=p_[:, h, hf * 128:(hf + 1) * 128], identity=identb[:, :])
                pt = pT.next()
                tr.I('dve', 'tensor_copy', out=pt[:, :, :], in_=pp.v(pp.h[:, :].rearrange("p (j k) -> p j k", k=128)))
                op_ = ops_.next()
                for h in range(4):
                    for hf in range(2):
                        tr.I('pe', 'matmul', out=op_[:, h * 128:(h + 1) * 128], lhsT=pt[:, h * 2 + hf, :],
                             rhs=b[:, 3 + hf, h * 128:(h + 1) * 128], start=(hf == 0), stop=(hf == 1))
                o_ = osb.next()
                tr.I('dve', 'reciprocal', out=m_[:, 8:12], in_=m_[:, 4:8])
                for h in range(4):
                    tr.I('act', 'activation', out=o_[:, h * 128:(h + 1) * 128], in_=op_[:, h * 128:(h + 1) * 128],
                         func=AF.Copy, scale=m_[:, 8 + h:9 + h])
                tr.I('act', 'activation', out=m_[:, 12:16], in_=m_[:, 4:8], func=AF.Ln)
                tr.I('dve', 'tensor_tensor', out=o_[:, 512:516], in0=m_[:, 12:16], in1=m_[:, 0:4], op=ALU.subtract)
                tr.dma('sp', og[r, n, :, :], o_[:, :])

            nb_ = len(blocks)
            lb = {0: loads(blocks[0]), 1: loads(blocks[1])}
            prev = None
            for i, blk in enumerate(blocks):
                if i + 2 < nb_:
                    lb[i + 2] = loads(blocks[i + 2])
                st = stage_a(blk, lb.pop(i))
                if prev is not None:
                    stage_b(*prev)
                prev = (blk, st)
            stage_b(*prev)
        if stop_after == 'AT':
            return

        with Phase(tr, f"m{li}") as P:
            bT = P.sb([128, 4, T], BF16, "bT")
            with Phase(tr, f"mg{li}") as Q:
                ogt = Ring([Q.sb([128, 3, 516], F32, "ogt") for _ in range(2)])
                l3 = Ring([Q.sb([128, 8, 4], F32, "l3") for _ in range(2)])
                mg = Ring([Q.sb([128, 512], F32, "mg") for _ in range(2)])
                mgb = Ring([Q.sb([128, 512], BF16, "mgb") for _ in range(2)])
                tp = Ring([Q.ps([128, 512], BF16, "tp") for _ in range(2)])
                ogv = OG.rearrange("g t c -> t g c")
                for t in range(NT):
                    o = ogt.next()
                    tr.dma('sp', o[:, :, :], ogv[t * 128:(t + 1) * 128, :, :])
                    l = l3.next()
                    tr.I('dve', 'tensor_tensor', out=l[:, 3, :], in0=o[:, 0, 512:516], in1=o[:, 1, 512:516], op=ALU.max)
                    tr.I('dve', 'tensor_tensor', out=l[:, 3, :], in0=l[:, 3, :], in1=o[:, 2, 512:516], op=ALU.max)
                    for gi in range(3):
                        tr.I('dve', 'tensor_tensor', out=l[:, gi, :], in0=o[:, gi, 512:516], in1=l[:, 3, :], op=ALU.subtract)
                    tr.I('act', 'activation', out=l[:, 0:3, :], in_=l[:, 0:3, :], func=AF.Exp)
                    tr.I('dve', 'tensor_tensor', out=l[:, 4, :], in0=l[:, 0, :], in1=l[:, 1, :], op=ALU.add)
                    tr.I('dve', 'tensor_tensor', out=l[:, 4, :], in0=l[:, 4, :], in1=l[:, 2, :], op=ALU.add)
                    tr.I('dve', 'reciprocal', out=l[:, 4, :], in_=l[:, 4, :])
                    for gi in range(3):
                        tr.I('dve', 'tensor_tensor', out=l[:, 5 + gi, :], in0=l[:, gi, :], in1=l[:, 4, :], op=ALU.mult)
                    m = mg.next()
                    for h in range(4):
                        hs = slice(h * 128, (h + 1) * 128)
                        tr.I('dve', 'tensor_scalar', out=m[:, hs], in0=o[:, 0, hs], scalar1=l[:, 5, h:h + 1], scalar2=None,
                             op0=ALU.mult)
                        for gi in (1, 2):
                            tr.I('dve', 'scalar_tensor_tensor', out=m[:, hs], in0=o[:, gi, hs], scalar=l[:, 5 + gi, h:h + 1],
                                 in1=m[:, hs], op0=ALU.mult, op1=ALU.add)
                    mb = mgb.next()
                    tr.I('act', 'activation', out=mb[:, :], in_=m[:, :], func=AF.Copy)
                    ps = tp.next()
                    for h in range(4):
                        tr.I('pe', 'transpose', out=ps[:, h * 128:(h + 1) * 128], in_=mb[:, h * 128:(h + 1) * 128],
                             identity=identb[:, :])
                    tr.I('dve', 'tensor_copy', out=bT[:, :, t * 128:(t + 1) * 128],
                         in_=ps.v(ps.h[:, :].rearrange("p (j k) -> p j k", k=128)))
            if stop_after == 'MG':
                tr.dma('sp', MT[0:512, :].rearrange("(c p) t -> p c t", p=128), bT[:, :, :])
                return
            with Phase(tr, f"ma{li}") as Q:
                Wa = Q.sb([128, 8, D], BF16, "Wa")
                Wb = Q.sb([128, 4, D], BF16, "Wb")
                for c4 in range(4):
                    _wload(tr, Wa[:, :, c4 * 512:(c4 + 1) * 512], g['w_a'][li], 0, 8, c4 * 512, 512)
                    _wload(tr, Wb[:, :, c4 * 512:(c4 + 1) * 512], g['w_b'][li], 0, 4, c4 * 512, 512)
                gts = Ring([Q.sb([128, 2, 512], BF16, "gts") for _ in range(3)])
                pa = Ring([Q.ps([128, 512], F32, "pa") for _ in range(2)])
                pb = Ring([Q.ps([128, 512], F32, "pb") for _ in range(2)])
                tm = Ring([Q.sb([128, 2, 512], F32, "tm") for _ in range(2)])
                mo = Ring([Q.sb([128, 512], BF16, "mo") for _ in range(3)])
                for tb in range(4):
                    ts = slice(tb * 512, (tb + 1) * 512)
                    for fc in range(16):
                        gt_ = gts.next()
                        tr.dma('act', gt_[:, 0, :], GT[fc * 128:(fc + 1) * 128, ts])
                        tr.dma('act', gt_[:, 1, :], GT[2048 + fc * 128:2048 + (fc + 1) * 128, ts])
                        a_ = pa.next()
                        for kc in range(8):
                            tr.I('pe', 'matmul', out=a_[:, :], lhsT=Wa[:, kc, fc * 128:(fc + 1) * 128], rhs=aT[:, kc, ts],
                                 start=(kc == 0), stop=(kc == 7))
                        b_ = pb.next()
                        for kc in range(4):
                            tr.I('pe', 'matmul', out=b_[:, :], lhsT=Wb[:, kc, fc * 128:(fc + 1) * 128], rhs=bT[:, kc, ts],
                                 start=(kc == 0), stop=(kc == 3))
                        t_ = tm.next()
                        tr.I('dve', 'tensor_tensor', out=t_[:, 0, :], in0=a_[:, :], in1=gt_[:, 0, :], op=ALU.mult)
                        tr.I('dve', 'tensor_tensor', out=t_[:, 1, :], in0=b_[:, :], in1=gt_[:, 1, :], op=ALU.mult)
                        m_ = mo.next()
                        tr.I('pool', 'tensor_tensor', out=m_[:, :], in0=t_[:, 0, :], in1=t_[:, 1, :], op=ALU.add)
                        tr.dma('sp', MT[fc * 128:(fc + 1) * 128, ts], m_[:, :])
    if stop_after == 'MA':
        return

    with Phase(tr, f"o{li}") as P:
        Wo = P.sb([128, 16, D], BF16, "Wo")
        for c4 in range(4):
            _wload(tr, Wo[:, :, c4 * 512:(c4 + 1) * 512], g['w_o'][li], 0, 16, c4 * 512, 512)
        gbc = P.sb([128, D], F32, "gbc")
        bbc = P.sb([128, D], F32, "bbc")
        tr.dma('sp', gbc[:, :], g['ln1_g'][li, :].partition_broadcast(128))
        tr.dma('sp', bbc[:, :], g['ln1_b'][li, :].partition_broadcast(128))
        mtb = Ring([P.sb([128, 16, 512], BF16, "mtb") for _ in range(2)])
        xt_ = Ring([P.sb([128, D], F32, "xt") for _ in range(2)])
        rr = Ring([P.sb([128, D], F32, "rr") for _ in range(2)])
        acc = Ring([P.ps([128, 512], F32, "acc") for _ in range(4)])
        mtv = MT.rearrange("(c p) t -> p c t", p=128)
        for tb in range(4):
            mb = mtb.next()
            tr.dma('act', mb[:, :, :], mtv[:, :, tb * 512:(tb + 1) * 512])
            for tt in range(4):
                t = tb * 4 + tt
                x_ = xt_.next()
                tr.dma('sp', x_[:, :], xin[t * 128:(t + 1) * 128, :])
                r_ = rr.next()
                for cb in range(4):
                    ps = acc.next()
                    for kc in range(16):
                        tr.I('pe', 'matmul', out=ps[:, :], lhsT=mb[:, kc, tt * 128:(tt + 1) * 128],
                             rhs=Wo[:, kc, cb * 512:(cb + 1) * 512], start=(kc == 0), stop=(kc == 15))
                    tr.I('dve', 'scalar_tensor_tensor', out=r_[:, cb * 512:(cb + 1) * 512], in0=x_[:, cb * 512:(cb + 1) * 512],
                         scalar=ALPHA, in1=ps[:, :], op0=ALU.mult, op1=ALU.add)
                layer_norm_rows(tr, P, r_, r_, D, gbc, bbc, lnt)
                tr.dma('act', XA[t * 128:(t + 1) * 128, :], r_[:, :])
    if stop_after == 'O':
        return

    with Phase(tr, f"r{li}") as R:
        sel = R.sb([128, NT, 32], F32, "sel")
        oh1 = R.sb([128, NT, 32], F32, "oh1")
        gates = R.sb([128, NT, 2], F32, "gates")
        slot_i = R.sb([128, NT, 2], I32, "slot_i")
        widx = R.sb([128, 64, 2], I32, "widx")
        with Phase(tr, f"rp{li}") as P:
            xT = P.sb([128, 16, T], BF16, "xT1")
            with Phase(tr, f"rt{li}") as Q:
                _transpose_rows(tr, Q, XA, xT, identb)
            Wr = P.sb([128, 16, 36], BF16, "Wr")
            tr.dma('pool', Wr[:, :, 0:4], g['w_grp'][li].rearrange("(c p) n -> p c n", p=128))
            tr.dma('pool', Wr[:, :, 4:36], g['w_rt'][li].rearrange("(c p) n -> p c n", p=128))
            brb = P.sb([128, 36], F32, "brb")
            tr.dma('sp', brb[:, 0:4], g['b_grp'][li, :].partition_broadcast(128))
            tr.dma('sp', brb[:, 4:36], g['b_rt'][li, :].partition_broadcast(128))
            Wpp = P.sb([128, 2, D], BF16, "Wpp")
            for c4 in range(4):
                _wload(tr, Wpp[:, :, c4 * 512:(c4 + 1) * 512], g['w_pp'][li], 0, 2, c4 * 512, 512)
            pT = P.sb([128, NT, 2, 128], BF16, "pT")
            with Phase(tr, f"rr{li}") as Q:
                psr = Ring([Q.ps([128, 64], F32, "psr") for _ in range(2)])
                lg = Ring([Q.sb([128, 36], F32, "lg") for _ in range(2)])
                msk = Ring([Q.sb([128, 32], F32, "msk") for _ in range(2)])
                sc = Ring([Q.sb([128, 32], F32, "sc") for _ in range(2)])
                pb_ = Ring([Q.sb([128, 256], BF16, "pb") for _ in range(2)])
                ptp = Ring([Q.ps([128, 256], BF16, "ptp") for _ in range(2)])
                for t in range(NT):
                    ps = psr.next()
                    for kc in range(16):
                        tr.I('pe', 'matmul', out=ps[:, 0:36], lhsT=xT[:, kc, t * 128:(t + 1) * 128], rhs=Wr[:, kc, :],
                             start=(kc == 0), stop=(kc == 15))
                    l = lg.next()
                    tr.I('dve', 'tensor_tensor', out=l[:, :], in0=ps[:, 0:36], in1=brb[:, :], op=ALU.add)
                    s = sc.next()
                    tr.I('dve', 'tensor_reduce', out=s[:, 0:1], in_=l[:, 0:4], axis=AX.X, op=ALU.max)
                    tr.I('dve', 'tensor_scalar', out=s[:, 1:2], in0=s[:, 0:1], scalar1=-1.0, scalar2=None, op0=ALU.mult)
                    tr.I('act', 'activation', out=s[:, 4:8], in_=l[:, 0:4], func=AF.Exp, bias=s[:, 1:2], scale=1.0,
                         accum_out=s[:, 2:3])
                    tr.I('dve', 'reciprocal', out=s[:, 3:4], in_=s[:, 2:3])
                    tr.I('dve', 'tensor_scalar', out=s[:, 8:12], in0=l[:, 0:4], scalar1=s[:, 0:1], scalar2=None,
                         op0=ALU.is_ge)
                    tr.I('dve', 'tensor_scalar', out=s[:, 8:12], in0=s[:, 8:12], scalar1=-1.0, scalar2=1.0e9,
                         op0=ALU.add, op1=ALU.mult)
                    m = msk.next()
                    for gg in range(4):
                        tr.I('dve', 'tensor_scalar', out=m[:, gg * 8:(gg + 1) * 8], in0=l[:, 4 + gg * 8:12 + gg * 8],
                             scalar1=s[:, 8 + gg:9 + gg], scalar2=None, op0=ALU.add)
                    tr.I('dve', 'max', out=s[:, 12:20], in_=m[:, :])
                    tr.I('dve', 'tensor_scalar', out=sel[:, t, :], in0=m[:, :], scalar1=s[:, 13:14], scalar2=None,
                         op0=ALU.is_ge)
                    tr.I('dve', 'tensor_scalar', out=oh1[:, t, :], in0=m[:, :], scalar1=s[:, 12:13], scalar2=None,
                         op0=ALU.is_ge)
                    tr.I('dve', 'tensor_tensor', out=s[:, 20:21], in0=s[:, 13:14], in1=s[:, 12:13], op=ALU.subtract)
                    tr.I('act', 'activation', out=s[:, 20:21], in_=s[:, 20:21], func=AF.Exp)
                    tr.I('dve', 'tensor_scalar', out=s[:, 21:22], in0=s[:, 20:21], scalar1=1.0, scalar2=None, op0=ALU.add)
                    tr.I('dve', 'reciprocal', out=s[:, 22:23], in_=s[:, 21:22])
                    tr.I('dve', 'tensor_tensor', out=gates[:, t, 0:1], in0=s[:, 22:23], in1=s[:, 3:4], op=ALU.mult)
                    tr.I('dve', 'tensor_tensor', out=gates[:, t, 1:2], in0=gates[:, t, 0:1], in1=s[:, 20:21], op=ALU.mult)
                    pb = pb_.next()
                    tr.dma('pool', pb[:, :], g['p_in'][li, t * 128:(t + 1) * 128, :])
                    pp = ptp.next()
                    for c in range(2):
                        tr.I('pe', 'transpose', out=pp[:, c * 128:(c + 1) * 128], in_=pb[:, c * 128:(c + 1) * 128],
                             identity=identb[:, :])
                    tr.I('act', 'activation', out=pT[:, t, :, :], in_=pp.v(pp.h[:, :].rearrange("p (c k) -> p c k", k=128)),
                         func=AF.Copy)
            with Phase(tr, f"rl{li}") as Q:
                wt = Ring([Q.sb([128, 16, 512], BF16, "wt") for _ in range(2)])
                pg = Ring([Q.ps([128, 512], F32, "pg") for _ in range(3)])
                pq = Ring([Q.ps([128, 512], F32, "pq") for _ in range(3)])
                sg = Ring([Q.sb([128, 512], F32, "sg") for _ in range(2)])
                x1 = Ring([Q.sb([128, 512], F32, "x1") for _ in range(3)])
                ro = Ring([Q.sb([128, 512], F32, "ro") for _ in range(3)])
                for cb in range(4):
                    cs_ = slice(cb * 512, (cb + 1) * 512)
                    w = wt.next()
                    _wload(tr, w[:, :, :], g['w_pg'][li], 0, 16, cb * 512, 512)
                    for t in range(NT):
                        x_ = x1.next()
                        tr.dma('act', x_[:, :], XA[t * 128:(t + 1) * 128, cs_])
                        a_ = pg.next()
                        for kc in range(16):
                            tr.I('pe', 'matmul', out=a_[:, :], lhsT=xT[:, kc, t * 128:(t + 1) * 128], rhs=w[:, kc, :],
                                 start=(kc == 0), stop=(kc == 15))
                        b_ = pq.next()
                        for kc in range(2):
                            tr.I('pe', 'matmul', out=b_[:, :], lhsT=pT[:, t, kc, :], rhs=Wpp[:, kc, cs_],
                                 start=(kc == 0), stop=(kc == 1))
                        s_ = sg.next()
                        tr.I('act', 'activation', out=s_[:, :], in_=a_[:, :], func=AF.Sigmoid)
                        tr.I('dve', 'tensor_tensor', out=s_[:, :], in0=s_[:, :], in1=b_[:, :], op=ALU.mult)
                        r_ = ro.next()
                        tr.I('dve', 'scalar_tensor_tensor', out=r_[:, :], in0=x_[:, :], scalar=ALPHA, in1=s_[:, :],
                             op0=ALU.mult, op1=ALU.add)
                        tr.dma('sp', R2[t * 128:(t + 1) * 128, cs_], r_[:, :])
        with Phase(tr, f"rs{li}") as P:
            U = cst[:, 784:912]
            prk = P.ps([128, NT, 32], F32, "prk")
            pct = P.ps([128, 32], F32, "pct")
            for t in range(NT):
                tr.I('pe', 'matmul', out=prk[:, t, :], lhsT=U, rhs=sel[:, t, :], start=True, stop=(t == 0))
                for t2 in range(t):
                    tr.I('pe', 'matmul', out=prk[:, t, :], lhsT=ones_f[:, :], rhs=sel[:, t2, :], start=False,
                         stop=(t2 == t - 1))
            for t in range(NT):
                tr.I('pe', 'matmul', out=pct[:, :], lhsT=ones_f[:, :], rhs=sel[:, t, :], start=(t == 0), stop=(t == NT - 1))
            cnt = P.sb([128, 32], F32, "cnt")
            nb_ = P.sb([128, 32], F32, "nb")
            cum = P.sb([128, 32], F32, "cum")
            base = P.sb([128, 32], F32, "base")
            on32 = P.sb([128, 32], F32, "on32")
            tr.I('dve', 'tensor_copy', out=cnt[:, :], in_=pct[:, :])
            tr.I('dve', 'tensor_copy', out=on32[:, :], in_=ones_f[:, 0:32])
            tr.I('dve', 'tensor_scalar', out=nb_[:, :], in0=cnt[:, :], scalar1=0.0, scalar2=None, op0=ALU.is_gt)
            for j in range(1, 8):
                tr.I('dve', 'scalar_tensor_tensor', out=nb_[:, :], in0=cnt[:, :], scalar=float(SLOTB * j), in1=nb_[:, :],
                     op0=ALU.is_gt, op1=ALU.add)
            tr.I('dve', 'tensor_tensor_scan', out=cum[:, :], data0=on32[:, :], data1=nb_[:, :], initial=0.0,
                 op0=ALU.mult, op1=ALU.add)
            tr.I('dve', 'tensor_tensor', out=base[:, :], in0=cum[:, :], in1=nb_[:, :], op=ALU.subtract)
            tr.I('dve', 'tensor_scalar', out=base[:, :], in0=base[:, :], scalar1=float(SLOTB), scalar2=None, op0=ALU.mult)
            slot = P.sb([128, NT, 32], F32, "slot")
            for t in range(NT):
                tr.I('dve', 'tensor_tensor', out=slot[:, t, :], in0=prk[:, t, :], in1=base[:, :], op=ALU.add)
            pr = P.sb([128, NT, 32], F32, "pr")
            sl = P.sb([128, NT, 2], F32, "sl")
            tr.I('dve', 'tensor_tensor', out=pr[:, :, :], in0=slot[:, :, :], in1=oh1[:, :, :], op=ALU.mult)
            tr.I('dve', 'tensor_reduce', out=sl[:, :, 0], in_=pr[:, :, :], axis=AX.X, op=ALU.add)
            tr.I('dve', 'tensor_tensor', out=pr[:, :, :], in0=slot[:, :, :], in1=sel[:, :, :], op=ALU.mult)
            tr.I('dve', 'tensor_reduce', out=sl[:, :, 1], in_=pr[:, :, :], axis=AX.X, op=ALU.add)
            tr.I('dve', 'tensor_tensor', out=sl[:, :, 1], in0=sl[:, :, 1], in1=sl[:, :, 0], op=ALU.subtract)
            tr.I('dve', 'tensor_copy', out=slot_i[:, :, :], in_=sl[:, :, :])
            be = P.sb([128, 64], F32, "be")
            tr.I('dve', 'tensor_scalar', out=be[:, :], in0=cst[:, 912:976], scalar1=cum[:, 0:1], scalar2=None, op0=ALU.is_ge)
            for e in range(1, 32):
                tr.I('dve', 'scalar_tensor_tensor', out=be[:, :], in0=cst[:, 912:976], scalar=cum[:, e:e + 1], in1=be[:, :],
                     op0=ALU.is_ge, op1=ALU.add)
            flag = P.sb([128, 64], F32, "flag")
            tr.I('dve', 'tensor_scalar', out=flag[:, :], in0=be[:, :], scalar1=31.5, scalar2=1.0e6, op0=ALU.is_ge, op1=ALU.mult)
            tr.I('dve', 'tensor_scalar', out=be[:, :], in0=be[:, :], scalar1=31.0, scalar2=128.0, op0=ALU.min, op1=ALU.mult)
            tr.I('dve', 'tensor_scalar', out=be[:, :], in0=be[:, :], scalar1=cst[:, 976:977], scalar2=None, op0=ALU.add)
            tr.I('dve', 'scalar_tensor_tensor', out=be[:, :], in0=be[:, :], scalar=2.0, in1=flag[:, :], op0=ALU.mult, op1=ALU.add)
            be2 = P.sb([128, 64, 2], F32, "be2")
            for h4 in range(2):
                tr.I('dve', 'tensor_scalar', out=be2[:, :, h4], in0=be[:, :], scalar1=float(li * 8192 + h4), scalar2=None,
                     op0=ALU.add)
            tr.I('dve', 'tensor_copy', out=widx[:, :, :], in_=be2[:, :, :])
        with Phase(tr, f"sc{li}") as P:
            xb = Ring([P.sb([128, D], BF16, "xb") for _ in range(3)])
            for t in range(NT):
                b = xb.next()
                tr.dma('pool', b[:, :], XA[t * 128:(t + 1) * 128, :])
                tr.dma('pool', XS[:, :], b[:, :], indirect=('out', slot_i[:, t, 0:1]))
                tr.dma('pool', XS[:, :], b[:, :], indirect=('out', slot_i[:, t, 1:2]))
        if stop_after == 'SC':
            return
        with Phase(tr, f"ex{li}") as P:
            stg = Ring([P.sb([128, 4096], F32, "stg") for _ in range(3)])
            wb = [[P.sb([128, 8192], BF16, f"wb{k}") for k in range(3)] for _ in range(2)]
            xs = Ring([P.sb([128, 2, D], BF16, "xs") for _ in range(2)])
            xsT = Ring([P.sb([128, 16, 256], BF16, "xsT") for _ in range(1)])
            tp = Ring([P.ps([128, 1024], BF16, "tp") for _ in range(2)])
            hp = Ring([P.ps([128, 1024], F32, "hp") for _ in range(2)])
            yp = Ring([P.ps([128, 512], F32, "yp") for _ in range(2)])
            hs = Ring([P.sb([128, 512], F32, "hs") for _ in range(1)])
            hb = Ring([P.sb([128, 512], BF16, "hb") for _ in range(2)])
            hT = Ring([P.sb([128, 4, 128], BF16, "hT") for _ in range(2)])
            ysb = Ring([P.sb([128, 1024], F32, "ysb") for _ in range(3)])
            w1v = g['w1'].rearrange("l (e p h c) f -> (l e p h) (c f)", p=128, h=2, c=8)
            w3v = g['w3'].rearrange("l (e p h c) f -> (l e p h) (c f)", p=128, h=2, c=8)
            w2v = g['w2'].rearrange("l (e p h c) n -> (l e p h) (c n)", p=128, h=2, c=2)
            wviews = (w1v, w3v, w2v)
            ceng = ['dve', 'dve', 'act', 'dve', 'dve', 'act']
            wbound = g['wbound_reg']

            def load_w(b, pieces):
                wset = wb[b % 2]
                for q_ in pieces:
                    k, h2 = q_ // 2, q_ % 2
                    s_ = stg.next()
                    tr.dma('pool', s_[:, :], wviews[k], indirect=('in', widx[:, b, h2:h2 + 1]), bounds_check=wbound,
                           oob_is_err=False)
                    dst_ = wset[k][:, h2 * 4096:(h2 + 1) * 4096]
                    ce = ceng[q_]
                    if ce == 'act':
                        tr.I('act', 'activation', out=dst_, in_=s_[:, :], func=AF.Copy)
                    else:
                        tr.I(ce, 'tensor_copy', out=dst_, in_=s_[:, :])

            def load_x(b):
                x_ = xs.next()
                tr.dma('sp', x_[:, :, :], XS[b * SLOTB:(b + 1) * SLOTB, :].rearrange("(h p) d -> p h d", p=128))
                return x_

            def xpose(x_):
                xt = xsT.next()
                for hf in range(2):
                    xv = x_.h[:, hf, :].rearrange("p (j c) -> p c j", c=16)
                    for half in range(2):
                        ps = tp.next()
                        for j in range(8):
                            c = half * 8 + j
                            tr.I('pe', 'transpose', out=ps[:, j * 128:(j + 1) * 128], in_=x_.v(xv[:, c, :]),
                                 identity=identb[:, :])
                        src = ps.v(ps.h[:, :].rearrange("p (j k) -> p j k", k=128))
                        dst = xt[:, half * 8:(half + 1) * 8, hf * 128:(hf + 1) * 128]
                        if half == 0:
                            tr.I('act', 'activation', out=dst, in_=src, func=AF.Copy)
                        else:
                            tr.I('dve', 'tensor_copy', out=dst, in_=src)
                return xt

            def compute_half(b, xt, hf):
                wset = wb[b % 2]
                h_ = hp.next()
                for k in range(2):
                    for c in range(16):
                        tr.I('pe', 'matmul', out=h_[:, k * 512:(k + 1) * 512], lhsT=xt[:, c, hf * 128:(hf + 1) * 128],
                             rhs=wset[k][:, c * 512:(c + 1) * 512], start=(c == 0), stop=(c == 15))
                s1 = hs.next()
                tr.I('act', 'activation', out=s1[:, :], in_=h_[:, 0:512], func=AF.Silu)
                hb_ = hb.next()
                tr.I('dve', 'tensor_tensor', out=hb_[:, :], in0=s1[:, :], in1=h_[:, 512:1024], op=ALU.mult)
                ps = tp.next()
                hv = hb_.h[:, :].rearrange("p (j c) -> p c j", c=4)
                for c in range(4):
                    tr.I('pe', 'transpose', out=ps[:, c * 128:(c + 1) * 128], in_=hb_.v(hv[:, c, :]), identity=identb[:, :])
                ht = hT.next()
                tr.I('act', 'activation', out=ht[:, :, :], in_=ps.v(ps.h[:, 0:512].rearrange("p (c k) -> p c k", k=128)),
                     func=AF.Copy)
                for cp in range(2):
                    y_ = ysb.next()
                    for c2 in range(2):
                        cb = cp * 2 + c2
                        yp_ = yp.next()
                        for c in range(4):
                            tr.I('pe', 'matmul', out=yp_[:, :], lhsT=ht[:, c, :],
                                 rhs=wset[2][:, c * D + cb * 512:c * D + (cb + 1) * 512], start=(c == 0), stop=(c == 3))
                        if c2 == 0:
                            tr.I('dve', 'tensor_copy', out=y_[:, c2 * 512:(c2 + 1) * 512], in_=yp_[:, :])
                        else:
                            tr.I('act', 'activation', out=y_[:, c2 * 512:(c2 + 1) * 512], in_=yp_[:, :], func=AF.Copy)
                    tr.dma('sp', YS[b * SLOTB + hf * 128:b * SLOTB + (hf + 1) * 128, cp * 1024:(cp + 1) * 1024], y_[:, :])

            load_w(0, range(6))
            x_next = load_x(0)
            for b in range(NBLK):
                x_cur = x_next
                if b + 1 < NBLK:
                    x_next = load_x(b + 1)
                xt = xpose(x_cur)
                if b + 1 < NBLK:
                    load_w(b + 1, range(0, 3))
                compute_half(b, xt, 0)
                if b + 1 < NBLK:
                    load_w(b + 1, range(3, 6))
                compute_half(b, xt, 1)
        with Phase(tr, f"cb{li}") as P:
            gbc = P.sb([128, D], F32, "gbc")
            bbc = P.sb([128, D], F32, "bbc")
            tr.dma('sp', gbc[:, :], g['ln2_g'][li, :].partition_broadcast(128))
            tr.dma('sp', bbc[:, :], g['ln2_b'][li, :].partition_broadcast(128))
            y1 = Ring([P.sb([128, D], F32, "y1") for _ in range(3)])
            y2 = Ring([P.sb([128, D], F32, "y2") for _ in range(3)])
            r2 = Ring([P.sb([128, D], F32, "r2") for _ in range(3)])
            def cb_load(t):
                a_, b_, r_ = y1.next(), y2.next(), r2.next()
                tr.dma('pool', a_[:, :], YS[:, :], indirect=('in', slot_i[:, t, 0:1]))
                tr.dma('pool', b_[:, :], YS[:, :], indirect=('in', slot_i[:, t, 1:2]))
                tr.dma('sp', r_[:, :], R2[t * 128:(t + 1) * 128, :])
                return a_, b_, r_

            nxt = cb_load(0)
            for t in range(NT):
                a_, b_, r_ = nxt
                if t + 1 < NT:
                    nxt = cb_load(t + 1)
                tr.I('dve', 'scalar_tensor_tensor', out=r_[:, :], in0=a_[:, :], scalar=gates[:, t, 0:1], in1=r_[:, :],
                     op0=ALU.mult, op1=ALU.add)
                tr.I('dve', 'scalar_tensor_tensor', out=r_[:, :], in0=b_[:, :], scalar=gates[:, t, 1:2], in1=r_[:, :],
                     op0=ALU.mult, op1=ALU.add)
                layer_norm_rows(tr, P, r_, r_, D, gbc, bbc, lnt)
                tr.dma('act', xout[t * 128:(t + 1) * 128, :], r_[:, :])


def make_consts(first_in_batch):
    c = np.zeros((128, 1024), np.float32)
    c[:, 0:128] = np.eye(128, dtype=np.float32)
    t = np.arange(128)[:, None]
    s = np.arange(128)[None, :]
    c[:, 128:256] = (s <= t).astype(np.float32)
    qi = np.arange(128)[:, None]
    kj = np.arange(256)[None, :]
    valid = (kj >= qi) & (kj <= qi + 128)
    m = np.where(valid, 0.0, NEG).astype(np.float32)
    c[:, 256:512] = m
    mh = m.copy()
    if first_in_batch:
        mh[:, 0:128] = NEG
    c[:, 512:768] = mh
    c[:, 768:784] = (500000.0 ** (-np.arange(0, 32, 2, dtype=np.float32) / 32)).astype(np.float32)[None, :]
    c[:, 784:912] = (t < s).astype(np.float32)
    c[:, 912:976] = np.arange(64, dtype=np.float32)[None, :]
    c[:, 976] = np.arange(128, dtype=np.float32)
    return c


def make_hidx(core):
    prev = (core % 4 - 1) % 4

    def row(e):
        e = np.asarray(e)
        out = np.zeros_like(e)
        off = 0
        for (r0, r1) in KV_CHUNKS:
            n_ = r1 - r0
            m = (e >= r0) & (e < r1)
            out = np.where(m, off + prev * n_ + (e - r0), out)
            off += 4 * n_
        return out

    cols = []
    i = np.arange(128)
    cols.append(row(5120 + i))
    cols.append(row(5248 + i))
    for r in range(4):
        cols.append(row(4096 + r + 4 * i))
        cols.append(row(4608 + r + 4 * i))
    for r in range(16):
        cols.append(row(r + 16 * i))
        cols.append(row(2048 + r + 16 * i))
    h = np.zeros((128, 48), np.int32)
    h[:, :len(cols)] = np.stack(cols, 1)
    return h


def core_inputs(core, inputs):
    b, q = core // 4, core % 4
    sl = slice(q * T, (q + 1) * T)
    m = {
        "x": np.ascontiguousarray(inputs["x"][b, sl]),
        "p": np.ascontiguousarray(inputs["p"][:, b, sl]),
        "pos": np.ascontiguousarray(inputs["positions"][b, sl].reshape(NT, 128).T.astype(np.int32)),
        "cst": make_consts(q == 0),
        "hidx": make_hidx(core),
    }
    for k in ("w_in", "w_s", "ln_v_g", "ln_v_b", "w_a", "w_b", "w_o", "ln1_g", "ln1_b", "w_grp", "b_grp",
              "w_rt", "b_rt", "w_pg", "w_pp", "ln2_g", "ln2_b"):
        m[k] = inputs[k]
    m["b_s"] = inputs["b_s"].reshape(-1, 8 * 128)
    m["w1"] = inputs["w1"].reshape(-1, 32 * D, 512)
    m["w3"] = inputs["w3"].reshape(-1, 32 * D, 512)
    m["w2"] = inputs["w2"].reshape(-1, 32 * 512, D)
    return m


_CACHE = {}


def kernel(**inputs):
    inputs = {k: np.asarray(v) for k, v in inputs.items()}
    if "nc" not in _CACHE:
        _CACHE["nc"] = build(nlayers=DEPTH, halo="ag")[0]
    nc = _CACHE["nc"]
    in_maps = [core_inputs(c, inputs) for c in range(NCORES)]
    res = run_bass_kernel_spmd(nc, in_maps, core_ids=list(range(NCORES)))
    out = np.zeros((2, 4 * T, D), np.float32)
    for c in range(NCORES):
        out[c // 4, (c % 4) * T:(c % 4 + 1) * T] = np.asarray(res.results[c]["y"])
    return out
```

```python
import numpy as np
from contextlib import ExitStack
import concourse.bass as bass
import concourse.mybir as mybir
from concourse.bass_utils import run_bass_kernel_spmd

F32 = mybir.dt.float32
BF16 = mybir.dt.bfloat16
I32 = mybir.dt.int32
AF = mybir.ActivationFunctionType
ALU = mybir.AluOpType
AX = mybir.AxisListType

NCORES = 8
DEPTH = 4
T = 2048
NT = 16
D = 2048
PW = 10752
ALPHA = float(8 ** 0.25)
EPS = 1e-5
SCALE = float(128 ** -0.5)
NEG = -30000.0
SLOTB = 256
NBLK = 47
NSLOT = NBLK * SLOTB
KVX_ROWS = 2048 + 2048 + 512 + 512 + 128 + 128
KV_CHUNKS = ((0, 1024), (1024, 2048), (2048, 3072), (3072, 4096), (4096, 5120), (5120, 5376))
TWO_PI = 2.0 * np.pi
C1 = 6.28125
C2 = float(np.float32(TWO_PI - C1))
C3 = float(TWO_PI - C1 - np.float64(np.float32(TWO_PI - C1)))


class _Eng:
    def __init__(self, key, eng, sem):
        self.key, self.eng, self.sem, self.count, self.seen = key, eng, sem, 0, {}


class _Slot:
    def __init__(self, sem):
        self.sem, self.total = sem, 0


class Buf:
    def __init__(self, name):
        self.name, self.w, self.r, self.slot = name, None, {}, None


class V:
    def __init__(self, ap, buf):
        self.ap, self.buf = ap, buf


class Tile:
    def __init__(self, h, buf):
        self.h, self.buf = h, buf

    def __getitem__(self, idx):
        return V(self.h[idx], self.buf)

    def v(self, ap):
        return V(ap, self.buf)


class Tracker:
    def __init__(self, nc, es, nslots=93):
        self.nc = nc
        mk = lambda n: es.enter_context(nc.semaphore(n))
        self.E = {
            'pe': _Eng('pe', nc.tensor, mk('c_pe')),
            'dve': _Eng('dve', nc.vector, mk('c_dve')),
            'act': _Eng('act', nc.scalar, mk('c_act')),
            'pool': _Eng('pool', nc.gpsimd, mk('c_pool')),
            'sp': _Eng('sp', nc.sync, mk('c_sp')),
        }
        self.cc = _Eng('cc', None, mk('c_cc'))
        self.slots = [_Slot(mk(f'd{i}')) for i in range(nslots)]
        self.free = list(self.slots)
        self.bufs = []
        self.ninst = 0

    def buf(self, name):
        b = Buf(name)
        self.bufs.append(b)
        return b

    def release(self, bufs):
        for b in bufs:
            if b.slot is not None:
                self.free.append(b.slot)
                b.slot = None
            if b in self.bufs:
                self.bufs.remove(b)

    def _slot(self, b):
        if b.slot is None:
            b.slot = self.free.pop()
        return b.slot

    def _waits(self, E, reads, writes, skip_slot=None):
        need = {}

        def add(ev):
            if ev is None:
                return
            kind, obj = ev[0], ev[1]
            if kind == 'd':
                sem, val = obj.sem, obj.total
            else:
                if obj is E and E.key == 'pe':
                    return
                sem, val = obj.sem, ev[2]
            k = id(sem)
            if k not in need or need[k][1] < val:
                need[k] = (sem, val)

        for b in reads:
            add(b.w)
        for b in writes:
            if not (skip_slot is not None and b.w is not None and b.w[0] == 'd' and b.w[1] is skip_slot):
                add(b.w)
            for ev in b.r.values():
                add(ev)
        for k, (sem, val) in need.items():
            if E.seen.get(k, 0) >= val:
                continue
            E.eng.wait_ge(sem, val)
            E.seen[k] = val

    def I(self, ek, meth, **kw):
        E = self.E[ek]
        reads, writes, args = [], [], {}
        for k, v in kw.items():
            if isinstance(v, V):
                (writes if k in ('out', 'accum_out') else reads).append(v.buf)
                args[k] = v.ap
            else:
                args[k] = v
        self._waits(E, reads, writes)
        ins = getattr(E.eng, meth)(**args)
        E.count += 1
        ins.then_inc(E.sem, 1)
        ev = ('e', E, E.count)
        for b in reads:
            b.r[E.key] = ev
        for b in writes:
            b.w = ev
            b.r = {}
        self.ninst += 1
        return ins

    def dma(self, qk, out, in_, indirect=None, **kw):
        E = self.E[qk]
        reads, writes = [], []
        o, i = out, in_
        if isinstance(out, V):
            writes.append(out.buf)
            o = out.ap
        if isinstance(in_, V):
            reads.append(in_.buf)
            i = in_.ap
        if indirect is not None:
            reads.append(indirect[1].buf)
        sb = (writes + reads)[0]
        slot = self._slot(sb)
        self._waits(E, reads, writes, skip_slot=slot)
        if indirect is None:
            ins = E.eng.dma_start(out=o, in_=i, **kw)
        elif indirect[0] == 'in':
            ins = E.eng.indirect_dma_start(out=o, out_offset=None, in_=i,
                                           in_offset=bass.IndirectOffsetOnAxis(ap=indirect[1].ap, axis=0), **kw)
        else:
            ins = E.eng.indirect_dma_start(out=o, out_offset=bass.IndirectOffsetOnAxis(ap=indirect[1].ap, axis=0),
                                           in_=i, in_offset=None, **kw)
        slot.total += 16
        ins.then_inc(slot.sem, 16)
        ev = ('d', slot)
        for b in reads:
            b.r[('d', id(slot))] = ev
        for b in writes:
            b.w = ev
            b.r = {}
        self.ninst += 1
        return ins

    def barrier(self):
        for E in self.E.values():
            for F in list(self.E.values()) + [self.cc]:
                if F is E or F.count == 0:
                    continue
                k = id(F.sem)
                if E.seen.get(k, 0) < F.count:
                    E.eng.wait_ge(F.sem, F.count)
                    E.seen[k] = F.count
            for s in self.slots:
                if s.total == 0:
                    continue
                k = id(s.sem)
                if E.seen.get(k, 0) < s.total:
                    E.eng.wait_ge(s.sem, s.total)
                    E.seen[k] = s.total
        for b in self.bufs:
            b.w, b.r = None, {}

    def final_wait(self):
        E = self.E['sp']
        for s in self.slots:
            if s.total and E.seen.get(id(s.sem), 0) < s.total:
                E.eng.wait_ge(s.sem, s.total)
                E.seen[id(s.sem)] = s.total


class Phase:
    def __init__(self, tr, name):
        self.tr, self.nc, self.name = tr, tr.nc, name
        self.es = ExitStack()
        self.bufs = []
        self.n = 0

    def __enter__(self):
        self.es.__enter__()
        return self

    def __exit__(self, *a):
        self.tr.barrier()
        self.tr.release(self.bufs)
        return self.es.__exit__(*a)

    def sb(self, shape, dt, name=None):
        self.n += 1
        nm = f"{self.name}_{name or 's'}{self.n}"
        h = self.es.enter_context(self.nc.sbuf_tensor(nm, list(shape), dt))
        b = self.tr.buf(nm)
        self.bufs.append(b)
        return Tile(h, b)

    def ps(self, shape, dt, name=None):
        self.n += 1
        nm = f"{self.name}_{name or 'p'}{self.n}"
        h = self.es.enter_context(self.nc.psum_tensor(nm, list(shape), dt))
        b = self.tr.buf(nm)
        self.bufs.append(b)
        return Tile(h, b)


class Ring:
    def __init__(self, items):
        self.items, self.i = items, 0

    def next(self):
        x = self.items[self.i % len(self.items)]
        self.i += 1
        return x


def layer_norm_rows(tr, ph, src, dst, W, g_bc, b_bc, tmp):
    nch = W // 512
    st, mv, rs = tmp['st'], tmp['mv'], tmp['rs']
    for c in range(nch):
        tr.I('dve', 'bn_stats', out=st[:, c, :], in_=src[:, c * 512:(c + 1) * 512])
    tr.I('dve', 'bn_aggr', out=mv[:, :], in_=st[:, 0:nch, :])
    tr.I('dve', 'tensor_scalar', out=rs[:, 0:1], in0=mv[:, 1:2], scalar1=EPS, scalar2=None, op0=ALU.add)
    tr.I('pool', 'tensor_tensor', out=rs[:, 1:2], in0=rs[:, 0:1], in1=tmp['nh'][:, 0:1], op=ALU.pow)
    tr.I('dve', 'tensor_scalar', out=dst[:, 0:W], in0=src[:, 0:W], scalar1=mv[:, 0:1], scalar2=rs[:, 1:2],
         op0=ALU.subtract, op1=ALU.mult)
    tr.I('dve', 'tensor_tensor', out=dst[:, 0:W], in0=dst[:, 0:W], in1=g_bc[:, 0:W], op=ALU.mult)
    tr.I('dve', 'tensor_tensor', out=dst[:, 0:W], in0=dst[:, 0:W], in1=b_bc[:, 0:W], op=ALU.add)


def build(nlayers=DEPTH, halo='ag', dbg=(), stop_after=None, wdepth=DEPTH):
    nc = bass.Bass("TRN2", target_bir_lowering=False)
    dt = nc.dram_tensor

    def ein(name, shape, dtype=F32):
        return dt(name, list(shape), dtype, kind="ExternalInput").ap()

    def scr(name, shape, dtype):
        kind = "ExternalOutput" if name in dbg else "Internal"
        return dt(name, list(shape), dtype, kind=kind).ap()

    x_in = ein("x", [T, D])
    p_in = ein("p", [wdepth, T, 256])
    pos_in = ein("pos", [128, NT], I32)
    cst_in = ein("cst", [128, 1024])
    hidx_in = ein("hidx", [128, 48], I32)
    w_in = ein("w_in", [wdepth, D, PW])
    w_s = ein("w_s", [wdepth, 8, 128, 128])
    b_s = ein("b_s", [wdepth, 8 * 128])
    ln_v_g = ein("ln_v_g", [wdepth, 1024])
    ln_v_b = ein("ln_v_b", [wdepth, 1024])
    w_a = ein("w_a", [wdepth, 1024, D])
    w_b = ein("w_b", [wdepth, 512, D])
    w_o = ein("w_o", [wdepth, D, D])
    ln1_g = ein("ln1_g", [wdepth, D])
    ln1_b = ein("ln1_b", [wdepth, D])
    w_grp = ein("w_grp", [wdepth, D, 4])
    b_grp = ein("b_grp", [wdepth, 4])
    w_rt = ein("w_rt", [wdepth, D, 32])
    b_rt = ein("b_rt", [wdepth, 32])
    w1 = ein("w1", [wdepth, 32 * D, 512])
    w3 = ein("w3", [wdepth, 32 * D, 512])
    w2 = ein("w2", [wdepth, 32 * 512, D])
    w_pg = ein("w_pg", [wdepth, D, D])
    w_pp = ein("w_pp", [wdepth, 256, D])
    ln2_g = ein("ln2_g", [wdepth, D])
    ln2_b = ein("ln2_b", [wdepth, D])
    y_out = dt("y", [T, D], F32, kind="ExternalOutput").ap()

    XA = scr("XA", [T, D], F32)
    XB = scr("XB", [T, D], F32)
    QK = scr("QK", [T, 3072], BF16)
    VV = scr("VV", [T, 1536], BF16)
    KVX32 = dt("KVX", [KVX_ROWS, 256], F32)
    KVX = KVX32[:, :].bitcast(BF16)
    if halo == 'ag':
        KVH32 = dt("KVH", [4 * KVX_ROWS, 256], F32)
        KVH = KVH32[:, :].bitcast(BF16)
    else:
        KVH32 = None
        KVH = None
    OG = scr("OG", [3, T, 516], F32)
    GT = scr("GT", [4096, T], BF16)
    MT = scr("MT", [D, T], BF16)
    R2 = scr("R2", [T, D], F32)
    XS = scr("XS", [NSLOT, D], BF16)
    YS = scr("YS", [NSLOT, D], F32)

    with ExitStack() as es:
        tr = Tracker(nc, es)
        with Phase(tr, "g") as G:
            cst = G.sb([128, 1024], F32, "cst")
            tr.dma('sp', cst[:, :], cst_in[:, :])
            ident_f = cst[:, 0:128]
            identb = G.sb([128, 128], BF16, "identb")
            tr.I('dve', 'tensor_copy', out=identb[:, :], in_=cst[:, 0:128])
            ones_f = G.sb([128, 128], F32, "ones")
            tr.I('dve', 'memset', ap=ones_f[:, :], constant=1.0) if False else None
            nc_ones = tr.I('dve', 'tensor_scalar', out=ones_f[:, :], in0=cst[:, 0:128], scalar1=0.0, scalar2=1.0,
                           op0=ALU.mult, op1=ALU.add)
            nh = G.sb([128, 1], F32, "nh")
            tr.I('dve', 'tensor_scalar', out=nh[:, :], in0=cst[:, 0:1], scalar1=0.0, scalar2=-0.5,
                 op0=ALU.mult, op1=ALU.add)
            hidx = G.sb([128, 48], I32, "hidx")
            tr.dma('sp', hidx[:, :], hidx_in[:, :])
            lnt = {'st': G.sb([128, 4, 6], F32, "st"), 'mv': G.sb([128, 2], F32, "mv"),
                   'rs': G.sb([128, 2], F32, "rs"), 'nh': nh}
            cos4 = G.sb([128, NT, 4, 16], F32, "cos4")
            sin4 = G.sb([128, NT, 4, 16], F32, "sin4")
            with Phase(tr, "rt") as P:
                posi = P.sb([128, NT], I32)
                posf = P.sb([128, NT], F32)
                ang = P.sb([128, NT, 16], F32)
                kf = P.sb([128, NT, 16], F32)
                ki = P.sb([128, NT, 16], I32)
                r = P.sb([128, NT, 16], F32)
                t1 = P.sb([128, NT, 16], F32)
                t2 = P.sb([128, NT, 16], F32)
                sn = P.sb([128, NT, 16], F32)
                cs = P.sb([128, NT, 16], F32)
                tr.dma('sp', posi[:, :], pos_in[:, :])
                tr.I('dve', 'tensor_copy', out=posf[:, :], in_=posi[:, :])
                for t in range(NT):
                    tr.I('dve', 'tensor_scalar', out=ang[:, t, :], in0=cst[:, 768:784], scalar1=posf[:, t:t + 1],
                         scalar2=None, op0=ALU.mult)

                def wrap(dst, src):
                    tr.I('dve', 'tensor_scalar', out=t1[:, :, :], in0=src[:, :, :], scalar1=float(np.pi),
                         scalar2=-TWO_PI, op0=ALU.is_gt, op1=ALU.mult)
                    tr.I('dve', 'tensor_scalar', out=t2[:, :, :], in0=src[:, :, :], scalar1=-float(np.pi),
                         scalar2=TWO_PI, op0=ALU.is_lt, op1=ALU.mult)
                    tr.I('dve', 'tensor_tensor', out=t1[:, :, :], in0=t1[:, :, :], in1=t2[:, :, :], op=ALU.add)
                    tr.I('dve', 'tensor_tensor', out=dst[:, :, :], in0=src[:, :, :], in1=t1[:, :, :], op=ALU.add)

                tr.I('dve', 'tensor_scalar', out=kf[:, :, :], in0=ang[:, :, :], scalar1=float(1.0 / TWO_PI),
                     scalar2=None, op0=ALU.mult)
                tr.I('dve', 'tensor_copy', out=ki[:, :, :], in_=kf[:, :, :])
                tr.I('dve', 'tensor_copy', out=kf[:, :, :], in_=ki[:, :, :])
                tr.I('dve', 'scalar_tensor_tensor', out=r[:, :, :], in0=kf[:, :, :], scalar=-C1, in1=ang[:, :, :],
                     op0=ALU.mult, op1=ALU.add)
                tr.I('dve', 'scalar_tensor_tensor', out=r[:, :, :], in0=kf[:, :, :], scalar=-C2, in1=r[:, :, :],
                     op0=ALU.mult, op1=ALU.add)
                tr.I('dve', 'scalar_tensor_tensor', out=r[:, :, :], in0=kf[:, :, :], scalar=-C3, in1=r[:, :, :],
                     op0=ALU.mult, op1=ALU.add)
                wrap(r, r)
                tr.I('act', 'activation', out=sn[:, :, :], in_=r[:, :, :], func=AF.Sin)
                tr.I('dve', 'tensor_scalar', out=r[:, :, :], in0=r[:, :, :], scalar1=float(np.pi / 2), scalar2=None,
                     op0=ALU.add)
                wrap(r, r)
                tr.I('act', 'activation', out=cs[:, :, :], in_=r[:, :, :], func=AF.Sin)
                for h in range(4):
                    tr.I('dve', 'tensor_copy', out=cos4[:, :, h, :], in_=cs[:, :, :])
                    tr.I('dve', 'tensor_copy', out=sin4[:, :, h, :], in_=sn[:, :, :])

            wbound_reg = nc.gpsimd.to_reg(wdepth * 8192 - 1)
            for li in range(nlayers):
                xin = x_in if li == 0 else XB
                xout = y_out if li == nlayers - 1 else XB
                _layer(nc, tr, G, li, xin, xout, locals(), halo, stop_after)
                if stop_after is not None:
                    break
        tr.final_wait()
    return nc, tr


def _transpose_rows(tr, P, src_dram, xT, identb, cast_q='pool'):
    xb = Ring([P.sb([128, D], BF16, "xb") for _ in range(2)])
    tp = Ring([P.ps([128, 1024], BF16, "tp") for _ in range(4)])
    for t in range(NT):
        b = xb.next()
        tr.dma('pool', b[:, :], src_dram[t * 128:(t + 1) * 128, :])
        for half in range(2):
            ps = tp.next()
            for j in range(8):
                c = half * 8 + j
                tr.I('pe', 'transpose', out=ps[:, j * 128:(j + 1) * 128], in_=b[:, c * 128:(c + 1) * 128],
                     identity=identb[:, :])
            eng = 'act' if half == 0 else 'dve'
            src = ps.v(ps.h[:, :].rearrange("p (j k) -> p j k", k=128))
            if eng == 'act':
                tr.I('act', 'activation', out=xT[:, half * 8:(half + 1) * 8, t * 128:(t + 1) * 128], in_=src,
                     func=AF.Copy)
            else:
                tr.I('dve', 'tensor_copy', out=xT[:, half * 8:(half + 1) * 8, t * 128:(t + 1) * 128], in_=src)


def _wload(tr, dst, w2d, r0, nrows_c, c0, ncols, q='pool'):
    src = w2d[r0:r0 + nrows_c * 128, c0:c0 + ncols].rearrange("(c p) n -> p c n", p=128)
    tr.dma(q, dst, src)


def _layer(nc, tr, G, li, xin, xout, g, halo, stop_after):
    cst, identb, ones_f, hidx, lnt = (g['cst'], g['identb'], g['ones_f'], g['hidx'], g['lnt'])
    cos4, sin4 = g['cos4'], g['sin4']
    w_in, QK, VV, GT, KVX, KVH, OG, MT, XA, R2, XS, YS = (g['w_in'], g['QK'], g['VV'], g['GT'], g['KVX'], g['KVH'],
                                                           g['OG'], g['MT'], g['XA'], g['R2'], g['XS'], g['YS'])
    Wi = w_in[li]

    with Phase(tr, f"A{li}") as A:
        uT = A.sb([128, 8, T], BF16, "uT")
        with Phase(tr, f"B{li}") as B:
            xT = B.sb([128, 16, T], BF16, "xT")
            with Phase(tr, f"t{li}") as P:
                _transpose_rows(tr, P, xin, xT, identb)
            vln = B.sb([128, NT, 1024], BF16, "vln")
            with Phase(tr, f"p{li}") as P:
                wt = Ring([P.sb([128, 16, 512], BF16, "wt") for _ in range(2)])
                acc = Ring([P.ps([128, 512], F32, "acc") for _ in range(4)])
                ot = Ring([P.sb([128, 512], BF16, "ot") for _ in range(3)])
                rt = Ring([P.sb([128, 4, 4, 16], F32, "rt") for _ in range(2)])
                for j in (range(7, 13) if stop_after == 'KV' else range(4, 13)):
                    w = wt.next()
                    _wload(tr, w[:, :, :], Wi, 0, 16, j * 512, 512)
                    for t in range(NT):
                        ps = acc.next()
                        for kc in range(16):
                            tr.I('pe', 'matmul', out=ps[:, :], lhsT=xT[:, kc, t * 128:(t + 1) * 128], rhs=w[:, kc, :],
                                 start=(kc == 0), stop=(kc == 15))
                        o = ot.next()
                        if j < 10:
                            p3 = ps.v(ps.h[:, :].rearrange("p (h d) -> p h d", h=4))
                            o3 = o.v(o.h[:, :].rearrange("p (h d) -> p h d", h=4))
                            x1 = ps.v(p3.ap[:, :, 0:16])
                            x2 = ps.v(p3.ap[:, :, 16:32])
                            r_ = rt.next()
                            tr.I('dve', 'tensor_tensor', out=r_[:, 0, :, :], in0=x1, in1=cos4[:, t, :, :], op=ALU.mult)
                            tr.I('dve', 'tensor_tensor', out=r_[:, 1, :, :], in0=x2, in1=sin4[:, t, :, :], op=ALU.mult)
                            tr.I('dve', 'tensor_tensor', out=r_[:, 2, :, :], in0=x2, in1=cos4[:, t, :, :], op=ALU.mult)
                            tr.I('dve', 'tensor_tensor', out=r_[:, 3, :, :], in0=x1, in1=sin4[:, t, :, :], op=ALU.mult)
                            tr.I('dve', 'tensor_tensor', out=o.v(o3.ap[:, :, 0:16]), in0=r_[:, 0, :, :],
                                 in1=r_[:, 1, :, :], op=ALU.subtract)
                            tr.I('dve', 'tensor_tensor', out=o.v(o3.ap[:, :, 16:32]), in0=r_[:, 2, :, :],
                                 in1=r_[:, 3, :, :], op=ALU.add)
                            tr.I('act', 'activation', out=o.v(o3.ap[:, :, 32:128]), in_=ps.v(p3.ap[:, :, 32:128]),
                                 func=AF.Copy)
                            tr.dma('sp', QK[t * 128:(t + 1) * 128, (j - 4) * 512:(j - 3) * 512], o[:, :])
                        else:
                            tr.I('act', 'activation', out=o[:, :], in_=ps[:, :], func=AF.Copy)
                            tr.dma('sp', VV[t * 128:(t + 1) * 128, (j - 10) * 512:(j - 9) * 512], o[:, :])
                tr.barrier()
                dummy = P.sb([128, 1], F32, "dummy")
                tr.dma('sp', V(KVX[0:2048, :], dummy.buf), QK[:, 1536 + 1024:1536 + 1536])
                tr.dma('sp', V(KVX[2048:4096, :], dummy.buf), VV[:, 1024:1536])
                tr.dma('sp', V(KVX[4096:4608, :], dummy.buf), QK[T - 512:T, 1536 + 512:1536 + 1024])
                tr.dma('sp', V(KVX[4608:5120, :], dummy.buf), VV[T - 512:T, 512:1024])
                tr.dma('sp', V(KVX[5120:5248, :], dummy.buf), QK[T - 128:T, 1536:1536 + 512])
                tr.dma('sp', V(KVX[5248:5376, :], dummy.buf), VV[T - 128:T, 0:512])
                if halo == 'ag':
                    dslot = dummy.buf.slot
                    EP = tr.E['pool']
                    EP.eng.wait_ge(dslot.sem, dslot.total)
                    EP.seen[id(dslot.sem)] = dslot.total
                    off = 0
                    for (r0, r1) in KV_CHUNKS:
                        n_ = r1 - r0
                        ins = nc.gpsimd.collective_compute("AllGather", ALU.bypass, replica_groups=[[0, 1, 2, 3], [4, 5, 6, 7]],
                                                           ins=[g['KVX32'][r0:r1, :]], outs=[g['KVH32'][off:off + 4 * n_, :]])
                        ins.then_inc(tr.cc.sem)
                        tr.cc.count += 1
                        off += 4 * n_
                for j in (() if stop_after == 'KV' else (0, 1, 13, 14, 15, 16, 17, 18, 19, 20)):
                    w = wt.next()
                    _wload(tr, w[:, :, :], Wi, 0, 16, j * 512, 512)
                    for fc in range(4):
                        for tb in range(4):
                            ps = acc.next()
                            for kc in range(16):
                                tr.I('pe', 'matmul', out=ps[:, :], lhsT=w[:, kc, fc * 128:(fc + 1) * 128],
                                     rhs=xT[:, kc, tb * 512:(tb + 1) * 512], start=(kc == 0), stop=(kc == 15))
                            if j < 2:
                                tr.I('act', 'activation', out=uT[:, j * 4 + fc, tb * 512:(tb + 1) * 512], in_=ps[:, :],
                                     func=AF.Gelu_apprx_tanh)
                            else:
                                o = ot.next()
                                tr.I('act', 'activation', out=o[:, :], in_=ps[:, :], func=AF.Sigmoid)
                                row = ((j - 13) * 4 + fc) * 128
                                tr.dma('sp', GT[row:row + 128, tb * 512:(tb + 1) * 512], o[:, :])
            with Phase(tr, f"v{li}") as P:
              if stop_after != 'KV':
                    wv = P.sb([128, 16, 1024], BF16, "wv")
                    _wload(tr, wv[:, :, 0:512], Wi, 0, 16, 1024, 512)
                    _wload(tr, wv[:, :, 512:1024], Wi, 0, 16, 1536, 512)
                    gbc = P.sb([128, 1024], F32, "gbc")
                    bbc = P.sb([128, 1024], F32, "bbc")
                    tr.dma('sp', gbc[:, :], g['ln_v_g'][li, :].partition_broadcast(128))
                    tr.dma('sp', bbc[:, :], g['ln_v_b'][li, :].partition_broadcast(128))
                    acc2 = Ring([P.ps([128, 1024], F32, "acc2") for _ in range(2)])
                    vg = Ring([P.sb([128, 1024], F32, "vg") for _ in range(2)])
                    vn = Ring([P.sb([128, 1024], F32, "vn") for _ in range(2)])
                    for t in range(NT):
                        ps = acc2.next()
                        for hf in range(2):
                            for kc in range(16):
                                tr.I('pe', 'matmul', out=ps[:, hf * 512:(hf + 1) * 512], lhsT=xT[:, kc, t * 128:(t + 1) * 128],
                                     rhs=wv[:, kc, hf * 512:(hf + 1) * 512], start=(kc == 0), stop=(kc == 15))
                        v_ = vg.next()
                        tr.I('act', 'activation', out=v_[:, :], in_=ps[:, :], func=AF.Gelu_apprx_tanh)
                        n_ = vn.next()
                        layer_norm_rows(tr, P, v_, n_, 1024, gbc, bbc, lnt)
                        tr.I('act', 'activation', out=vln[:, t, :], in_=n_[:, :], func=AF.Copy)
            with Phase(tr, f"s{li}") as P:
              if stop_after != 'KV':
                    wsf = P.sb([128, 8, 128], F32, "wsf")
                    tr.dma('sp', wsf[:, :, :], g['w_s'][li].rearrange("g t s -> t g s"))
                    wsb = P.sb([128, 8, 128], BF16, "wsb")
                    for gg in range(8):
                        tr.I('dve', 'tensor_tensor', out=wsb[:, gg, :], in0=wsf[:, gg, :], in1=cst[:, 128:256], op=ALU.mult)
                    wsT = P.sb([128, 8, 128], BF16, "wsT")
                    tps = P.ps([128, 1024], BF16, "tps")
                    for gg in range(8):
                        tr.I('pe', 'transpose', out=tps[:, gg * 128:(gg + 1) * 128], in_=wsb[:, gg, :], identity=identb[:, :])
                    tr.I('dve', 'tensor_copy', out=wsT[:, :, :], in_=tps.v(tps.h[:, :].rearrange("p (g k) -> p g k", k=128)))
                    bsb = P.sb([128, 8, 128], F32, "bsb")
                    tr.dma('sp', bsb[:, :, :], g['b_s'][li, :].partition_broadcast(128).rearrange("p (g t) -> p g t", t=128))
                    zps = Ring([P.ps([128, 1024], F32, "zps") for _ in range(2)])
                    zt = Ring([P.sb([128, 8, 128], F32, "zt") for _ in range(2)])
                    for n in range(NT):
                        ps = zps.next()
                        for gg in range(8):
                            tr.I('pe', 'matmul', out=ps[:, gg * 128:(gg + 1) * 128], lhsT=vln[:, n, gg * 128:(gg + 1) * 128],
                                 rhs=wsT[:, gg, :], start=True, stop=True)
                        z = zt.next()
                        tr.I('dve', 'tensor_tensor', out=z[:, :, :], in0=ps.v(ps.h[:, :].rearrange("p (g t) -> p g t", t=128)),
                             in1=bsb[:, :, :], op=ALU.add)
                        tr.I('dve', 'tensor_tensor', out=uT[:, :, n * 128:(n + 1) * 128], in0=z[:, :, :],
                             in1=uT[:, :, n * 128:(n + 1) * 128], op=ALU.mult)
        if stop_after == 'A':
            with Phase(tr, "dbgA") as P:
                tr.dma('sp', g['XA'][0:128, :].rearrange("p (c t) -> p c t", c=1)[:, 0, 0:T] if False else
                       g['MT'][0:1024, :].rearrange("(c p) t -> p c t", p=128), uT[:, :, :])
            return
        aT = uT

        hsrc = KVH if KVH is not None else None
        if stop_after == 'KV':
            return

        with Phase(tr, f"at{li}") as P:
            qkv = Ring([P.sb([128, 5, 512], BF16, "qkv") for _ in range(4)])
            tps = Ring([P.ps([128, 2048], BF16, "tps") for _ in range(1)])
            qkT = Ring([P.sb([128, 1536], BF16, "qkT") for _ in range(2)])
            sps = Ring([P.ps([128, 1024], F32, "sps") for _ in range(1)])
            ssb = Ring([P.sb([128, 4, 256], F32, "ssb") for _ in range(2)])
            psb = Ring([P.sb([128, 4, 256], BF16, "psb") for _ in range(2)])
            ptp = Ring([P.ps([128, 1024], BF16, "ptp") for _ in range(2)])
            pT = Ring([P.sb([128, 8, 128], BF16, "pT") for _ in range(2)])
            ops_ = Ring([P.ps([128, 512], F32, "ops") for _ in range(2)])
            osb = Ring([P.sb([128, 516], F32, "osb") for _ in range(2)])
            sm = Ring([P.sb([128, 16], F32, "sm") for _ in range(2)])
            mask4 = P.sb([128, 4, 256], F32, "mask4")
            maskh4 = P.sb([128, 4, 256], F32, "maskh4")
            for h in range(4):
                tr.I('dve', 'tensor_copy', out=mask4[:, h, :], in_=cst[:, 256:512])
                tr.I('dve', 'tensor_copy', out=maskh4[:, h, :], in_=cst[:, 512:768])
            blocks = []
            hcol = 0
            for gi, dil in enumerate((1, 4, 16)):
                nb = 16 // dil
                for r in range(dil):
                    for n in range(nb):
                        hc = None
                        if n == 0:
                            hc = hcol
                            hcol += 2
                        blocks.append((gi, dil, r, n, hc))

            def loads(blk):
                gi, dil, r, n, hc = blk
                qv = QK.rearrange("(n i r) c -> r n i c", r=dil, i=128)
                vv = VV.rearrange("(n i r) c -> r n i c", r=dil, i=128)
                qc, kc_, vc = gi * 512, 1536 + gi * 512, gi * 512
                b = qkv.next()
                war = dict(b.buf.r)
                tr.dma('sp', b[:, 0, :], qv[r, n, :, qc:qc + 512])
                tr.dma('sp', b[:, 2, :], qv[r, n, :, kc_:kc_ + 512])
                tr.dma('sp', b[:, 4, :], vv[r, n, :, vc:vc + 512])
                first = (n == 0)
                if not first:
                    tr.dma('sp', b[:, 1, :], qv[r, n - 1, :, kc_:kc_ + 512])
                    tr.dma('sp', b[:, 3, :], vv[r, n - 1, :, vc:vc + 512])
                elif hsrc is not None:
                    b.buf.r = dict(war)
                    tr.dma('pool', b[:, 1, :], hsrc[:, :], indirect=('in', hidx[:, hc:hc + 1]))
                    tr.dma('pool', b[:, 3, :], hsrc[:, :], indirect=('in', hidx[:, hc + 1:hc + 2]))
                else:
                    tr.dma('sp', b[:, 1, :], qv[r, n, :, kc_:kc_ + 512])
                    tr.dma('sp', b[:, 3, :], vv[r, n, :, vc:vc + 512])
                return b

            def stage_a(blk, b):
                gi, dil, r, n, hc = blk
                first = (n == 0)
                tp = tps.next()
                for h in range(4):
                    tr.I('pe', 'transpose', out=tp[:, h * 128:(h + 1) * 128], in_=b[:, 0, h * 128:(h + 1) * 128],
                         identity=identb[:, :])
                    for hf in range(2):
                        o0 = 512 + h * 256 + hf * 128
                        tr.I('pe', 'transpose', out=tp[:, o0:o0 + 128], in_=b[:, 1 + hf, h * 128:(h + 1) * 128],
                             identity=identb[:, :])
                qt = qkT.next()
                tr.I('dve', 'tensor_copy', out=qt[:, :], in_=tp[:, 0:1536])
                sp_ = sps.next()
                for h in range(4):
                    tr.I('pe', 'matmul', out=sp_[:, h * 256:(h + 1) * 256], lhsT=qt[:, h * 128:(h + 1) * 128],
                         rhs=qt[:, 512 + h * 256:512 + (h + 1) * 256], start=True, stop=True)
                s_ = ssb.next()
                mk = maskh4 if first else mask4
                tr.I('dve', 'scalar_tensor_tensor', out=s_[:, :, :],
                     in0=sp_.v(sp_.h[:, :].rearrange("p (h k) -> p h k", k=256)), scalar=SCALE, in1=mk[:, :, :],
                     op0=ALU.mult, op1=ALU.add)
                m_ = sm.next()
                tr.I('dve', 'tensor_reduce', out=m_[:, 0:4], in_=s_[:, :, :], axis=AX.X, op=ALU.max, negate=True)
                p_ = psb.next()
                for h in range(4):
                    tr.I('act', 'activation', out=p_[:, h, :], in_=s_[:, h, :], func=AF.Exp, bias=m_[:, h:h + 1],
                         scale=1.0, accum_out=m_[:, 4 + h:5 + h])
                return (b, m_, p_)

            def stage_b(blk, st):
                gi, dil, r, n, hc = blk
                b, m_, p_ = st
                og = OG[gi].rearrange("(n i r) c -> r n i c", r=dil, i=128)
                pp = ptp.next()
                for h in range(4):
                    for hf in range(2):
                        j = h * 2 + hf
                        tr.I('pe', 'transpose', out=pp[:, j * 128:(j + 1) * 128],
                             in_=p_[:, h, hf * 128:(hf + 1) * 128], identity=identb[:, :])
                pt = pT.next()
                tr.I('dve', 'tensor_copy', out=pt[:, :, :], in_=pp.v(pp.h[:, :].rearrange("p (j k) -> p j k", k=128)))
                op_ = ops_.next()
                for h in range(4):
                    for hf in range(2):
                        tr.I('pe', 'matmul', out=op_[:, h * 128:(h + 1) * 128], lhsT=pt[:, h * 2 + hf, :],
                             rhs=b[:, 3 + hf, h * 128:(h + 1) * 128], start=(hf == 0), stop=(hf == 1))
                o_ = osb.next()
                tr.I('dve', 'reciprocal', out=m_[:, 8:12], in_=m_[:, 4:8])
                for h in range(4):
                    tr.I('act', 'activation', out=o_[:, h * 128:(h + 1) * 128], in_=op_[:, h * 128:(h + 1) * 128],
                         func=AF.Copy, scale=m_[:, 8 + h:9 + h])
                tr.I('act', 'activation', out=m_[:, 12:16], in_=m_[:, 4:8], func=AF.Ln)
                tr.I('dve', 'tensor_tensor', out=o_[:, 512:516], in0=m_[:, 12:16], in1=m_[:, 0:4], op=ALU.subtract)
                tr.dma('sp', og[r, n, :, :], o_[:, :])

            nb_ = len(blocks)
            lb = {0: loads(blocks[0]), 1: loads(blocks[1])}
            prev = None
            for i, blk in enumerate(blocks):
                if i + 2 < nb_:
                    lb[i + 2] = loads(blocks[i + 2])
                st = stage_a(blk, lb.pop(i))
                if prev is not None:
                    stage_b(*prev)
                prev = (blk, st)
            stage_b(*prev)
        if stop_after == 'AT':
            return

        with Phase(tr, f"m{li}") as P:
            bT = P.sb([128, 4, T], BF16, "bT")
            with Phase(tr, f"mg{li}") as Q:
                ogt = Ring([Q.sb([128, 3, 516], F32, "ogt") for _ in range(2)])
                l3 = Ring([Q.sb([128, 8, 4], F32, "l3") for _ in range(2)])
                mg = Ring([Q.sb([128, 512], F32, "mg") for _ in range(2)])
                mgb = Ring([Q.sb([128, 512], BF16, "mgb") for _ in range(2)])
                tp = Ring([Q.ps([128, 512], BF16, "tp") for _ in range(2)])
                ogv = OG.rearrange("g t c -> t g c")
                for t in range(NT):
                    o = ogt.next()
                    tr.dma('sp', o[:, :, :], ogv[t * 128:(t + 1) * 128, :, :])
                    l = l3.next()
                    tr.I('dve', 'tensor_tensor', out=l[:, 3, :], in0=o[:, 0, 512:516], in1=o[:, 1, 512:516], op=ALU.max)
                    tr.I('dve', 'tensor_tensor', out=l[:, 3, :], in0=l[:, 3, :], in1=o[:, 2, 512:516], op=ALU.max)
                    for gi in range(3):
                        tr.I('dve', 'tensor_tensor', out=l[:, gi, :], in0=o[:, gi, 512:516], in1=l[:, 3, :], op=ALU.subtract)
                    tr.I('act', 'activation', out=l[:, 0:3, :], in_=l[:, 0:3, :], func=AF.Exp)
                    tr.I('dve', 'tensor_tensor', out=l[:, 4, :], in0=l[:, 0, :], in1=l[:, 1, :], op=ALU.add)
                    tr.I('dve', 'tensor_tensor', out=l[:, 4, :], in0=l[:, 4, :], in1=l[:, 2, :], op=ALU.add)
                    tr.I('dve', 'reciprocal', out=l[:, 4, :], in_=l[:, 4, :])
                    for gi in range(3):
                        tr.I('dve', 'tensor_tensor', out=l[:, 5 + gi, :], in0=l[:, gi, :], in1=l[:, 4, :], op=ALU.mult)
                    m = mg.next()
                    for h in range(4):
                        hs = slice(h * 128, (h + 1) * 128)
                        tr.I('dve', 'tensor_scalar', out=m[:, hs], in0=o[:, 0, hs], scalar1=l[:, 5, h:h + 1], scalar2=None,
                             op0=ALU.mult)
                        for gi in (1, 2):
                            tr.I('dve', 'scalar_tensor_tensor', out=m[:, hs], in0=o[:, gi, hs], scalar=l[:, 5 + gi, h:h + 1],
                                 in1=m[:, hs], op0=ALU.mult, op1=ALU.add)
                    mb = mgb.next()
                    tr.I('act', 'activation', out=mb[:, :], in_=m[:, :], func=AF.Copy)
                    ps = tp.next()
                    for h in range(4):
                        tr.I('pe', 'transpose', out=ps[:, h * 128:(h + 1) * 128], in_=mb[:, h * 128:(h + 1) * 128],
                             identity=identb[:, :])
                    tr.I('dve', 'tensor_copy', out=bT[:, :, t * 128:(t + 1) * 128],
                         in_=ps.v(ps.h[:, :].rearrange("p (j k) -> p j k", k=128)))
            if stop_after == 'MG':
                tr.dma('sp', MT[0:512, :].rearrange("(c p) t -> p c t", p=128), bT[:, :, :])
                return
            with Phase(tr, f"ma{li}") as Q:
                Wa = Q.sb([128, 8, D], BF16, "Wa")
                Wb = Q.sb([128, 4, D], BF16, "Wb")
                for c4 in range(4):
                    _wload(tr, Wa[:, :, c4 * 512:(c4 + 1) * 512], g['w_a'][li], 0, 8, c4 * 512, 512)
                    _wload(tr, Wb[:, :, c4 * 512:(c4 + 1) * 512], g['w_b'][li], 0, 4, c4 * 512, 512)
                gts = Ring([Q.sb([128, 2, 512], BF16, "gts") for _ in range(3)])
                pa = Ring([Q.ps([128, 512], F32, "pa") for _ in range(2)])
                pb = Ring([Q.ps([128, 512], F32, "pb") for _ in range(2)])
                tm = Ring([Q.sb([128, 2, 512], F32, "tm") for _ in range(2)])
                mo = Ring([Q.sb([128, 512], BF16, "mo") for _ in range(3)])
                for tb in range(4):
                    ts = slice(tb * 512, (tb + 1) * 512)
                    for fc in range(16):
                        gt_ = gts.next()
                        tr.dma('act', gt_[:, 0, :], GT[fc * 128:(fc + 1) * 128, ts])
                        tr.dma('act', gt_[:, 1, :], GT[2048 + fc * 128:2048 + (fc + 1) * 128, ts])
                        a_ = pa.next()
                        for kc in range(8):
                            tr.I('pe', 'matmul', out=a_[:, :], lhsT=Wa[:, kc, fc * 128:(fc + 1) * 128], rhs=aT[:, kc, ts],
                                 start=(kc == 0), stop=(kc == 7))
                        b_ = pb.next()
                        for kc in range(4):
                            tr.I('pe', 'matmul', out=b_[:, :], lhsT=Wb[:, kc, fc * 128:(fc + 1) * 128], rhs=bT[:, kc, ts],
                                 start=(kc == 0), stop=(kc == 3))
                        t_ = tm.next()
                        tr.I('dve', 'tensor_tensor', out=t_[:, 0, :], in0=a_[:, :], in1=gt_[:, 0, :], op=ALU.mult)
                        tr.I('dve', 'tensor_tensor', out=t_[:, 1, :], in0=b_[:, :], in1=gt_[:, 1, :], op=ALU.mult)
                        m_ = mo.next()
                        tr.I('pool', 'tensor_tensor', out=m_[:, :], in0=t_[:, 0, :], in1=t_[:, 1, :], op=ALU.add)
                        tr.dma('sp', MT[fc * 128:(fc + 1) * 128, ts], m_[:, :])
    if stop_after == 'MA':
        return

    with Phase(tr, f"o{li}") as P:
        Wo = P.sb([128, 16, D], BF16, "Wo")
        for c4 in range(4):
            _wload(tr, Wo[:, :, c4 * 512:(c4 + 1) * 512], g['w_o'][li], 0, 16, c4 * 512, 512)
        gbc = P.sb([128, D], F32, "gbc")
        bbc = P.sb([128, D], F32, "bbc")
        tr.dma('sp', gbc[:, :], g['ln1_g'][li, :].partition_broadcast(128))
        tr.dma('sp', bbc[:, :], g['ln1_b'][li, :].partition_broadcast(128))
        mtb = Ring([P.sb([128, 16, 512], BF16, "mtb") for _ in range(2)])
        xt_ = Ring([P.sb([128, D], F32, "xt") for _ in range(2)])
        rr = Ring([P.sb([128, D], F32, "rr") for _ in range(2)])
        acc = Ring([P.ps([128, 512], F32, "acc") for _ in range(4)])
        mtv = MT.rearrange("(c p) t -> p c t", p=128)
        for tb in range(4):
            mb = mtb.next()
            tr.dma('act', mb[:, :, :], mtv[:, :, tb * 512:(tb + 1) * 512])
            for tt in range(4):
                t = tb * 4 + tt
                x_ = xt_.next()
                tr.dma('sp', x_[:, :], xin[t * 128:(t + 1) * 128, :])
                r_ = rr.next()
                for cb in range(4):
                    ps = acc.next()
                    for kc in range(16):
                        tr.I('pe', 'matmul', out=ps[:, :], lhsT=mb[:, kc, tt * 128:(tt + 1) * 128],
                             rhs=Wo[:, kc, cb * 512:(cb + 1) * 512], start=(kc == 0), stop=(kc == 15))
                    tr.I('dve', 'scalar_tensor_tensor', out=r_[:, cb * 512:(cb + 1) * 512], in0=x_[:, cb * 512:(cb + 1) * 512],
                         scalar=ALPHA, in1=ps[:, :], op0=ALU.mult, op1=ALU.add)
                layer_norm_rows(tr, P, r_, r_, D, gbc, bbc, lnt)
                tr.dma('act', XA[t * 128:(t + 1) * 128, :], r_[:, :])
    if stop_after == 'O':
        return

    with Phase(tr, f"r{li}") as R:
        sel = R.sb([128, NT, 32], F32, "sel")
        oh1 = R.sb([128, NT, 32], F32, "oh1")
        gates = R.sb([128, NT, 2], F32, "gates")
        slot_i = R.sb([128, NT, 2], I32, "slot_i")
        widx = R.sb([128, 64, 2], I32, "widx")
        with Phase(tr, f"rp{li}") as P:
            xT = P.sb([128, 16, T], BF16, "xT1")
            with Phase(tr, f"rt{li}") as Q:
                _transpose_rows(tr, Q, XA, xT, identb)
            Wr = P.sb([128, 16, 36], BF16, "Wr")
            tr.dma('pool', Wr[:, :, 0:4], g['w_grp'][li].rearrange("(c p) n -> p c n", p=128))
            tr.dma('pool', Wr[:, :, 4:36], g['w_rt'][li].rearrange("(c p) n -> p c n", p=128))
            brb = P.sb([128, 36], F32, "brb")
            tr.dma('sp', brb[:, 0:4], g['b_grp'][li, :].partition_broadcast(128))
            tr.dma('sp', brb[:, 4:36], g['b_rt'][li, :].partition_broadcast(128))
            Wpp = P.sb([128, 2, D], BF16, "Wpp")
            for c4 in range(4):
                _wload(tr, Wpp[:, :, c4 * 512:(c4 + 1) * 512], g['w_pp'][li], 0, 2, c4 * 512, 512)
            pT = P.sb([128, NT, 2, 128], BF16, "pT")
            with Phase(tr, f"rr{li}") as Q:
                psr = Ring([Q.ps([128, 64], F32, "psr") for _ in range(2)])
                lg = Ring([Q.sb([128, 36], F32, "lg") for _ in range(2)])
                msk = Ring([Q.sb([128, 32], F32, "msk") for _ in range(2)])
                sc = Ring([Q.sb([128, 32], F32, "sc") for _ in range(2)])
                pb_ = Ring([Q.sb([128, 256], BF16, "pb") for _ in range(2)])
                ptp = Ring([Q.ps([128, 256], BF16, "ptp") for _ in range(2)])
                for t in range(NT):
                    ps = psr.next()
                    for kc in range(16):
                        tr.I('pe', 'matmul', out=ps[:, 0:36], lhsT=xT[:, kc, t * 128:(t + 1) * 128], rhs=Wr[:, kc, :],
                             start=(kc == 0), stop=(kc == 15))
                    l = lg.next()
                    tr.I('dve', 'tensor_tensor', out=l[:, :], in0=ps[:, 0:36], in1=brb[:, :], op=ALU.add)
                    s = sc.next()
                    tr.I('dve', 'tensor_reduce', out=s[:, 0:1], in_=l[:, 0:4], axis=AX.X, op=ALU.max)
                    tr.I('dve', 'tensor_scalar', out=s[:, 1:2], in0=s[:, 0:1], scalar1=-1.0, scalar2=None, op0=ALU.mult)
                    tr.I('act', 'activation', out=s[:, 4:8], in_=l[:, 0:4], func=AF.Exp, bias=s[:, 1:2], scale=1.0,
                         accum_out=s[:, 2:3])
                    tr.I('dve', 'reciprocal', out=s[:, 3:4], in_=s[:, 2:3])
                    tr.I('dve', 'tensor_scalar', out=s[:, 8:12], in0=l[:, 0:4], scalar1=s[:, 0:1], scalar2=None,
                         op0=ALU.is_ge)
                    tr.I('dve', 'tensor_scalar', out=s[:, 8:12], in0=s[:, 8:12], scalar1=-1.0, scalar2=1.0e9,
                         op0=ALU.add, op1=ALU.mult)
                    m = msk.next()
                    for gg in range(4):
                        tr.I('dve', 'tensor_scalar', out=m[:, gg * 8:(gg + 1) * 8], in0=l[:, 4 + gg * 8:12 + gg * 8],
                             scalar1=s[:, 8 + gg:9 + gg], scalar2=None, op0=ALU.add)
                    tr.I('dve', 'max', out=s[:, 12:20], in_=m[:, :])
                    tr.I('dve', 'tensor_scalar', out=sel[:, t, :], in0=m[:, :], scalar1=s[:, 13:14], scalar2=None,
                         op0=ALU.is_ge)
                    tr.I('dve', 'tensor_scalar', out=oh1[:, t, :], in0=m[:, :], scalar1=s[:, 12:13], scalar2=None,
                         op0=ALU.is_ge)
                    tr.I('dve', 'tensor_tensor', out=s[:, 20:21], in0=s[:, 13:14], in1=s[:, 12:13], op=ALU.subtract)
                    tr.I('act', 'activation', out=s[:, 20:21], in_=s[:, 20:21], func=AF.Exp)
                    tr.I('dve', 'tensor_scalar', out=s[:, 21:22], in0=s[:, 20:21], scalar1=1.0, scalar2=None, op0=ALU.add)
                    tr.I('dve', 'reciprocal', out=s[:, 22:23], in_=s[:, 21:22])
                    tr.I('dve', 'tensor_tensor', out=gates[:, t, 0:1], in0=s[:, 22:23], in1=s[:, 3:4], op=ALU.mult)
                    tr.I('dve', 'tensor_tensor', out=gates[:, t, 1:2], in0=gates[:, t, 0:1], in1=s[:, 20:21], op=ALU.mult)
                    pb = pb_.next()
                    tr.dma('pool', pb[:, :], g['p_in'][li, t * 128:(t + 1) * 128, :])
                    pp = ptp.next()
                    for c in range(2):
                        tr.I('pe', 'transpose', out=pp[:, c * 128:(c + 1) * 128], in_=pb[:, c * 128:(c + 1) * 128],
                             identity=identb[:, :])
                    tr.I('act', 'activation', out=pT[:, t, :, :], in_=pp.v(pp.h[:, :].rearrange("p (c k) -> p c k", k=128)),
                         func=AF.Copy)
            with Phase(tr, f"rs{li}") as P:
                U = cst[:, 784:912]
                prk = P.ps([128, NT, 32], F32, "prk")
                pct = P.ps([128, 32], F32, "pct")
                for t in range(NT):
                    tr.I('pe', 'matmul', out=prk[:, t, :], lhsT=U, rhs=sel[:, t, :], start=True, stop=(t == 0))
                    for t2 in range(t):
                        tr.I('pe', 'matmul', out=prk[:, t, :], lhsT=ones_f[:, :], rhs=sel[:, t2, :], start=False,
                             stop=(t2 == t - 1))
                for t in range(NT):
                    tr.I('pe', 'matmul', out=pct[:, :], lhsT=ones_f[:, :], rhs=sel[:, t, :], start=(t == 0), stop=(t == NT - 1))
                cnt = P.sb([128, 32], F32, "cnt")
                nb_ = P.sb([128, 32], F32, "nb")
                cum = P.sb([128, 32], F32, "cum")
                base = P.sb([128, 32], F32, "base")
                on32 = P.sb([128, 32], F32, "on32")
                tr.I('dve', 'tensor_copy', out=cnt[:, :], in_=pct[:, :])
                tr.I('dve', 'tensor_copy', out=on32[:, :], in_=ones_f[:, 0:32])
                tr.I('dve', 'tensor_scalar', out=nb_[:, :], in0=cnt[:, :], scalar1=0.0, scalar2=None, op0=ALU.is_gt)
                for j in range(1, 8):
                    tr.I('dve', 'scalar_tensor_tensor', out=nb_[:, :], in0=cnt[:, :], scalar=float(SLOTB * j), in1=nb_[:, :],
                         op0=ALU.is_gt, op1=ALU.add)
                tr.I('dve', 'tensor_tensor_scan', out=cum[:, :], data0=on32[:, :], data1=nb_[:, :], initial=0.0,
                     op0=ALU.mult, op1=ALU.add)
                tr.I('dve', 'tensor_tensor', out=base[:, :], in0=cum[:, :], in1=nb_[:, :], op=ALU.subtract)
                tr.I('dve', 'tensor_scalar', out=base[:, :], in0=base[:, :], scalar1=float(SLOTB), scalar2=None, op0=ALU.mult)
                slot = P.sb([128, NT, 32], F32, "slot")
                for t in range(NT):
                    tr.I('dve', 'tensor_tensor', out=slot[:, t, :], in0=prk[:, t, :], in1=base[:, :], op=ALU.add)
                pr = P.sb([128, NT, 32], F32, "pr")
                sl = P.sb([128, NT, 2], F32, "sl")
                tr.I('dve', 'tensor_tensor', out=pr[:, :, :], in0=slot[:, :, :], in1=oh1[:, :, :], op=ALU.mult)
                tr.I('dve', 'tensor_reduce', out=sl[:, :, 0], in_=pr[:, :, :], axis=AX.X, op=ALU.add)
                tr.I('dve', 'tensor_tensor', out=pr[:, :, :], in0=slot[:, :, :], in1=sel[:, :, :], op=ALU.mult)
                tr.I('dve', 'tensor_reduce', out=sl[:, :, 1], in_=pr[:, :, :], axis=AX.X, op=ALU.add)
                tr.I('dve', 'tensor_tensor', out=sl[:, :, 1], in0=sl[:, :, 1], in1=sl[:, :, 0], op=ALU.subtract)
                tr.I('dve', 'tensor_copy', out=slot_i[:, :, :], in_=sl[:, :, :])
                be = P.sb([128, 64], F32, "be")
                tr.I('dve', 'tensor_scalar', out=be[:, :], in0=cst[:, 912:976], scalar1=cum[:, 0:1], scalar2=None, op0=ALU.is_ge)
                for e in range(1, 32):
                    tr.I('dve', 'scalar_tensor_tensor', out=be[:, :], in0=cst[:, 912:976], scalar=cum[:, e:e + 1], in1=be[:, :],
                         op0=ALU.is_ge, op1=ALU.add)
                flag = P.sb([128, 64], F32, "flag")
                tr.I('dve', 'tensor_scalar', out=flag[:, :], in0=be[:, :], scalar1=31.5, scalar2=1.0e6, op0=ALU.is_ge, op1=ALU.mult)
                tr.I('dve', 'tensor_scalar', out=be[:, :], in0=be[:, :], scalar1=31.0, scalar2=128.0, op0=ALU.min, op1=ALU.mult)
                tr.I('dve', 'tensor_scalar', out=be[:, :], in0=be[:, :], scalar1=cst[:, 976:977], scalar2=None, op0=ALU.add)
                tr.I('dve', 'scalar_tensor_tensor', out=be[:, :], in0=be[:, :], scalar=2.0, in1=flag[:, :], op0=ALU.mult, op1=ALU.add)
                be2 = P.sb([128, 64, 2], F32, "be2")
                for h4 in range(2):
                    tr.I('dve', 'tensor_scalar', out=be2[:, :, h4], in0=be[:, :], scalar1=float(li * 8192 + h4), scalar2=None,
                         op0=ALU.add)
                tr.I('dve', 'tensor_copy', out=widx[:, :, :], in_=be2[:, :, :])
            with Phase(tr, f"rl{li}") as Q:
                wt = Ring([Q.sb([128, 16, 512], BF16, "wt") for _ in range(4)])
                wts = []
                for cb in range(4):
                    w = wt.next()
                    _wload(tr, w[:, :, :], g['w_pg'][li], 0, 16, cb * 512, 512)
                    wts.append(w)
                xb = Ring([Q.sb([128, D], BF16, "xb") for _ in range(3)])
                for t in range(NT):
                    b = xb.next()
                    tr.dma('pool', b[:, :], XA[t * 128:(t + 1) * 128, :])
                    tr.dma('pool', XS[:, :], b[:, :], indirect=('out', slot_i[:, t, 0:1]))
                    tr.dma('pool', XS[:, :], b[:, :], indirect=('out', slot_i[:, t, 1:2]))
                pg = Ring([Q.ps([128, 512], F32, "pg") for _ in range(3)])
                pq = Ring([Q.ps([128, 512], F32, "pq") for _ in range(3)])
                sg = Ring([Q.sb([128, 512], F32, "sg") for _ in range(2)])
                x1 = Ring([Q.sb([128, 512], F32, "x1") for _ in range(3)])
                ro = Ring([Q.sb([128, 512], F32, "ro") for _ in range(3)])
                for cb in range(4):
                    cs_ = slice(cb * 512, (cb + 1) * 512)
                    w = wts[cb]
                    for t in range(NT):
                        x_ = x1.next()
                        tr.dma('act', x_[:, :], XA[t * 128:(t + 1) * 128, cs_])
                        a_ = pg.next()
                        for kc in range(16):
                            tr.I('pe', 'matmul', out=a_[:, :], lhsT=xT[:, kc, t * 128:(t + 1) * 128], rhs=w[:, kc, :],
                                 start=(kc == 0), stop=(kc == 15))
                        b_ = pq.next()
                        for kc in range(2):
                            tr.I('pe', 'matmul', out=b_[:, :], lhsT=pT[:, t, kc, :], rhs=Wpp[:, kc, cs_],
                                 start=(kc == 0), stop=(kc == 1))
                        s_ = sg.next()
                        tr.I('act', 'activation', out=s_[:, :], in_=a_[:, :], func=AF.Sigmoid)
                        tr.I('dve', 'tensor_tensor', out=s_[:, :], in0=s_[:, :], in1=b_[:, :], op=ALU.mult)
                        r_ = ro.next()
                        tr.I('dve', 'scalar_tensor_tensor', out=r_[:, :], in0=x_[:, :], scalar=ALPHA, in1=s_[:, :],
                             op0=ALU.mult, op1=ALU.add)
                        tr.dma('sp', R2[t * 128:(t + 1) * 128, cs_], r_[:, :])
        if stop_after == 'SC':
            return
        with Phase(tr, f"ex{li}") as P:
            stg = Ring([P.sb([128, 4096], F32, "stg") for _ in range(3)])
            wb = [[P.sb([128, 8192], BF16, f"wb{k}") for k in range(3)] for _ in range(2)]
            xs = Ring([P.sb([128, 2, D], BF16, "xs") for _ in range(2)])
            xsT = Ring([P.sb([128, 16, 256], BF16, "xsT") for _ in range(1)])
            tp = Ring([P.ps([128, 1024], BF16, "tp") for _ in range(2)])
            hp = Ring([P.ps([128, 1024], F32, "hp") for _ in range(2)])
            yp = Ring([P.ps([128, 512], F32, "yp") for _ in range(2)])
            hs = Ring([P.sb([128, 512], F32, "hs") for _ in range(1)])
            hb = Ring([P.sb([128, 512], BF16, "hb") for _ in range(2)])
            hT = Ring([P.sb([128, 4, 128], BF16, "hT") for _ in range(2)])
            ysb = Ring([P.sb([128, 1024], F32, "ysb") for _ in range(3)])
            w1v = g['w1'].rearrange("l (e p h c) f -> (l e p h) (c f)", p=128, h=2, c=8)
            w3v = g['w3'].rearrange("l (e p h c) f -> (l e p h) (c f)", p=128, h=2, c=8)
            w2v = g['w2'].rearrange("l (e p h c) n -> (l e p h) (c n)", p=128, h=2, c=2)
            wviews = (w1v, w3v, w2v)
            ceng = ['dve', 'dve', 'act', 'dve', 'dve', 'act']
            wbound = g['wbound_reg']

            def load_w(b, pieces):
                wset = wb[b % 2]
                for q_ in pieces:
                    k, h2 = q_ // 2, q_ % 2
                    s_ = stg.next()
                    tr.dma('pool', s_[:, :], wviews[k], indirect=('in', widx[:, b, h2:h2 + 1]), bounds_check=wbound,
                           oob_is_err=False)
                    dst_ = wset[k][:, h2 * 4096:(h2 + 1) * 4096]
                    ce = ceng[q_]
                    if ce == 'act':
                        tr.I('act', 'activation', out=dst_, in_=s_[:, :], func=AF.Copy)
                    else:
                        tr.I(ce, 'tensor_copy', out=dst_, in_=s_[:, :])

            def load_x(b):
                x_ = xs.next()
                tr.dma('sp', x_[:, :, :], XS[b * SLOTB:(b + 1) * SLOTB, :].rearrange("(h p) d -> p h d", p=128))
                return x_

            def xpose(x_):
                xt = xsT.next()
                for hf in range(2):
                    xv = x_.h[:, hf, :].rearrange("p (j c) -> p c j", c=16)
                    for half in range(2):
                        ps = tp.next()
                        for j in range(8):
                            c = half * 8 + j
                            tr.I('pe', 'transpose', out=ps[:, j * 128:(j + 1) * 128], in_=x_.v(xv[:, c, :]),
                                 identity=identb[:, :])
                        src = ps.v(ps.h[:, :].rearrange("p (j k) -> p j k", k=128))
                        dst = xt[:, half * 8:(half + 1) * 8, hf * 128:(hf + 1) * 128]
                        if half == 0:
                            tr.I('act', 'activation', out=dst, in_=src, func=AF.Copy)
                        else:
                            tr.I('dve', 'tensor_copy', out=dst, in_=src)
                return xt

            def compute_half(b, xt, hf):
                wset = wb[b % 2]
                h_ = hp.next()
                for k in range(2):
                    for c in range(16):
                        tr.I('pe', 'matmul', out=h_[:, k * 512:(k + 1) * 512], lhsT=xt[:, c, hf * 128:(hf + 1) * 128],
                             rhs=wset[k][:, c * 512:(c + 1) * 512], start=(c == 0), stop=(c == 15))
                s1 = hs.next()
                tr.I('act', 'activation', out=s1[:, :], in_=h_[:, 0:512], func=AF.Silu)
                hb_ = hb.next()
                tr.I('dve', 'tensor_tensor', out=hb_[:, :], in0=s1[:, :], in1=h_[:, 512:1024], op=ALU.mult)
                ps = tp.next()
                hv = hb_.h[:, :].rearrange("p (j c) -> p c j", c=4)
                for c in range(4):
                    tr.I('pe', 'transpose', out=ps[:, c * 128:(c + 1) * 128], in_=hb_.v(hv[:, c, :]), identity=identb[:, :])
                ht = hT.next()
                tr.I('act', 'activation', out=ht[:, :, :], in_=ps.v(ps.h[:, 0:512].rearrange("p (c k) -> p c k", k=128)),
                     func=AF.Copy)
                for cp in range(2):
                    y_ = ysb.next()
                    for c2 in range(2):
                        cb = cp * 2 + c2
                        yp_ = yp.next()
                        for c in range(4):
                            tr.I('pe', 'matmul', out=yp_[:, :], lhsT=ht[:, c, :],
                                 rhs=wset[2][:, c * D + cb * 512:c * D + (cb + 1) * 512], start=(c == 0), stop=(c == 3))
                        if c2 == 0:
                            tr.I('dve', 'tensor_copy', out=y_[:, c2 * 512:(c2 + 1) * 512], in_=yp_[:, :])
                        else:
                            tr.I('act', 'activation', out=y_[:, c2 * 512:(c2 + 1) * 512], in_=yp_[:, :], func=AF.Copy)
                    tr.dma('sp', YS[b * SLOTB + hf * 128:b * SLOTB + (hf + 1) * 128, cp * 1024:(cp + 1) * 1024], y_[:, :])

            load_w(0, range(6))
            x_next = load_x(0)
            for b in range(NBLK):
                x_cur = x_next
                if b + 1 < NBLK:
                    x_next = load_x(b + 1)
                xt = xpose(x_cur)
                if b + 1 < NBLK:
                    load_w(b + 1, range(0, 3))
                compute_half(b, xt, 0)
                if b + 1 < NBLK:
                    load_w(b + 1, range(3, 6))
                compute_half(b, xt, 1)
        with Phase(tr, f"cb{li}") as P:
            gbc = P.sb([128, D], F32, "gbc")
            bbc = P.sb([128, D], F32, "bbc")
            tr.dma('sp', gbc[:, :], g['ln2_g'][li, :].partition_broadcast(128))
            tr.dma('sp', bbc[:, :], g['ln2_b'][li, :].partition_broadcast(128))
            y1 = Ring([P.sb([128, D], F32, "y1") for _ in range(3)])
            y2 = Ring([P.sb([128, D], F32, "y2") for _ in range(3)])
            r2 = Ring([P.sb([128, D], F32, "r2") for _ in range(3)])
            def cb_load(t):
                a_, b_, r_ = y1.next(), y2.next(), r2.next()
                tr.dma('pool', a_[:, :], YS[:, :], indirect=('in', slot_i[:, t, 0:1]))
                tr.dma('pool', b_[:, :], YS[:, :], indirect=('in', slot_i[:, t, 1:2]))
                tr.dma('sp', r_[:, :], R2[t * 128:(t + 1) * 128, :])
                return a_, b_, r_

            nxt = cb_load(0)
            for t in range(NT):
                a_, b_, r_ = nxt
                if t + 1 < NT:
                    nxt = cb_load(t + 1)
                tr.I('dve', 'scalar_tensor_tensor', out=r_[:, :], in0=a_[:, :], scalar=gates[:, t, 0:1], in1=r_[:, :],
                     op0=ALU.mult, op1=ALU.add)
                tr.I('dve', 'scalar_tensor_tensor', out=r_[:, :], in0=b_[:, :], scalar=gates[:, t, 1:2], in1=r_[:, :],
                     op0=ALU.mult, op1=ALU.add)
                layer_norm_rows(tr, P, r_, r_, D, gbc, bbc, lnt)
                tr.dma('act', xout[t * 128:(t + 1) * 128, :], r_[:, :])


def make_consts(first_in_batch):
    c = np.zeros((128, 1024), np.float32)
    c[:, 0:128] = np.eye(128, dtype=np.float32)
    t = np.arange(128)[:, None]
    s = np.arange(128)[None, :]
    c[:, 128:256] = (s <= t).astype(np.float32)
    qi = np.arange(128)[:, None]
    kj = np.arange(256)[None, :]
    valid = (kj >= qi) & (kj <= qi + 128)
    m = np.where(valid, 0.0, NEG).astype(np.float32)
    c[:, 256:512] = m
    mh = m.copy()
    if first_in_batch:
        mh[:, 0:128] = NEG
    c[:, 512:768] = mh
    c[:, 768:784] = (500000.0 ** (-np.arange(0, 32, 2, dtype=np.float32) / 32)).astype(np.float32)[None, :]
    c[:, 784:912] = (t < s).astype(np.float32)
    c[:, 912:976] = np.arange(64, dtype=np.float32)[None, :]
    c[:, 976] = np.arange(128, dtype=np.float32)
    return c


def make_hidx(core):
    prev = (core % 4 - 1) % 4

    def row(e):
        e = np.asarray(e)
        out = np.zeros_like(e)
        off = 0
        for (r0, r1) in KV_CHUNKS:
            n_ = r1 - r0
            m = (e >= r0) & (e < r1)
            out = np.where(m, off + prev * n_ + (e - r0), out)
            off += 4 * n_
        return out

    cols = []
    i = np.arange(128)
    cols.append(row(5120 + i))
    cols.append(row(5248 + i))
    for r in range(4):
        cols.append(row(4096 + r + 4 * i))
        cols.append(row(4608 + r + 4 * i))
    for r in range(16):
        cols.append(row(r + 16 * i))
        cols.append(row(2048 + r + 16 * i))
    h = np.zeros((128, 48), np.int32)
    h[:, :len(cols)] = np.stack(cols, 1)
    return h


def core_inputs(core, inputs):
    b, q = core // 4, core % 4
    sl = slice(q * T, (q + 1) * T)
    m = {
        "x": np.ascontiguousarray(inputs["x"][b, sl]),
        "p": np.ascontiguousarray(inputs["p"][:, b, sl]),
        "pos": np.ascontiguousarray(inputs["positions"][b, sl].reshape(NT, 128).T.astype(np.int32)),
        "cst": make_consts(q == 0),
        "hidx": make_hidx(core),
    }
    for k in ("w_in", "w_s", "ln_v_g", "ln_v_b", "w_a", "w_b", "w_o", "ln1_g", "ln1_b", "w_grp", "b_grp",
              "w_rt", "b_rt", "w_pg", "w_pp", "ln2_g", "ln2_b"):
        m[k] = inputs[k]
    m["b_s"] = inputs["b_s"].reshape(-1, 8 * 128)
    m["w1"] = inputs["w1"].reshape(-1, 32 * D, 512)
    m["w3"] = inputs["w3"].reshape(-1, 32 * D, 512)
    m["w2"] = inputs["w2"].reshape(-1, 32 * 512, D)
    return m


_CACHE = {}


def kernel(**inputs):
    inputs = {k: np.asarray(v) for k, v in inputs.items()}
    if "nc" not in _CACHE:
        _CACHE["nc"] = build(nlayers=DEPTH, halo="ag")[0]
    nc = _CACHE["nc"]
    in_maps = [core_inputs(c, inputs) for c in range(NCORES)]
    res = run_bass_kernel_spmd(nc, in_maps, core_ids=list(range(NCORES)))
    out = np.zeros((2, 4 * T, D), np.float32)
    for c in range(NCORES):
        out[c // 4, (c % 4) * T:(c % 4 + 1) * T] = np.asarray(res.results[c]["y"])
    return out
```

```python
import numpy as np
from contextlib import ExitStack
import concourse.bass as bass
import concourse.mybir as mybir
from concourse.bass_utils import run_bass_kernel_spmd

F32 = mybir.dt.float32
BF16 = mybir.dt.bfloat16
I32 = mybir.dt.int32
AF = mybir.ActivationFunctionType
ALU = mybir.AluOpType
AX = mybir.AxisListType

NCORES = 8
DEPTH = 4
T = 2048
NT = 16
D = 2048
PW = 10752
ALPHA = float(8 ** 0.25)
EPS = 1e-5
SCALE = float(128 ** -0.5)
NEG = -30000.0
SLOTB = 256
NBLK = 47
NSLOT = NBLK * SLOTB
KVX_ROWS = 2048 + 2048 + 512 + 512 + 128 + 128
KV_CHUNKS = ((0, 1024), (1024, 2048), (2048, 3072), (3072, 4096), (4096, 5120), (5120, 5376))
TWO_PI = 2.0 * np.pi
C1 = 6.28125
C2 = float(np.float32(TWO_PI - C1))
C3 = float(TWO_PI - C1 - np.float64(np.float32(TWO_PI - C1)))


class _Eng:
    def __init__(self, key, eng, sem):
        self.key, self.eng, self.sem, self.count, self.seen = key, eng, sem, 0, {}


class _Slot:
    def __init__(self, sem):
        self.sem, self.total = sem, 0


class Buf:
    def __init__(self, name):
        self.name, self.w, self.r, self.slot = name, None, {}, None


class V:
    def __init__(self, ap, buf):
        self.ap, self.buf = ap, buf


class Tile:
    def __init__(self, h, buf):
        self.h, self.buf = h, buf

    def __getitem__(self, idx):
        return V(self.h[idx], self.buf)

    def v(self, ap):
        return V(ap, self.buf)


class Tracker:
    def __init__(self, nc, es, nslots=93):
        self.nc = nc
        mk = lambda n: es.enter_context(nc.semaphore(n))
        self.E = {
            'pe': _Eng('pe', nc.tensor, mk('c_pe')),
            'dve': _Eng('dve', nc.vector, mk('c_dve')),
            'act': _Eng('act', nc.scalar, mk('c_act')),
            'pool': _Eng('pool', nc.gpsimd, mk('c_pool')),
            'sp': _Eng('sp', nc.sync, mk('c_sp')),
        }
        self.cc = _Eng('cc', None, mk('c_cc'))
        self.slots = [_Slot(mk(f'd{i}')) for i in range(nslots)]
        self.free = list(self.slots)
        self.bufs = []
        self.ninst = 0

    def buf(self, name):
        b = Buf(name)
        self.bufs.append(b)
        return b

    def release(self, bufs):
        for b in bufs:
            if b.slot is not None:
                self.free.append(b.slot)
                b.slot = None
            if b in self.bufs:
                self.bufs.remove(b)

    def _slot(self, b):
        if b.slot is None:
            b.slot = self.free.pop()
        return b.slot

    def _waits(self, E, reads, writes, skip_slot=None):
        need = {}

        def add(ev):
            if ev is None:
                return
            kind, obj = ev[0], ev[1]
            if kind == 'd':
                sem, val = obj.sem, obj.total
            else:
                if obj is E and E.key == 'pe':
                    return
                sem, val = obj.sem, ev[2]
            k = id(sem)
            if k not in need or need[k][1] < val:
                need[k] = (sem, val)

        for b in reads:
            add(b.w)
        for b in writes:
            if not (skip_slot is not None and b.w is not None and b.w[0] == 'd' and b.w[1] is skip_slot):
                add(b.w)
            for ev in b.r.values():
                add(ev)
        for k, (sem, val) in need.items():
            if E.seen.get(k, 0) >= val:
                continue
            E.eng.wait_ge(sem, val)
            E.seen[k] = val

    def I(self, ek, meth, **kw):
        E = self.E[ek]
        reads, writes, args = [], [], {}
        for k, v in kw.items():
            if isinstance(v, V):
                (writes if k in ('out', 'accum_out') else reads).append(v.buf)
                args[k] = v.ap
            else:
                args[k] = v
        self._waits(E, reads, writes)
        ins = getattr(E.eng, meth)(**args)
        E.count += 1
        ins.then_inc(E.sem, 1)
        ev = ('e', E, E.count)
        for b in reads:
            b.r[E.key] = ev
        for b in writes:
            b.w = ev
            b.r = {}
        self.ninst += 1
        return ins

    def dma(self, qk, out, in_, indirect=None, **kw):
        E = self.E[qk]
        reads, writes = [], []
        o, i = out, in_
        if isinstance(out, V):
            writes.append(out.buf)
            o = out.ap
        if isinstance(in_, V):
            reads.append(in_.buf)
            i = in_.ap
        if indirect is not None:
            reads.append(indirect[1].buf)
        sb = (writes + reads)[0]
        slot = self._slot(sb)
        self._waits(E, reads, writes, skip_slot=slot)
        if indirect is None:
            ins = E.eng.dma_start(out=o, in_=i, **kw)
        elif indirect[0] == 'in':
            ins = E.eng.indirect_dma_start(out=o, out_offset=None, in_=i,
                                           in_offset=bass.IndirectOffsetOnAxis(ap=indirect[1].ap, axis=0), **kw)
        else:
            ins = E.eng.indirect_dma_start(out=o, out_offset=bass.IndirectOffsetOnAxis(ap=indirect[1].ap, axis=0),
                                           in_=i, in_offset=None, **kw)
        slot.total += 16
        ins.then_inc(slot.sem, 16)
        ev = ('d', slot)
        for b in reads:
            b.r[('d', id(slot))] = ev
        for b in writes:
            b.w = ev
            b.r = {}
        self.ninst += 1
        return ins

    def barrier(self):
        for E in self.E.values():
            for F in list(self.E.values()) + [self.cc]:
                if F is E or F.count == 0:
                    continue
                k = id(F.sem)
                if E.seen.get(k, 0) < F.count:
                    E.eng.wait_ge(F.sem, F.count)
                    E.seen[k] = F.count
            for s in self.slots:
                if s.total == 0:
                    continue
                k = id(s.sem)
                if E.seen.get(k, 0) < s.total:
                    E.eng.wait_ge(s.sem, s.total)
                    E.seen[k] = s.total
        for b in self.bufs:
            b.w, b.r = None, {}

    def final_wait(self):
        E = self.E['sp']
        for s in self.slots:
            if s.total and E.seen.get(id(s.sem), 0) < s.total:
                E.eng.wait_ge(s.sem, s.total)
                E.seen[id(s.sem)] = s.total


class Phase:
    def __init__(self, tr, name):
        self.tr, self.nc, self.name = tr, tr.nc, name
        self.es = ExitStack()
        self.bufs = []
        self.n = 0

    def __enter__(self):
        self.es.__enter__()
        return self

    def __exit__(self, *a):
        self.tr.barrier()
        self.tr.release(self.bufs)
        return self.es.__exit__(*a)

    def sb(self, shape, dt, name=None):
        self.n += 1
        nm = f"{self.name}_{name or 's'}{self.n}"
        h = self.es.enter_context(self.nc.sbuf_tensor(nm, list(shape), dt))
        b = self.tr.buf(nm)
        self.bufs.append(b)
        return Tile(h, b)

    def ps(self, shape, dt, name=None):
        self.n += 1
        nm = f"{self.name}_{name or 'p'}{self.n}"
        h = self.es.enter_context(self.nc.psum_tensor(nm, list(shape), dt))
        b = self.tr.buf(nm)
        self.bufs.append(b)
        return Tile(h, b)


class Ring:
    def __init__(self, items):
        self.items, self.i = items, 0

    def next(self):
        x = self.items[self.i % len(self.items)]
        self.i += 1
        return x


def layer_norm_rows(tr, ph, src, dst, W, g_bc, b_bc, tmp):
    nch = W // 512
    st, mv, rs = tmp['st'], tmp['mv'], tmp['rs']
    for c in range(nch):
        tr.I('dve', 'bn_stats', out=st[:, c, :], in_=src[:, c * 512:(c + 1) * 512])
    tr.I('dve', 'bn_aggr', out=mv[:, :], in_=st[:, 0:nch, :])
    tr.I('dve', 'tensor_scalar', out=rs[:, 0:1], in0=mv[:, 1:2], scalar1=EPS, scalar2=None, op0=ALU.add)
    tr.I('pool', 'tensor_tensor', out=rs[:, 1:2], in0=rs[:, 0:1], in1=tmp['nh'][:, 0:1], op=ALU.pow)
    tr.I('dve', 'tensor_scalar', out=dst[:, 0:W], in0=src[:, 0:W], scalar1=mv[:, 0:1], scalar2=rs[:, 1:2],
         op0=ALU.subtract, op1=ALU.mult)
    tr.I('dve', 'tensor_tensor', out=dst[:, 0:W], in0=dst[:, 0:W], in1=g_bc[:, 0:W], op=ALU.mult)
    tr.I('dve', 'tensor_tensor', out=dst[:, 0:W], in0=dst[:, 0:W], in1=b_bc[:, 0:W], op=ALU.add)


def build(nlayers=DEPTH, halo='ag', dbg=(), stop_after=None, wdepth=DEPTH):
    nc = bass.Bass("TRN2", target_bir_lowering=False)
    dt = nc.dram_tensor

    def ein(name, shape, dtype=F32):
        return dt(name, list(shape), dtype, kind="ExternalInput").ap()

    def scr(name, shape, dtype):
        kind = "ExternalOutput" if name in dbg else "Internal"
        return dt(name, list(shape), dtype, kind=kind).ap()

    x_in = ein("x", [T, D])
    p_in = ein("p", [wdepth, T, 256])
    pos_in = ein("pos", [128, NT], I32)
    cst_in = ein("cst", [128, 1024])
    hidx_in = ein("hidx", [128, 48], I32)
    w_in = ein("w_in", [wdepth, D, PW])
    w_s = ein("w_s", [wdepth, 8, 128, 128])
    b_s = ein("b_s", [wdepth, 8 * 128])
    ln_v_g = ein("ln_v_g", [wdepth, 1024])
    ln_v_b = ein("ln_v_b", [wdepth, 1024])
    w_a = ein("w_a", [wdepth, 1024, D])
    w_b = ein("w_b", [wdepth, 512, D])
    w_o = ein("w_o", [wdepth, D, D])
    ln1_g = ein("ln1_g", [wdepth, D])
    ln1_b = ein("ln1_b", [wdepth, D])
    w_grp = ein("w_grp", [wdepth, D, 4])
    b_grp = ein("b_grp", [wdepth, 4])
    w_rt = ein("w_rt", [wdepth, D, 32])
    b_rt = ein("b_rt", [wdepth, 32])
    w1 = ein("w1", [wdepth, 32 * D, 512])
    w3 = ein("w3", [wdepth, 32 * D, 512])
    w2 = ein("w2", [wdepth, 32 * 512, D])
    w_pg = ein("w_pg", [wdepth, D, D])
    w_pp = ein("w_pp", [wdepth, 256, D])
    ln2_g = ein("ln2_g", [wdepth, D])
    ln2_b = ein("ln2_b", [wdepth, D])
    y_out = dt("y", [T, D], F32, kind="ExternalOutput").ap()

    XA = scr("XA", [T, D], F32)
    XB = scr("XB", [T, D], F32)
    QK = scr("QK", [T, 3072], BF16)
    VV = scr("VV", [T, 1536], BF16)
    KVX32 = dt("KVX", [KVX_ROWS, 256], F32)
    KVX = KVX32[:, :].bitcast(BF16)
    if halo == 'ag':
        KVH32 = dt("KVH", [4 * KVX_ROWS, 256], F32)
        KVH = KVH32[:, :].bitcast(BF16)
    else:
        KVH32 = None
        KVH = None
    OG = scr("OG", [3, T, 516], F32)
    GT = scr("GT", [4096, T], BF16)
    MT = scr("MT", [D, T], BF16)
    R2 = scr("R2", [T, D], F32)
    XS = scr("XS", [NSLOT, D], BF16)
    YS = scr("YS", [NSLOT, D], F32)

    with ExitStack() as es:
        tr = Tracker(nc, es)
        with Phase(tr, "g") as G:
            cst = G.sb([128, 1024], F32, "cst")
            tr.dma('sp', cst[:, :], cst_in[:, :])
            ident_f = cst[:, 0:128]
            identb = G.sb([128, 128], BF16, "identb")
            tr.I('dve', 'tensor_copy', out=identb[:, :], in_=cst[:, 0:128])
            ones_f = G.sb([128, 128], F32, "ones")
            tr.I('dve', 'memset', ap=ones_f[:, :], constant=1.0) if False else None
            nc_ones = tr.I('dve', 'tensor_scalar', out=ones_f[:, :], in0=cst[:, 0:128], scalar1=0.0, scalar2=1.0,
                           op0=ALU.mult, op1=ALU.add)
            nh = G.sb([128, 1], F32, "nh")
            tr.I('dve', 'tensor_scalar', out=nh[:, :], in0=cst[:, 0:1], scalar1=0.0, scalar2=-0.5,
                 op0=ALU.mult, op1=ALU.add)
            hidx = G.sb([128, 48], I32, "hidx")
            tr.dma('sp', hidx[:, :], hidx_in[:, :])
            lnt = {'st': G.sb([128, 4, 6], F32, "st"), 'mv': G.sb([128, 2], F32, "mv"),
                   'rs': G.sb([128, 2], F32, "rs"), 'nh': nh}
            cos4 = G.sb([128, NT, 4, 16], F32, "cos4")
            sin4 = G.sb([128, NT, 4, 16], F32, "sin4")
            with Phase(tr, "rt") as P:
                posi = P.sb([128, NT], I32)
                posf = P.sb([128, NT], F32)
                ang = P.sb([128, NT, 16], F32)
                kf = P.sb([128, NT, 16], F32)
                ki = P.sb([128, NT, 16], I32)
                r = P.sb([128, NT, 16], F32)
                t1 = P.sb([128, NT, 16], F32)
                t2 = P.sb([128, NT, 16], F32)
                sn = P.sb([128, NT, 16], F32)
                cs = P.sb([128, NT, 16], F32)
                tr.dma('sp', posi[:, :], pos_in[:, :])
                tr.I('dve', 'tensor_copy', out=posf[:, :], in_=posi[:, :])
                for t in range(NT):
                    tr.I('dve', 'tensor_scalar', out=ang[:, t, :], in0=cst[:, 768:784], scalar1=posf[:, t:t + 1],
                         scalar2=None, op0=ALU.mult)

                def wrap(dst, src):
                    tr.I('dve', 'tensor_scalar', out=t1[:, :, :], in0=src[:, :, :], scalar1=float(np.pi),
                         scalar2=-TWO_PI, op0=ALU.is_gt, op1=ALU.mult)
                    tr.I('dve', 'tensor_scalar', out=t2[:, :, :], in0=src[:, :, :], scalar1=-float(np.pi),
                         scalar2=TWO_PI, op0=ALU.is_lt, op1=ALU.mult)
                    tr.I('dve', 'tensor_tensor', out=t1[:, :, :], in0=t1[:, :, :], in1=t2[:, :, :], op=ALU.add)
                    tr.I('dve', 'tensor_tensor', out=dst[:, :, :], in0=src[:, :, :], in1=t1[:, :, :], op=ALU.add)

                tr.I('dve', 'tensor_scalar', out=kf[:, :, :], in0=ang[:, :, :], scalar1=float(1.0 / TWO_PI),
                     scalar2=None, op0=ALU.mult)
                tr.I('dve', 'tensor_copy', out=ki[:, :, :], in_=kf[:, :, :])
                tr.I('dve', 'tensor_copy', out=kf[:, :, :], in_=ki[:, :, :])
                tr.I('dve', 'scalar_tensor_tensor', out=r[:, :, :], in0=kf[:, :, :], scalar=-C1, in1=ang[:, :, :],
                     op0=ALU.mult, op1=ALU.add)
                tr.I('dve', 'scalar_tensor_tensor', out=r[:, :, :], in0=kf[:, :, :], scalar=-C2, in1=r[:, :, :],
                     op0=ALU.mult, op1=ALU.add)
                tr.I('dve', 'scalar_tensor_tensor', out=r[:, :, :], in0=kf[:, :, :], scalar=-C3, in1=r[:, :, :],
                     op0=ALU.mult, op1=ALU.add)
                wrap(r, r)
                tr.I('act', 'activation', out=sn[:, :, :], in_=r[:, :, :], func=AF.Sin)
                tr.I('dve', 'tensor_scalar', out=r[:, :, :], in0=r[:, :, :], scalar1=float(np.pi / 2), scalar2=None,
                     op0=ALU.add)
                wrap(r, r)
                tr.I('act', 'activation', out=cs[:, :, :], in_=r[:, :, :], func=AF.Sin)
                for h in range(4):
                    tr.I('dve', 'tensor_copy', out=cos4[:, :, h, :], in_=cs[:, :, :])
                    tr.I('dve', 'tensor_copy', out=sin4[:, :, h, :], in_=sn[:, :, :])

            wbound_reg = nc.gpsimd.to_reg(wdepth * 8192 - 1)
            for li in range(nlayers):
                xin = x_in if li == 0 else XB
                xout = y_out if li == nlayers - 1 else XB
                _layer(nc, tr, G, li, xin, xout, locals(), halo, stop_after)
                if stop_after is not None:
                    break
        tr.final_wait()
    return nc, tr


def _transpose_rows(tr, P, src_dram, xT, identb, cast_q='pool'):
    xb = Ring([P.sb([128, D], BF16, "xb") for _ in range(2)])
    tp = Ring([P.ps([128, 1024], BF16, "tp") for _ in range(4)])
    for t in range(NT):
        b = xb.next()
        tr.dma('pool', b[:, :], src_dram[t * 128:(t + 1) * 128, :])
        for half in range(2):
            ps = tp.next()
            for j in range(8):
                c = half * 8 + j
                tr.I('pe', 'transpose', out=ps[:, j * 128:(j + 1) * 128], in_=b[:, c * 128:(c + 1) * 128],
                     identity=identb[:, :])
            eng = 'act' if half == 0 else 'dve'
            src = ps.v(ps.h[:, :].rearrange("p (j k) -> p j k", k=128))
            if eng == 'act':
                tr.I('act', 'activation', out=xT[:, half * 8:(half + 1) * 8, t * 128:(t + 1) * 128], in_=src,
                     func=AF.Copy)
            else:
                tr.I('dve', 'tensor_copy', out=xT[:, half * 8:(half + 1) * 8, t * 128:(t + 1) * 128], in_=src)


def _wload(tr, dst, w2d, r0, nrows_c, c0, ncols, q='pool'):
    src = w2d[r0:r0 + nrows_c * 128, c0:c0 + ncols].rearrange("(c p) n -> p c n", p=128)
    tr.dma(q, dst, src)


def _layer(nc, tr, G, li, xin, xout, g, halo, stop_after):
    cst, identb, ones_f, hidx, lnt = (g['cst'], g['identb'], g['ones_f'], g['hidx'], g['lnt'])
    cos4, sin4 = g['cos4'], g['sin4']
    w_in, QK, VV, GT, KVX, KVH, OG, MT, XA, R2, XS, YS = (g['w_in'], g['QK'], g['VV'], g['GT'], g['KVX'], g['KVH'],
                                                           g['OG'], g['MT'], g['XA'], g['R2'], g['XS'], g['YS'])
    Wi = w_in[li]

    with Phase(tr, f"A{li}") as A:
        uT = A.sb([128, 8, T], BF16, "uT")
        with Phase(tr, f"B{li}") as B:
            xT = B.sb([128, 16, T], BF16, "xT")
            with Phase(tr, f"t{li}") as P:
                _transpose_rows(tr, P, xin, xT, identb)
            vln = B.sb([128, NT, 1024], BF16, "vln")
            with Phase(tr, f"p{li}") as P:
                wt = Ring([P.sb([128, 16, 512], BF16, "wt") for _ in range(2)])
                acc = Ring([P.ps([128, 512], F32, "acc") for _ in range(4)])
                ot = Ring([P.sb([128, 512], BF16, "ot") for _ in range(3)])
                rt = Ring([P.sb([128, 4, 4, 16], F32, "rt") for _ in range(2)])
                for j in (range(7, 13) if stop_after == 'KV' else range(4, 13)):
                    w = wt.next()
                    _wload(tr, w[:, :, :], Wi, 0, 16, j * 512, 512)
                    for t in range(NT):
                        ps = acc.next()
                        for kc in range(16):
                            tr.I('pe', 'matmul', out=ps[:, :], lhsT=xT[:, kc, t * 128:(t + 1) * 128], rhs=w[:, kc, :],
                                 start=(kc == 0), stop=(kc == 15))
                        o = ot.next()
                        if j < 10:
                            p3 = ps.v(ps.h[:, :].rearrange("p (h d) -> p h d", h=4))
                            o3 = o.v(o.h[:, :].rearrange("p (h d) -> p h d", h=4))
                            x1 = ps.v(p3.ap[:, :, 0:16])
                            x2 = ps.v(p3.ap[:, :, 16:32])
                            r_ = rt.next()
                            tr.I('dve', 'tensor_tensor', out=r_[:, 0, :, :], in0=x1, in1=cos4[:, t, :, :], op=ALU.mult)
                            tr.I('dve', 'tensor_tensor', out=r_[:, 1, :, :], in0=x2, in1=sin4[:, t, :, :], op=ALU.mult)
                            tr.I('dve', 'tensor_tensor', out=r_[:, 2, :, :], in0=x2, in1=cos4[:, t, :, :], op=ALU.mult)
                            tr.I('dve', 'tensor_tensor', out=r_[:, 3, :, :], in0=x1, in1=sin4[:, t, :, :], op=ALU.mult)
                            tr.I('dve', 'tensor_tensor', out=o.v(o3.ap[:, :, 0:16]), in0=r_[:, 0, :, :],
                                 in1=r_[:, 1, :, :], op=ALU.subtract)
                            tr.I('dve', 'tensor_tensor', out=o.v(o3.ap[:, :, 16:32]), in0=r_[:, 2, :, :],
                                 in1=r_[:, 3, :, :], op=ALU.add)
                            tr.I('act', 'activation', out=o.v(o3.ap[:, :, 32:128]), in_=ps.v(p3.ap[:, :, 32:128]),
                                 func=AF.Copy)
                            tr.dma('sp', QK[t * 128:(t + 1) * 128, (j - 4) * 512:(j - 3) * 512], o[:, :])
                        else:
                            tr.I('act', 'activation', out=o[:, :], in_=ps[:, :], func=AF.Copy)
                            tr.dma('sp', VV[t * 128:(t + 1) * 128, (j - 10) * 512:(j - 9) * 512], o[:, :])
                tr.barrier()
                dummy = P.sb([128, 1], F32, "dummy")
                tr.dma('sp', V(KVX[0:2048, :], dummy.buf), QK[:, 1536 + 1024:1536 + 1536])
                tr.dma('sp', V(KVX[2048:4096, :], dummy.buf), VV[:, 1024:1536])
                tr.dma('sp', V(KVX[4096:4608, :], dummy.buf), QK[T - 512:T, 1536 + 512:1536 + 1024])
                tr.dma('sp', V(KVX[4608:5120, :], dummy.buf), VV[T - 512:T, 512:1024])
                tr.dma('sp', V(KVX[5120:5248, :], dummy.buf), QK[T - 128:T, 1536:1536 + 512])
                tr.dma('sp', V(KVX[5248:5376, :], dummy.buf), VV[T - 128:T, 0:512])
                if halo == 'ag':
                    dslot = dummy.buf.slot
                    EP = tr.E['pool']
                    EP.eng.wait_ge(dslot.sem, dslot.total)
                    EP.seen[id(dslot.sem)] = dslot.total
                    off = 0
                    for (r0, r1) in KV_CHUNKS:
                        n_ = r1 - r0
                        ins = nc.gpsimd.collective_compute("AllGather", ALU.bypass, replica_groups=[[0, 1, 2, 3], [4, 5, 6, 7]],
                                                           ins=[g['KVX32'][r0:r1, :]], outs=[g['KVH32'][off:off + 4 * n_, :]])
                        ins.then_inc(tr.cc.sem)
                        tr.cc.count += 1
                        off += 4 * n_
                for j in (() if stop_after == 'KV' else (0, 1, 13, 14, 15, 16, 17, 18, 19, 20)):
                    w = wt.next()
                    _wload(tr, w[:, :, :], Wi, 0, 16, j * 512, 512)
                    for fc in range(4):
                        for tb in range(4):
                            ps = acc.next()
                            for kc in range(16):
                                tr.I('pe', 'matmul', out=ps[:, :], lhsT=w[:, kc, fc * 128:(fc + 1) * 128],
                                     rhs=xT[:, kc, tb * 512:(tb + 1) * 512], start=(kc == 0), stop=(kc == 15))
                            if j < 2:
                                tr.I('act', 'activation', out=uT[:, j * 4 + fc, tb * 512:(tb + 1) * 512], in_=ps[:, :],
                                     func=AF.Gelu_apprx_tanh)
                            else:
                                o = ot.next()
                                tr.I('act', 'activation', out=o[:, :], in_=ps[:, :], func=AF.Sigmoid)
                                row = ((j - 13) * 4 + fc) * 128
                                tr.dma('sp', GT[row:row + 128, tb * 512:(tb + 1) * 512], o[:, :])
            with Phase(tr, f"v{li}") as P:
              if stop_after != 'KV':
                    wv = P.sb([128, 16, 1024], BF16, "wv")
                    _wload(tr, wv[:, :, 0:512], Wi, 0, 16, 1024, 512)
                    _wload(tr, wv[:, :, 512:1024], Wi, 0, 16, 1536, 512)
                    gbc = P.sb([128, 1024], F32, "gbc")
                    bbc = P.sb([128, 1024], F32, "bbc")
                    tr.dma('sp', gbc[:, :], g['ln_v_g'][li, :].partition_broadcast(128))
                    tr.dma('sp', bbc[:, :], g['ln_v_b'][li, :].partition_broadcast(128))
                    acc2 = Ring([P.ps([128, 1024], F32, "acc2") for _ in range(2)])
                    vg = Ring([P.sb([128, 1024], F32, "vg") for _ in range(2)])
                    vn = Ring([P.sb([128, 1024], F32, "vn") for _ in range(2)])
                    for t in range(NT):
                        ps = acc2.next()
                        for hf in range(2):
                            for kc in range(16):
                                tr.I('pe', 'matmul', out=ps[:, hf * 512:(hf + 1) * 512], lhsT=xT[:, kc, t * 128:(t + 1) * 128],
                                     rhs=wv[:, kc, hf * 512:(hf + 1) * 512], start=(kc == 0), stop=(kc == 15))
                        v_ = vg.next()
                        tr.I('act', 'activation', out=v_[:, :], in_=ps[:, :], func=AF.Gelu_apprx_tanh)
                        n_ = vn.next()
                        layer_norm_rows(tr, P, v_, n_, 1024, gbc, bbc, lnt)
                        tr.I('act', 'activation', out=vln[:, t, :], in_=n_[:, :], func=AF.Copy)
            with Phase(tr, f"s{li}") as P:
              if stop_after != 'KV':
                    wsf = P.sb([128, 8, 128], F32, "wsf")
                    tr.dma('sp', wsf[:, :, :], g['w_s'][li].rearrange("g t s -> t g s"))
                    wsb = P.sb([128, 8, 128], BF16, "wsb")
                    for gg in range(8):
                        tr.I('dve', 'tensor_tensor', out=wsb[:, gg, :], in0=wsf[:, gg, :], in1=cst[:, 128:256], op=ALU.mult)
                    wsT = P.sb([128, 8, 128], BF16, "wsT")
                    tps = P.ps([128, 1024], BF16, "tps")
                    for gg in range(8):
                        tr.I('pe', 'transpose', out=tps[:, gg * 128:(gg + 1) * 128], in_=wsb[:, gg, :], identity=identb[:, :])
                    tr.I('dve', 'tensor_copy', out=wsT[:, :, :], in_=tps.v(tps.h[:, :].rearrange("p (g k) -> p g k", k=128)))
                    bsb = P.sb([128, 8, 128], F32, "bsb")
                    tr.dma('sp', bsb[:, :, :], g['b_s'][li, :].partition_broadcast(128).rearrange("p (g t) -> p g t", t=128))
                    zps = Ring([P.ps([128, 1024], F32, "zps") for _ in range(2)])
                    zt = Ring([P.sb([128, 8, 128], F32, "zt") for _ in range(2)])
                    for n in range(NT):
                        ps = zps.next()
                        for gg in range(8):
                            tr.I('pe', 'matmul', out=ps[:, gg * 128:(gg + 1) * 128], lhsT=vln[:, n, gg * 128:(gg + 1) * 128],
                                 rhs=wsT[:, gg, :], start=True, stop=True)
                        z = zt.next()
                        tr.I('dve', 'tensor_tensor', out=z[:, :, :], in0=ps.v(ps.h[:, :].rearrange("p (g t) -> p g t", t=128)),
                             in1=bsb[:, :, :], op=ALU.add)
                        tr.I('dve', 'tensor_tensor', out=uT[:, :, n * 128:(n + 1) * 128], in0=z[:, :, :],
                             in1=uT[:, :, n * 128:(n + 1) * 128], op=ALU.mult)
        if stop_after == 'A':
            with Phase(tr, "dbgA") as P:
                tr.dma('sp', g['XA'][0:128, :].rearrange("p (c t) -> p c t", c=1)[:, 0, 0:T] if False else
                       g['MT'][0:1024, :].rearrange("(c p) t -> p c t", p=128), uT[:, :, :])
            return
        aT = uT

        hsrc = KVH if KVH is not None else None
        if stop_after == 'KV':
            return

        with Phase(tr, f"at{li}") as P:
            qkv = Ring([P.sb([128, 5, 512], BF16, "qkv") for _ in range(4)])
            tps = Ring([P.ps([128, 2048], BF16, "tps") for _ in range(1)])
            qkT = Ring([P.sb([128, 1536], BF16, "qkT") for _ in range(2)])
            sps = Ring([P.ps([128, 1024], F32, "sps") for _ in range(1)])
            ssb = Ring([P.sb([128, 4, 256], F32, "ssb") for _ in range(2)])
            psb = Ring([P.sb([128, 4, 256], BF16, "psb") for _ in range(2)])
            ptp = Ring([P.ps([128, 1024], BF16, "ptp") for _ in range(2)])
            pT = Ring([P.sb([128, 8, 128], BF16, "pT") for _ in range(2)])
            ops_ = Ring([P.ps([128, 512], F32, "ops") for _ in range(2)])
            osb = Ring([P.sb([128, 516], F32, "osb") for _ in range(2)])
            sm = Ring([P.sb([128, 16], F32, "sm") for _ in range(2)])
            mask4 = P.sb([128, 4, 256], F32, "mask4")
            maskh4 = P.sb([128, 4, 256], F32, "maskh4")
            for h in range(4):
                tr.I('dve', 'tensor_copy', out=mask4[:, h, :], in_=cst[:, 256:512])
                tr.I('dve', 'tensor_copy', out=maskh4[:, h, :], in_=cst[:, 512:768])
            blocks = []
            hcol = 0
            for gi, dil in enumerate((1, 4, 16)):
                nb = 16 // dil
                for r in range(dil):
                    for n in range(nb):
                        hc = None
                        if n == 0:
                            hc = hcol
                            hcol += 2
                        blocks.append((gi, dil, r, n, hc))

            def loads(blk):
                gi, dil, r, n, hc = blk
                qv = QK.rearrange("(n i r) c -> r n i c", r=dil, i=128)
                vv = VV.rearrange("(n i r) c -> r n i c", r=dil, i=128)
                qc, kc_, vc = gi * 512, 1536 + gi * 512, gi * 512
                b = qkv.next()
                war = dict(b.buf.r)
                tr.dma('sp', b[:, 0, :], qv[r, n, :, qc:qc + 512])
                tr.dma('sp', b[:, 2, :], qv[r, n, :, kc_:kc_ + 512])
                tr.dma('sp', b[:, 4, :], vv[r, n, :, vc:vc + 512])
                first = (n == 0)
                if not first:
                    tr.dma('sp', b[:, 1, :], qv[r, n - 1, :, kc_:kc_ + 512])
                    tr.dma('sp', b[:, 3, :], vv[r, n - 1, :, vc:vc + 512])
                elif hsrc is not None:
                    b.buf.r = dict(war)
                    tr.dma('pool', b[:, 1, :], hsrc[:, :], indirect=('in', hidx[:, hc:hc + 1]))
                    tr.dma('pool', b[:, 3, :], hsrc[:, :], indirect=('in', hidx[:, hc + 1:hc + 2]))
                else:
                    tr.dma('sp', b[:, 1, :], qv[r, n, :, kc_:kc_ + 512])
                    tr.dma('sp', b[:, 3, :], vv[r, n, :, vc:vc + 512])
                return b

            def stage_a(blk, b):
                gi, dil, r, n, hc = blk
                first = (n == 0)
                tp = tps.next()
                for h in range(4):
                    tr.I('pe', 'transpose', out=tp[:, h * 128:(h + 1) * 128], in_=b[:, 0, h * 128:(h + 1) * 128],
                         identity=identb[:, :])
                    for hf in range(2):
                        o0 = 512 + h * 256 + hf * 128
                        tr.I('pe', 'transpose', out=tp[:, o0:o0 + 128], in_=b[:, 1 + hf, h * 128:(h + 1) * 128],
                             identity=identb[:, :])
                qt = qkT.next()
                tr.I('dve', 'tensor_copy', out=qt[:, :], in_=tp[:, 0:1536])
                sp_ = sps.next()
                for h in range(4):
                    tr.I('pe', 'matmul', out=sp_[:, h * 256:(h + 1) * 256], lhsT=qt[:, h * 128:(h + 1) * 128],
                         rhs=qt[:, 512 + h * 256:512 + (h + 1) * 256], start=True, stop=True)
                s_ = ssb.next()
                mk = maskh4 if first else mask4
                tr.I('dve', 'scalar_tensor_tensor', out=s_[:, :, :],
                     in0=sp_.v(sp_.h[:, :].rearrange("p (h k) -> p h k", k=256)), scalar=SCALE, in1=mk[:, :, :],
                     op0=ALU.mult, op1=ALU.add)
                m_ = sm.next()
                tr.I('dve', 'tensor_reduce', out=m_[:, 0:4], in_=s_[:, :, :], axis=AX.X, op=ALU.max, negate=True)
                p_ = psb.next()
                for h in range(4):
                    tr.I('act', 'activation', out=p_[:, h, :], in_=s_[:, h, :], func=AF.Exp, bias=m_[:, h:h + 1],
                         scale=1.0, accum_out=m_[:, 4 + h:5 + h])
                return (b, m_, p_)

            def stage_b(blk, st):
                gi, dil, r, n, hc = blk
                b, m_, p_ = st
                og = OG[gi].rearrange("(n i r) c -> r n i c", r=dil, i=128)
                pp = ptp.next()
                for h in range(4):
                    for hf in range(2):
                        j = h * 2 + hf
                        tr.I('pe', 'transpose', out=pp[:, j * 128:(j + 1) * 128],
                             in_=p_[:, h, hf * 128:(hf + 1) * 128], identity=identb[:, :])
                pt = pT.next()
                tr.I('dve', 'tensor_copy', out=pt[:, :, :], in_=pp.v(pp.h[:, :].rearrange("p (j k) -> p j k", k=128)))
                op_ = ops_.next()
                for h in range(4):
                    for hf in range(2):
                        tr.I('pe', 'matmul', out=op_[:, h * 128:(h + 1) * 128], lhsT=pt[:, h * 2 + hf, :],
                             rhs=b[:, 3 + hf, h * 128:(h + 1) * 128], start=(hf == 0), stop=(hf == 1))
                o_ = osb.next()
                tr.I('dve', 'reciprocal', out=m_[:, 8:12], in_=m_[:, 4:8])
                for h in range(4):
                    tr.I('act', 'activation', out=o_[:, h * 128:(h + 1) * 128], in_=op_[:, h * 128:(h + 1) * 128],
                         func=AF.Copy, scale=m_[:, 8 + h:9 + h])
                tr.I('act', 'activation', out=m_[:, 12:16], in_=m_[:, 4:8], func=AF.Ln)
                tr.I('dve', 'tensor_tensor', out=o_[:, 512:516], in0=m_[:, 12:16], in1=m_[:, 0:4], op=ALU.subtract)
                tr.dma('sp', og[r, n, :, :], o_[:, :])

            nb_ = len(blocks)
            lb = {0: loads(blocks[0]), 1: loads(blocks[1])}
            prev = None
            for i, blk in enumerate(blocks):
                if i + 2 < nb_:
                    lb[i + 2] = loads(blocks[i + 2])
                st = stage_a(blk, lb.pop(i))
                if prev is not None:
                    stage_b(*prev)
                prev = (blk, st)
            stage_b(*prev)
        if stop_after == 'AT':
            return

        with Phase(tr, f"m{li}") as P:
            bT = P.sb([128, 4, T], BF16, "bT")
            with Phase(tr, f"mg{li}") as Q:
                ogt = Ring([Q.sb([128, 3, 516], F32, "ogt") for _ in range(2)])
                l3 = Ring([Q.sb([128, 8, 4], F32, "l3") for _ in range(2)])
                mg = Ring([Q.sb([128, 512], F32, "mg") for _ in range(2)])
                mgb = Ring([Q.sb([128, 512], BF16, "mgb") for _ in range(2)])
                tp = Ring([Q.ps([128, 512], BF16, "tp") for _ in range(2)])
                ogv = OG.rearrange("g t c -> t g c")
                for t in range(NT):
                    o = ogt.next()
                    tr.dma('sp', o[:, :, :], ogv[t * 128:(t + 1) * 128, :, :])
                    l = l3.next()
                    tr.I('dve', 'tensor_tensor', out=l[:, 3, :], in0=o[:, 0, 512:516], in1=o[:, 1, 512:516], op=ALU.max)
                    tr.I('dve', 'tensor_tensor', out=l[:, 3, :], in0=l[:, 3, :], in1=o[:, 2, 512:516], op=ALU.max)
                    for gi in range(3):
                        tr.I('dve', 'tensor_tensor', out=l[:, gi, :], in0=o[:, gi, 512:516], in1=l[:, 3, :], op=ALU.subtract)
                    tr.I('act', 'activation', out=l[:, 0:3, :], in_=l[:, 0:3, :], func=AF.Exp)
                    tr.I('dve', 'tensor_tensor', out=l[:, 4, :], in0=l[:, 0, :], in1=l[:, 1, :], op=ALU.add)
                    tr.I('dve', 'tensor_tensor', out=l[:, 4, :], in0=l[:, 4, :], in1=l[:, 2, :], op=ALU.add)
                    tr.I('dve', 'reciprocal', out=l[:, 4, :], in_=l[:, 4, :])
                    for gi in range(3):
                        tr.I('dve', 'tensor_tensor', out=l[:, 5 + gi, :], in0=l[:, gi, :], in1=l[:, 4, :], op=ALU.mult)
                    m = mg.next()
                    for h in range(4):
                        hs = slice(h * 128, (h + 1) * 128)
                        tr.I('dve', 'tensor_scalar', out=m[:, hs], in0=o[:, 0, hs], scalar1=l[:, 5, h:h + 1], scalar2=None,
                             op0=ALU.mult)
                        for gi in (1, 2):
                            tr.I('dve', 'scalar_tensor_tensor', out=m[:, hs], in0=o[:, gi, hs], scalar=l[:, 5 + gi, h:h + 1],
                                 in1=m[:, hs], op0=ALU.mult, op1=ALU.add)
                    mb = mgb.next()
                    tr.I('act', 'activation', out=mb[:, :], in_=m[:, :], func=AF.Copy)
                    ps = tp.next()
                    for h in range(4):
                        tr.I('pe', 'transpose', out=ps[:, h * 128:(h + 1) * 128], in_=mb[:, h * 128:(h + 1) * 128],
                             identity=identb[:, :])
                    tr.I('dve', 'tensor_copy', out=bT[:, :, t * 128:(t + 1) * 128],
                         in_=ps.v(ps.h[:, :].rearrange("p (j k) -> p j k", k=128)))
            if stop_after == 'MG':
                tr.dma('sp', MT[0:512, :].rearrange("(c p) t -> p c t", p=128), bT[:, :, :])
                return
            with Phase(tr, f"ma{li}") as Q:
                Wa = Q.sb([128, 8, D], BF16, "Wa")
                Wb = Q.sb([128, 4, D], BF16, "Wb")
                for c4 in range(4):
                    _wload(tr, Wa[:, :, c4 * 512:(c4 + 1) * 512], g['w_a'][li], 0, 8, c4 * 512, 512)
                    _wload(tr, Wb[:, :, c4 * 512:(c4 + 1) * 512], g['w_b'][li], 0, 4, c4 * 512, 512)
                gts = Ring([Q.sb([128, 2, 512], BF16, "gts") for _ in range(3)])
                pa = Ring([Q.ps([128, 512], F32, "pa") for _ in range(2)])
                pb = Ring([Q.ps([128, 512], F32, "pb") for _ in range(2)])
                tm = Ring([Q.sb([128, 2, 512], F32, "tm") for _ in range(2)])
                mo = Ring([Q.sb([128, 512], BF16, "mo") for _ in range(3)])
                for tb in range(4):
                    ts = slice(tb * 512, (tb + 1) * 512)
                    for fc in range(16):
                        gt_ = gts.next()
                        tr.dma('act', gt_[:, 0, :], GT[fc * 128:(fc + 1) * 128, ts])
                        tr.dma('act', gt_[:, 1, :], GT[2048 + fc * 128:2048 + (fc + 1) * 128, ts])
                        a_ = pa.next()
                        for kc in range(8):
                            tr.I('pe', 'matmul', out=a_[:, :], lhsT=Wa[:, kc, fc * 128:(fc + 1) * 128], rhs=aT[:, kc, ts],
                                 start=(kc == 0), stop=(kc == 7))
                        b_ = pb.next()
                        for kc in range(4):
                            tr.I('pe', 'matmul', out=b_[:, :], lhsT=Wb[:, kc, fc * 128:(fc + 1) * 128], rhs=bT[:, kc, ts],
                                 start=(kc == 0), stop=(kc == 3))
                        t_ = tm.next()
                        tr.I('dve', 'tensor_tensor', out=t_[:, 0, :], in0=a_[:, :], in1=gt_[:, 0, :], op=ALU.mult)
                        tr.I('dve', 'tensor_tensor', out=t_[:, 1, :], in0=b_[:, :], in1=gt_[:, 1, :], op=ALU.mult)
                        m_ = mo.next()
                        tr.I('pool', 'tensor_tensor', out=m_[:, :], in0=t_[:, 0, :], in1=t_[:, 1, :], op=ALU.add)
                        tr.dma('sp', MT[fc * 128:(fc + 1) * 128, ts], m_[:, :])
    if stop_after == 'MA':
        return

    with Phase(tr, f"o{li}") as P:
        Wo = P.sb([128, 16, D], BF16, "Wo")
        for c4 in range(4):
            _wload(tr, Wo[:, :, c4 * 512:(c4 + 1) * 512], g['w_o'][li], 0, 16, c4 * 512, 512)
        gbc = P.sb([128, D], F32, "gbc")
        bbc = P.sb([128, D], F32, "bbc")
        tr.dma('sp', gbc[:, :], g['ln1_g'][li, :].partition_broadcast(128))
        tr.dma('sp', bbc[:, :], g['ln1_b'][li, :].partition_broadcast(128))
        mtb = Ring([P.sb([128, 16, 512], BF16, "mtb") for _ in range(2)])
        xt_ = Ring([P.sb([128, D], F32, "xt") for _ in range(2)])
        rr = Ring([P.sb([128, D], F32, "rr") for _ in range(2)])
        acc = Ring([P.ps([128, 512], F32, "acc") for _ in range(4)])
        mtv = MT.rearrange("(c p) t -> p c t", p=128)
        mbs = {}
        for tb in range(2):
            mbs[tb] = mtb.next()
            tr.dma('act', mbs[tb][:, :, :], mtv[:, :, tb * 512:(tb + 1) * 512])
        for tb in range(4):
            mb = mbs.pop(tb)
            for tt in range(4):
                t = tb * 4 + tt
                x_ = xt_.next()
                tr.dma('sp', x_[:, :], xin[t * 128:(t + 1) * 128, :])
                r_ = rr.next()
                for cb in range(4):
                    ps = acc.next()
                    for kc in range(16):
                        tr.I('pe', 'matmul', out=ps[:, :], lhsT=mb[:, kc, tt * 128:(tt + 1) * 128],
                             rhs=Wo[:, kc, cb * 512:(cb + 1) * 512], start=(kc == 0), stop=(kc == 15))
                    tr.I('dve', 'scalar_tensor_tensor', out=r_[:, cb * 512:(cb + 1) * 512], in0=x_[:, cb * 512:(cb + 1) * 512],
                         scalar=ALPHA, in1=ps[:, :], op0=ALU.mult, op1=ALU.add)
                layer_norm_rows(tr, P, r_, r_, D, gbc, bbc, lnt)
                tr.dma('pool', XA[t * 128:(t + 1) * 128, :], r_[:, :])
            if tb + 2 < 4:
                mbs[tb + 2] = mtb.next()
                tr.dma('act', mbs[tb + 2][:, :, :], mtv[:, :, (tb + 2) * 512:(tb + 3) * 512])
    if stop_after == 'O':
        return

    with Phase(tr, f"r{li}") as R:
        sel = R.sb([128, NT, 32], F32, "sel")
        oh1 = R.sb([128, NT, 32], F32, "oh1")
        gates = R.sb([128, NT, 2], F32, "gates")
        slot_i = R.sb([128, NT, 2], I32, "slot_i")
        widx = R.sb([128, 64, 2], I32, "widx")
        with Phase(tr, f"rp{li}") as P:
            xT = P.sb([128, 16, T], BF16, "xT1")
            with Phase(tr, f"rt{li}") as Q:
                _transpose_rows(tr, Q, XA, xT, identb)
            Wr = P.sb([128, 16, 36], BF16, "Wr")
            tr.dma('pool', Wr[:, :, 0:4], g['w_grp'][li].rearrange("(c p) n -> p c n", p=128))
            tr.dma('pool', Wr[:, :, 4:36], g['w_rt'][li].rearrange("(c p) n -> p c n", p=128))
            brb = P.sb([128, 36], F32, "brb")
            tr.dma('sp', brb[:, 0:4], g['b_grp'][li, :].partition_broadcast(128))
            tr.dma('sp', brb[:, 4:36], g['b_rt'][li, :].partition_broadcast(128))
            Wpp = P.sb([128, 2, D], BF16, "Wpp")
            for c4 in range(4):
                _wload(tr, Wpp[:, :, c4 * 512:(c4 + 1) * 512], g['w_pp'][li], 0, 2, c4 * 512, 512)
            pT = P.sb([128, NT, 2, 128], BF16, "pT")
            with Phase(tr, f"rr{li}") as Q:
                psr = Ring([Q.ps([128, 64], F32, "psr") for _ in range(2)])
                lg = Ring([Q.sb([128, 36], F32, "lg") for _ in range(2)])
                msk = Ring([Q.sb([128, 32], F32, "msk") for _ in range(2)])
                sc = Ring([Q.sb([128, 32], F32, "sc") for _ in range(2)])
                pb_ = Ring([Q.sb([128, 256], BF16, "pb") for _ in range(2)])
                ptp = Ring([Q.ps([128, 256], BF16, "ptp") for _ in range(2)])
                for t in range(NT):
                    ps = psr.next()
                    for kc in range(16):
                        tr.I('pe', 'matmul', out=ps[:, 0:36], lhsT=xT[:, kc, t * 128:(t + 1) * 128], rhs=Wr[:, kc, :],
                             start=(kc == 0), stop=(kc == 15))
                    l = lg.next()
                    tr.I('dve', 'tensor_tensor', out=l[:, :], in0=ps[:, 0:36], in1=brb[:, :], op=ALU.add)
                    s = sc.next()
                    tr.I('dve', 'tensor_reduce', out=s[:, 0:1], in_=l[:, 0:4], axis=AX.X, op=ALU.max)
                    tr.I('dve', 'tensor_scalar', out=s[:, 1:2], in0=s[:, 0:1], scalar1=-1.0, scalar2=None, op0=ALU.mult)
                    tr.I('act', 'activation', out=s[:, 4:8], in_=l[:, 0:4], func=AF.Exp, bias=s[:, 1:2], scale=1.0,
                         accum_out=s[:, 2:3])
                    tr.I('dve', 'reciprocal', out=s[:, 3:4], in_=s[:, 2:3])
                    tr.I('dve', 'tensor_scalar', out=s[:, 8:12], in0=l[:, 0:4], scalar1=s[:, 0:1], scalar2=None,
                         op0=ALU.is_ge)
                    tr.I('dve', 'tensor_scalar', out=s[:, 8:12], in0=s[:, 8:12], scalar1=-1.0, scalar2=1.0e9,
                         op0=ALU.add, op1=ALU.mult)
                    m = msk.next()
                    for gg in range(4):
                        tr.I('dve', 'tensor_scalar', out=m[:, gg * 8:(gg + 1) * 8], in0=l[:, 4 + gg * 8:12 + gg * 8],
                             scalar1=s[:, 8 + gg:9 + gg], scalar2=None, op0=ALU.add)
                    tr.I('dve', 'max', out=s[:, 12:20], in_=m[:, :])
                    tr.I('dve', 'tensor_scalar', out=sel[:, t, :], in0=m[:, :], scalar1=s[:, 13:14], scalar2=None,
                         op0=ALU.is_ge)
                    tr.I('dve', 'tensor_scalar', out=oh1[:, t, :], in0=m[:, :], scalar1=s[:, 12:13], scalar2=None,
                         op0=ALU.is_ge)
                    tr.I('dve', 'tensor_tensor', out=s[:, 20:21], in0=s[:, 13:14], in1=s[:, 12:13], op=ALU.subtract)
                    tr.I('act', 'activation', out=s[:, 20:21], in_=s[:, 20:21], func=AF.Exp)
                    tr.I('dve', 'tensor_scalar', out=s[:, 21:22], in0=s[:, 20:21], scalar1=1.0, scalar2=None, op0=ALU.add)
                    tr.I('dve', 'reciprocal', out=s[:, 22:23], in_=s[:, 21:22])
                    tr.I('dve', 'tensor_tensor', out=gates[:, t, 0:1], in0=s[:, 22:23], in1=s[:, 3:4], op=ALU.mult)
                    tr.I('dve', 'tensor_tensor', out=gates[:, t, 1:2], in0=gates[:, t, 0:1], in1=s[:, 20:21], op=ALU.mult)
                    pb = pb_.next()
                    tr.dma('pool', pb[:, :], g['p_in'][li, t * 128:(t + 1) * 128, :])
                    pp = ptp.next()
                    for c in range(2):
                        tr.I('pe', 'transpose', out=pp[:, c * 128:(c + 1) * 128], in_=pb[:, c * 128:(c + 1) * 128],
                             identity=identb[:, :])
                    tr.I('act', 'activation', out=pT[:, t, :, :], in_=pp.v(pp.h[:, :].rearrange("p (c k) -> p c k", k=128)),
                         func=AF.Copy)
            with Phase(tr, f"rs{li}") as P:
                U = cst[:, 784:912]
                prk = P.ps([128, NT, 32], F32, "prk")
                pct = P.ps([128, 32], F32, "pct")
                for t in range(NT):
                    tr.I('pe', 'matmul', out=prk[:, t, :], lhsT=U, rhs=sel[:, t, :], start=True, stop=(t == 0))
                    for t2 in range(t):
                        tr.I('pe', 'matmul', out=prk[:, t, :], lhsT=ones_f[:, :], rhs=sel[:, t2, :], start=False,
                             stop=(t2 == t - 1))
                for t in range(NT):
                    tr.I('pe', 'matmul', out=pct[:, :], lhsT=ones_f[:, :], rhs=sel[:, t, :], start=(t == 0), stop=(t == NT - 1))
                cnt = P.sb([128, 32], F32, "cnt")
                nb_ = P.sb([128, 32], F32, "nb")
                cum = P.sb([128, 32], F32, "cum")
                base = P.sb([128, 32], F32, "base")
                on32 = P.sb([128, 32], F32, "on32")
                tr.I('dve', 'tensor_copy', out=cnt[:, :], in_=pct[:, :])
                tr.I('dve', 'tensor_copy', out=on32[:, :], in_=ones_f[:, 0:32])
                tr.I('dve', 'tensor_scalar', out=nb_[:, :], in0=cnt[:, :], scalar1=0.0, scalar2=None, op0=ALU.is_gt)
                for j in range(1, 8):
                    tr.I('dve', 'scalar_tensor_tensor', out=nb_[:, :], in0=cnt[:, :], scalar=float(SLOTB * j), in1=nb_[:, :],
                         op0=ALU.is_gt, op1=ALU.add)
                tr.I('dve', 'tensor_tensor_scan', out=cum[:, :], data0=on32[:, :], data1=nb_[:, :], initial=0.0,
                     op0=ALU.mult, op1=ALU.add)
                tr.I('dve', 'tensor_tensor', out=base[:, :], in0=cum[:, :], in1=nb_[:, :], op=ALU.subtract)
                tr.I('dve', 'tensor_scalar', out=base[:, :], in0=base[:, :], scalar1=float(SLOTB), scalar2=None, op0=ALU.mult)
                slot = P.sb([128, NT, 32], F32, "slot")
                for t in range(NT):
                    tr.I('dve', 'tensor_tensor', out=slot[:, t, :], in0=prk[:, t, :], in1=base[:, :], op=ALU.add)
                pr = P.sb([128, NT, 32], F32, "pr")
                sl = P.sb([128, NT, 2], F32, "sl")
                tr.I('dve', 'tensor_tensor', out=pr[:, :, :], in0=slot[:, :, :], in1=oh1[:, :, :], op=ALU.mult)
                tr.I('dve', 'tensor_reduce', out=sl[:, :, 0], in_=pr[:, :, :], axis=AX.X, op=ALU.add)
                tr.I('dve', 'tensor_tensor', out=pr[:, :, :], in0=slot[:, :, :], in1=sel[:, :, :], op=ALU.mult)
                tr.I('dve', 'tensor_reduce', out=sl[:, :, 1], in_=pr[:, :, :], axis=AX.X, op=ALU.add)
                tr.I('dve', 'tensor_tensor', out=sl[:, :, 1], in0=sl[:, :, 1], in1=sl[:, :, 0], op=ALU.subtract)
                tr.I('dve', 'tensor_copy', out=slot_i[:, :, :], in_=sl[:, :, :])
                be = P.sb([128, 64], F32, "be")
                tr.I('dve', 'tensor_scalar', out=be[:, :], in0=cst[:, 912:976], scalar1=cum[:, 0:1], scalar2=None, op0=ALU.is_ge)
                for e in range(1, 32):
                    tr.I('dve', 'scalar_tensor_tensor', out=be[:, :], in0=cst[:, 912:976], scalar=cum[:, e:e + 1], in1=be[:, :],
                         op0=ALU.is_ge, op1=ALU.add)
                flag = P.sb([128, 64], F32, "flag")
                tr.I('dve', 'tensor_scalar', out=flag[:, :], in0=be[:, :], scalar1=31.5, scalar2=1.0e6, op0=ALU.is_ge, op1=ALU.mult)
                tr.I('dve', 'tensor_scalar', out=be[:, :], in0=be[:, :], scalar1=31.0, scalar2=128.0, op0=ALU.min, op1=ALU.mult)
                tr.I('dve', 'tensor_scalar', out=be[:, :], in0=be[:, :], scalar1=cst[:, 976:977], scalar2=None, op0=ALU.add)
                tr.I('dve', 'scalar_tensor_tensor', out=be[:, :], in0=be[:, :], scalar=2.0, in1=flag[:, :], op0=ALU.mult, op1=ALU.add)
                be2 = P.sb([128, 64, 2], F32, "be2")
                for h4 in range(2):
                    tr.I('dve', 'tensor_scalar', out=be2[:, :, h4], in0=be[:, :], scalar1=float(li * 8192 + h4), scalar2=None,
                         op0=ALU.add)
                tr.I('dve', 'tensor_copy', out=widx[:, :, :], in_=be2[:, :, :])
            with Phase(tr, f"rl{li}") as Q:
                wt = Ring([Q.sb([128, 16, 512], BF16, "wt") for _ in range(4)])
                wts = []
                for cb in range(4):
                    w = wt.next()
                    _wload(tr, w[:, :, :], g['w_pg'][li], 0, 16, cb * 512, 512)
                    wts.append(w)
                xb = Ring([Q.sb([128, D], BF16, "xb") for _ in range(3)])
                for t in range(NT):
                    b = xb.next()
                    tr.dma('pool', b[:, :], XA[t * 128:(t + 1) * 128, :])
                    tr.dma('pool', XS[:, :], b[:, :], indirect=('out', slot_i[:, t, 0:1]))
                    tr.dma('pool', XS[:, :], b[:, :], indirect=('out', slot_i[:, t, 1:2]))
                pg = Ring([Q.ps([128, 512], F32, "pg") for _ in range(3)])
                pq = Ring([Q.ps([128, 512], F32, "pq") for _ in range(3)])
                sg = Ring([Q.sb([128, 512], F32, "sg") for _ in range(2)])
                x1 = Ring([Q.sb([128, 512], F32, "x1") for _ in range(3)])
                ro = Ring([Q.sb([128, 512], F32, "ro") for _ in range(3)])
                for cb in range(4):
                    cs_ = slice(cb * 512, (cb + 1) * 512)
                    w = wts[cb]
                    for t in range(NT):
                        x_ = x1.next()
                        tr.dma('act', x_[:, :], XA[t * 128:(t + 1) * 128, cs_])
                        a_ = pg.next()
                        for kc in range(16):
                            tr.I('pe', 'matmul', out=a_[:, :], lhsT=xT[:, kc, t * 128:(t + 1) * 128], rhs=w[:, kc, :],
                                 start=(kc == 0), stop=(kc == 15))
                        b_ = pq.next()
                        for kc in range(2):
                            tr.I('pe', 'matmul', out=b_[:, :], lhsT=pT[:, t, kc, :], rhs=Wpp[:, kc, cs_],
                                 start=(kc == 0), stop=(kc == 1))
                        s_ = sg.next()
                        tr.I('act', 'activation', out=s_[:, :], in_=a_[:, :], func=AF.Sigmoid)
                        tr.I('dve', 'tensor_tensor', out=s_[:, :], in0=s_[:, :], in1=b_[:, :], op=ALU.mult)
                        r_ = ro.next()
                        tr.I('dve', 'scalar_tensor_tensor', out=r_[:, :], in0=x_[:, :], scalar=ALPHA, in1=s_[:, :],
                             op0=ALU.mult, op1=ALU.add)
                        tr.dma('sp', R2[t * 128:(t + 1) * 128, cs_], r_[:, :])
        if stop_after == 'SC':
            return
        with Phase(tr, f"ex{li}") as P:
            stg = Ring([P.sb([128, 4096], F32, "stg") for _ in range(3)])
            wb = [[P.sb([128, 8192], BF16, f"wb{k}") for k in range(3)] for _ in range(2)]
            xs = Ring([P.sb([128, 2, D], BF16, "xs") for _ in range(2)])
            xsT = Ring([P.sb([128, 16, 256], BF16, "xsT") for _ in range(1)])
            tp = Ring([P.ps([128, 1024], BF16, "tp") for _ in range(2)])
            hp = Ring([P.ps([128, 1024], F32, "hp") for _ in range(2)])
            yp = Ring([P.ps([128, 512], F32, "yp") for _ in range(2)])
            hs = Ring([P.sb([128, 512], F32, "hs") for _ in range(1)])
            hb = Ring([P.sb([128, 512], BF16, "hb") for _ in range(2)])
            hT = Ring([P.sb([128, 4, 128], BF16, "hT") for _ in range(2)])
            ysb = Ring([P.sb([128, 1024], F32, "ysb") for _ in range(3)])
            w1v = g['w1'].rearrange("l (e p h c) f -> (l e p h) (c f)", p=128, h=2, c=8)
            w3v = g['w3'].rearrange("l (e p h c) f -> (l e p h) (c f)", p=128, h=2, c=8)
            w2v = g['w2'].rearrange("l (e p h c) n -> (l e p h) (c n)", p=128, h=2, c=2)
            wviews = (w1v, w3v, w2v)
            ceng = ['dve', 'dve', 'act', 'dve', 'dve', 'act']
            wbound = g['wbound_reg']

            def load_w(b, pieces):
                wset = wb[b % 2]
                for q_ in pieces:
                    k, h2 = q_ // 2, q_ % 2
                    s_ = stg.next()
                    tr.dma('pool', s_[:, :], wviews[k], indirect=('in', widx[:, b, h2:h2 + 1]), bounds_check=wbound,
                           oob_is_err=False)
                    dst_ = wset[k][:, h2 * 4096:(h2 + 1) * 4096]
                    ce = ceng[q_]
                    if ce == 'act':
                        tr.I('act', 'activation', out=dst_, in_=s_[:, :], func=AF.Copy)
                    else:
                        tr.I(ce, 'tensor_copy', out=dst_, in_=s_[:, :])

            def load_x(b):
                x_ = xs.next()
                tr.dma('sp', x_[:, :, :], XS[b * SLOTB:(b + 1) * SLOTB, :].rearrange("(h p) d -> p h d", p=128))
                return x_

            def xpose(x_):
                xt = xsT.next()
                for hf in range(2):
                    xv = x_.h[:, hf, :].rearrange("p (j c) -> p c j", c=16)
                    for half in range(2):
                        ps = tp.next()
                        for j in range(8):
                            c = half * 8 + j
                            tr.I('pe', 'transpose', out=ps[:, j * 128:(j + 1) * 128], in_=x_.v(xv[:, c, :]),
                                 identity=identb[:, :])
                        src = ps.v(ps.h[:, :].rearrange("p (j k) -> p j k", k=128))
                        dst = xt[:, half * 8:(half + 1) * 8, hf * 128:(hf + 1) * 128]
                        if half == 0:
                            tr.I('act', 'activation', out=dst, in_=src, func=AF.Copy)
                        else:
                            tr.I('dve', 'tensor_copy', out=dst, in_=src)
                return xt

            def compute_half(b, xt, hf):
                wset = wb[b % 2]
                h_ = hp.next()
                for k in range(2):
                    for c in range(16):
                        tr.I('pe', 'matmul', out=h_[:, k * 512:(k + 1) * 512], lhsT=xt[:, c, hf * 128:(hf + 1) * 128],
                             rhs=wset[k][:, c * 512:(c + 1) * 512], start=(c == 0), stop=(c == 15))
                s1 = hs.next()
                tr.I('act', 'activation', out=s1[:, :], in_=h_[:, 0:512], func=AF.Silu)
                hb_ = hb.next()
                tr.I('dve', 'tensor_tensor', out=hb_[:, :], in0=s1[:, :], in1=h_[:, 512:1024], op=ALU.mult)
                ps = tp.next()
                hv = hb_.h[:, :].rearrange("p (j c) -> p c j", c=4)
                for c in range(4):
                    tr.I('pe', 'transpose', out=ps[:, c * 128:(c + 1) * 128], in_=hb_.v(hv[:, c, :]), identity=identb[:, :])
                ht = hT.next()
                tr.I('act', 'activation', out=ht[:, :, :], in_=ps.v(ps.h[:, 0:512].rearrange("p (c k) -> p c k", k=128)),
                     func=AF.Copy)
                for cp in range(2):
                    y_ = ysb.next()
                    for c2 in range(2):
                        cb = cp * 2 + c2
                        yp_ = yp.next()
                        for c in range(4):
                            tr.I('pe', 'matmul', out=yp_[:, :], lhsT=ht[:, c, :],
                                 rhs=wset[2][:, c * D + cb * 512:c * D + (cb + 1) * 512], start=(c == 0), stop=(c == 3))
                        if c2 == 0:
                            tr.I('dve', 'tensor_copy', out=y_[:, c2 * 512:(c2 + 1) * 512], in_=yp_[:, :])
                        else:
                            tr.I('act', 'activation', out=y_[:, c2 * 512:(c2 + 1) * 512], in_=yp_[:, :], func=AF.Copy)
                    tr.dma('sp', YS[b * SLOTB + hf * 128:b * SLOTB + (hf + 1) * 128, cp * 1024:(cp + 1) * 1024], y_[:, :])

            load_w(0, range(6))
            x_next = load_x(0)
            for b in range(NBLK):
                x_cur = x_next
                if b + 1 < NBLK:
                    x_next = load_x(b + 1)
                xt = xpose(x_cur)
                if b + 1 < NBLK:
                    load_w(b + 1, range(0, 3))
                compute_half(b, xt, 0)
                if b + 1 < NBLK:
                    load_w(b + 1, range(3, 6))
                compute_half(b, xt, 1)
        with Phase(tr, f"cb{li}") as P:
            gbc = P.sb([128, D], F32, "gbc")
            bbc = P.sb([128, D], F32, "bbc")
            tr.dma('sp', gbc[:, :], g['ln2_g'][li, :].partition_broadcast(128))
            tr.dma('sp', bbc[:, :], g['ln2_b'][li, :].partition_broadcast(128))
            y1 = Ring([P.sb([128, D], F32, "y1") for _ in range(3)])
            y2 = Ring([P.sb([128, D], F32, "y2") for _ in range(3)])
            r2 = Ring([P.sb([128, D], F32, "r2") for _ in range(3)])
            def cb_load(t):
                a_, b_, r_ = y1.next(), y2.next(), r2.next()
                tr.dma('pool', a_[:, :], YS[:, :], indirect=('in', slot_i[:, t, 0:1]))
                tr.dma('pool', b_[:, :], YS[:, :], indirect=('in', slot_i[:, t, 1:2]))
                tr.dma('sp', r_[:, :], R2[t * 128:(t + 1) * 128, :])
                return a_, b_, r_

            nxt = cb_load(0)
            for t in range(NT):
                a_, b_, r_ = nxt
                if t + 1 < NT:
                    nxt = cb_load(t + 1)
                tr.I('dve', 'scalar_tensor_tensor', out=r_[:, :], in0=a_[:, :], scalar=gates[:, t, 0:1], in1=r_[:, :],
                     op0=ALU.mult, op1=ALU.add)
                tr.I('dve', 'scalar_tensor_tensor', out=r_[:, :], in0=b_[:, :], scalar=gates[:, t, 1:2], in1=r_[:, :],
                     op0=ALU.mult, op1=ALU.add)
                layer_norm_rows(tr, P, r_, r_, D, gbc, bbc, lnt)
                tr.dma('act', xout[t * 128:(t + 1) * 128, :], r_[:, :])


def make_consts(first_in_batch):
    c = np.zeros((128, 1024), np.float32)
    c[:, 0:128] = np.eye(128, dtype=np.float32)
    t = np.arange(128)[:, None]
    s = np.arange(128)[None, :]
    c[:, 128:256] = (s <= t).astype(np.float32)
    qi = np.arange(128)[:, None]
    kj = np.arange(256)[None, :]
    valid = (kj >= qi) & (kj <= qi + 128)
    m = np.where(valid, 0.0, NEG).astype(np.float32)
    c[:, 256:512] = m
    mh = m.copy()
    if first_in_batch:
        mh[:, 0:128] = NEG
    c[:, 512:768] = mh
    c[:, 768:784] = (500000.0 ** (-np.arange(0, 32, 2, dtype=np.float32) / 32)).astype(np.float32)[None, :]
    c[:, 784:912] = (t < s).astype(np.float32)
    c[:, 912:976] = np.arange(64, dtype=np.float32)[None, :]
    c[:, 976] = np.arange(128, dtype=np.float32)
    return c


def make_hidx(core):
    prev = (core % 4 - 1) % 4

    def row(e):
        e = np.asarray(e)
        out = np.zeros_like(e)
        off = 0
        for (r0, r1) in KV_CHUNKS:
            n_ = r1 - r0
            m = (e >= r0) & (e < r1)
            out = np.where(m, off + prev * n_ + (e - r0), out)
            off += 4 * n_
        return out

    cols = []
    i = np.arange(128)
    cols.append(row(5120 + i))
    cols.append(row(5248 + i))
    for r in range(4):
        cols.append(row(4096 + r + 4 * i))
        cols.append(row(4608 + r + 4 * i))
    for r in range(16):
        cols.append(row(r + 16 * i))
        cols.append(row(2048 + r + 16 * i))
    h = np.zeros((128, 48), np.int32)
    h[:, :len(cols)] = np.stack(cols, 1)
    return h


def core_inputs(core, inputs):
    b, q = core // 4, core % 4
    sl = slice(q * T, (q + 1) * T)
    m = {
        "x": np.ascontiguousarray(inputs["x"][b, sl]),
        "p": np.ascontiguousarray(inputs["p"][:, b, sl]),
        "pos": np.ascontiguousarray(inputs["positions"][b, sl].reshape(NT, 128).T.astype(np.int32)),
        "cst": make_consts(q == 0),
        "hidx": make_hidx(core),
    }
    for k in ("w_in", "w_s", "ln_v_g", "ln_v_b", "w_a", "w_b", "w_o", "ln1_g", "ln1_b", "w_grp", "b_grp",
              "w_rt", "b_rt", "w_pg", "w_pp", "ln2_g", "ln2_b"):
        m[k] = inputs[k]
    m["b_s"] = inputs["b_s"].reshape(-1, 8 * 128)
    m["w1"] = inputs["w1"].reshape(-1, 32 * D, 512)
    m["w3"] = inputs["w3"].reshape(-1, 32 * D, 512)
    m["w2"] = inputs["w2"].reshape(-1, 32 * 512, D)
    return m


_CACHE = {}


def kernel(**inputs):
    inputs = {k: np.asarray(v) for k, v in inputs.items()}
    if "nc" not in _CACHE:
        _CACHE["nc"] = build(nlayers=DEPTH, halo="ag")[0]
    nc = _CACHE["nc"]
    in_maps = [core_inputs(c, inputs) for c in range(NCORES)]
    res = run_bass_kernel_spmd(nc, in_maps, core_ids=list(range(NCORES)))
    out = np.zeros((2, 4 * T, D), np.float32)
    for c in range(NCORES):
        out[c // 4, (c % 4) * T:(c % 4 + 1) * T] = np.asarray(res.results[c]["y"])
    return out
```
